# Optimizing a Trainium2 kernel written in Bass

```python
import jax, jax.numpy as jnp
from jax import lax
import numpy as np

D_MODEL = 1024
BATCH = 4
SEQ = 4096
DEPTH = 4

D_MIX = D_MODEL
ATT_HEAD_DIM = 64
ATT_WIDTH = D_MIX // 2
ATT_HEADS = ATT_WIDTH // ATT_HEAD_DIM
DILATED_GROUPS = ((128, 1), (512, 4), (2048, 16))
ROPE_THETA = 500000.0
ROPE_DIM = ATT_HEAD_DIM // 4
MLSTM_WIDTH = D_MIX - ATT_WIDTH
MLSTM_HEADS = 4
MLSTM_HEAD_DIM = MLSTM_WIDTH // MLSTM_HEADS
MLSTM_CHUNK = 64
CONV_WIDTH = 5
N_GATES = 4
IN_WIDTH = 3 * ATT_WIDTH + 4 * MLSTM_WIDTH + N_GATES * MLSTM_HEADS
D_FF = ((8 * D_MODEL // 3 + 127) // 128) * 128
N_NORMS = 6
NORM_EPS = 1e-6
NEG_BIG = -1e30

kernel_name = "hybrid_dilated_attn_mlstm_macaron"


def rms_norm(x, gain):
    xf = x.astype(jnp.float32)
    y = xf * lax.rsqrt(jnp.mean(xf * xf, axis=-1, keepdims=True) + NORM_EPS)
    return (y * gain).astype(x.dtype)


def swiglu(h, w_in, w_out):
    gu = h @ w_in
    g, u = jnp.split(gu, 2, axis=-1)
    return (jax.nn.silu(g) * u) @ w_out


def split_heads(t, n_heads):
    b, s, w = t.shape
    return t.reshape(b, s, n_heads, w // n_heads).transpose(0, 2, 1, 3)


def merge_heads(t):
    b, h, s, dh = t.shape
    return t.transpose(0, 2, 1, 3).reshape(b, s, h * dh)


def partial_rotary(t, positions):
    half = ROPE_DIM // 2
    inv_freq = ROPE_THETA ** (-jnp.arange(half, dtype=jnp.float32) / half)
    ang = positions.astype(jnp.float32)[:, None] * inv_freq[None, :]
    cos, sin = jnp.cos(ang), jnp.sin(ang)
    t1 = t[..., :half].astype(jnp.float32)
    t2 = t[..., half:ROPE_DIM].astype(jnp.float32)
    rot = jnp.concatenate([t1 * cos - t2 * sin, t2 * cos + t1 * sin], axis=-1).astype(t.dtype)
    return jnp.concatenate([rot, t[..., ROPE_DIM:]], axis=-1)


def dilated_window_attention(q, k, v, window, dilation):
    b, h, s, dh = q.shape
    n_side = (window // 2) // dilation
    blk = n_side
    unit = dilation * blk
    s_pad = -(-s // unit) * unit
    n_blk = s_pad // unit
    sub_len = s_pad // dilation

    def to_blocks(t):
        t = jnp.pad(t, ((0, 0), (0, 0), (0, s_pad - s), (0, 0)))
        t = t.reshape(b, h, sub_len, dilation, dh).transpose(0, 1, 3, 2, 4)
        return t.reshape(b, h, dilation, n_blk, blk, dh)

    def neighbours(t):
        tp = jnp.pad(t, ((0, 0), (0, 0), (0, 0), (1, 1), (0, 0), (0, 0)))
        return jnp.concatenate([tp[:, :, :, :-2], tp[:, :, :, 1:-1], tp[:, :, :, 2:]], axis=4)

    qb = to_blocks(q)
    kn = neighbours(to_blocks(k))
    vn = neighbours(to_blocks(v))

    r = jnp.arange(dilation)[:, None, None, None]
    bi = jnp.arange(n_blk)[None, :, None, None]
    qi = jnp.arange(blk)[None, None, :, None]
    kj = jnp.arange(3 * blk)[None, None, None, :]
    m_q = bi * blk + qi
    m_k = bi * blk - blk + kj
    valid = (m_k >= 0) & (m_k * dilation + r < s) & (jnp.abs(m_q - m_k) <= n_side)

    scores = jnp.einsum('bhrnid,bhrnjd->bhrnij', qb, kn).astype(jnp.float32)
    scores = jnp.where(valid, scores, NEG_BIG)
    lse = jax.nn.logsumexp(scores, axis=-1)
    p = jnp.exp(scores - lse[..., None])
    out = jnp.einsum('bhrnij,bhrnjd->bhrnid', p, vn.astype(jnp.float32))

    def from_blocks(t):
        rest = t.shape[5:]
        t = t.reshape((b, h, dilation, sub_len) + rest)
        t = jnp.moveaxis(t, 2, 3).reshape((b, h, s_pad) + rest)
        return t[:, :, :s]

    return from_blocks(out), from_blocks(lse)


def dilated_attention_mixture(q, k, v):
    outs, lses = [], []
    for window, dilation in DILATED_GROUPS:
        o, l = dilated_window_attention(q, k, v, window, dilation)
        outs.append(o)
        lses.append(l)
    weights = jax.nn.softmax(jnp.stack(lses, axis=0), axis=0)
    return jnp.sum(weights[..., None] * jnp.stack(outs, axis=0), axis=0)


def mlstm_direction(q, k, v, i_pre, f_pre):
    b, h, s, dh = q.shape
    n_chunks = s // MLSTM_CHUNK
    log_f = jax.nn.log_sigmoid(f_pre)
    tril = jnp.tril(jnp.ones((MLSTM_CHUNK, MLSTM_CHUNK), dtype=bool))

    def chunks(t):
        return jnp.moveaxis(t.reshape((b, h, n_chunks, MLSTM_CHUNK) + t.shape[3:]), 2, 0)

    def step(carry, inp):
        c_mat, n_vec, m_prev = carry
        qc, kc, vc, ic, lfc = inp
        cum = jnp.cumsum(lfc, axis=-1)
        decay = cum[..., :, None] - cum[..., None, :] + ic[..., None, :]
        decay = jnp.where(tril, decay, -jnp.inf)
        inter = cum + m_prev[..., None]
        m_t = jnp.maximum(inter, jnp.max(decay, axis=-1))
        w_intra = jnp.exp(decay - m_t[..., None])
        w_inter = jnp.exp(inter - m_t)
        qk = jnp.einsum('bhtd,bhsd->bhts', qc, kc) * w_intra
        num = (jnp.einsum('bhts,bhse->bhte', qk, vc)
               + w_inter[..., None] * jnp.einsum('bhed,bhtd->bhte', c_mat, qc))
        den = jnp.sum(qk, axis=-1) + w_inter * jnp.einsum('bhd,bhtd->bht', n_vec, qc)
        h_out = num / jnp.maximum(jnp.abs(den), jnp.exp(-m_t))[..., None]
        total = cum[..., -1]
        w_src = total[..., None] - cum + ic
        m_new = jnp.maximum(total + m_prev, jnp.max(w_src, axis=-1))
        carry_scale = jnp.exp(total + m_prev - m_new)
        w_src = jnp.exp(w_src - m_new[..., None])
        c_new = carry_scale[..., None, None] * c_mat + jnp.einsum('bhs,bhse,bhsd->bhed', w_src, vc, kc)
        n_new = carry_scale[..., None] * n_vec + jnp.einsum('bhs,bhsd->bhd', w_src, kc)
        return (c_new, n_new, m_new), h_out

    init = (jnp.zeros((b, h, dh, dh), jnp.float32),
            jnp.zeros((b, h, dh), jnp.float32),
            jnp.full((b, h), NEG_BIG, jnp.float32))
    _, hs = lax.scan(step, init, (chunks(q), chunks(k), chunks(v), chunks(i_pre), chunks(log_f)))
    return jnp.moveaxis(hs, 0, 2).reshape(b, h, s, dh)


def token_mixer(h, w_in, conv_w, conv_b, gate_bias, mlstm_norm_gain, w_out):
    b, s, _ = h.shape
    proj = h @ w_in
    sizes = [ATT_WIDTH] * 3 + [MLSTM_WIDTH] * 4 + [N_GATES * MLSTM_HEADS]
    cuts = np.cumsum(sizes)[:-1].tolist()
    aq, ak, av, mq, mk, mv, mo, mg = jnp.split(proj, cuts, axis=-1)

    positions = jnp.arange(s)
    aq = partial_rotary(split_heads(aq, ATT_HEADS), positions) * (ATT_HEAD_DIM ** -0.5)
    ak = partial_rotary(split_heads(ak, ATT_HEADS), positions)
    av = split_heads(av, ATT_HEADS)
    attn_out = merge_heads(dilated_attention_mixture(aq, ak, av))

    qk = jnp.concatenate([mq, mk], axis=-1)
    qk = lax.conv_general_dilated(qk, conv_w.reshape(CONV_WIDTH, 1, 2 * MLSTM_WIDTH).astype(qk.dtype),
                                  window_strides=(1,), padding='SAME',
                                  dimension_numbers=('NWC', 'WIO', 'NWC'),
                                  feature_group_count=2 * MLSTM_WIDTH)
    qk = jax.nn.silu(qk + conv_b)
    mq, mk = jnp.split(qk, 2, axis=-1)
    mq = split_heads(mq, MLSTM_HEADS).astype(jnp.float32)
    mk = split_heads(mk, MLSTM_HEADS).astype(jnp.float32) * (MLSTM_HEAD_DIM ** -0.5)
    mv = split_heads(mv, MLSTM_HEADS).astype(jnp.float32)
    gates = mg.astype(jnp.float32).reshape(b, s, N_GATES, MLSTM_HEADS) + gate_bias
    gates = gates.transpose(2, 0, 3, 1)
    h_fwd = mlstm_direction(mq, mk, mv, gates[0], gates[1])
    flip = lambda t: jnp.flip(t, axis=2)
    h_bwd = flip(mlstm_direction(flip(mq), flip(mk), flip(mv), flip(gates[2]), flip(gates[3])))
    cell = h_fwd + h_bwd
    cell = cell * lax.rsqrt(jnp.mean(cell * cell, axis=-1, keepdims=True) + NORM_EPS)
    mlstm_out = jax.nn.sigmoid(mo.astype(jnp.float32)) * (merge_heads(cell) * mlstm_norm_gain)

    merged = jnp.concatenate([attn_out, mlstm_out], axis=-1).astype(h.dtype)
    return merged @ w_out


def setup_inputs(seed: int = 0) -> dict:
    key = jax.random.key(seed)
    ks = jax.random.split(key, 12)
    f32 = jnp.float32
    x = jax.random.normal(ks[0], (BATCH, SEQ, D_MODEL), f32)
    norm_gain = 1.0 + 0.05 * jax.random.normal(ks[1], (DEPTH, N_NORMS, D_MODEL), f32)
    ffn_w_in = jax.random.normal(ks[2], (DEPTH, 2, D_MODEL, 2 * D_FF), f32) * D_MODEL ** -0.5
    ffn_w_out = jax.random.normal(ks[3], (DEPTH, 2, D_FF, D_MODEL), f32) * D_FF ** -0.5
    mix_w_in = jax.random.normal(ks[4], (DEPTH, D_MODEL, IN_WIDTH), f32) * D_MODEL ** -0.5
    conv_w = jax.random.normal(ks[5], (DEPTH, CONV_WIDTH, 2 * MLSTM_WIDTH), f32) * CONV_WIDTH ** -0.5
    conv_b = 0.01 * jax.random.normal(ks[6], (DEPTH, 2 * MLSTM_WIDTH), f32)
    i_bias = 0.1 * jax.random.normal(ks[7], (DEPTH, 2, MLSTM_HEADS), f32)
    f_bias = (jnp.linspace(3.0, 6.0, MLSTM_HEADS, dtype=f32)[None, None, :]
              + 0.1 * jax.random.normal(ks[8], (DEPTH, 2, MLSTM_HEADS), f32))
    gate_bias = jnp.stack([i_bias[:, 0], f_bias[:, 0], i_bias[:, 1], f_bias[:, 1]], axis=1)
    mlstm_norm_gain = 1.0 + 0.05 * jax.random.normal(ks[9], (DEPTH, MLSTM_WIDTH), f32)
    mix_w_out = jax.random.normal(ks[10], (DEPTH, D_MIX, D_MODEL), f32) * D_MIX ** -0.5
    return {"x": x, "norm_gain": norm_gain, "ffn_w_in": ffn_w_in, "ffn_w_out": ffn_w_out,
            "mix_w_in": mix_w_in, "conv_w": conv_w, "conv_b": conv_b, "gate_bias": gate_bias,
            "mlstm_norm_gain": mlstm_norm_gain, "mix_w_out": mix_w_out}


def reference(x, norm_gain, ffn_w_in, ffn_w_out, mix_w_in, conv_w, conv_b, gate_bias,
              mlstm_norm_gain, mix_w_out):
    for layer in range(DEPTH):
        g = norm_gain[layer]
        h = swiglu(rms_norm(x, g[0]), ffn_w_in[layer, 0], ffn_w_out[layer, 0])
        x = x + 0.5 * rms_norm(h, g[1])
        h = token_mixer(rms_norm(x, g[2]), mix_w_in[layer], conv_w[layer], conv_b[layer],
                        gate_bias[layer], mlstm_norm_gain[layer], mix_w_out[layer])
        x = x + rms_norm(h, g[3])
        h = swiglu(rms_norm(x, g[4]), ffn_w_in[layer, 1], ffn_w_out[layer, 1])
        x = x + 0.5 * rms_norm(h, g[5])
    return x
```

```python
from contextlib import ExitStack
import math

import numpy as np
import ml_dtypes

import concourse.bass as bass
import concourse.mybir as mybir
from concourse.bass_utils import run_bass_kernel_spmd

F32 = mybir.dt.float32
BF16 = mybir.dt.bfloat16
AF = mybir.ActivationFunctionType
ALU = mybir.AluOpType
AX = mybir.AxisListType

D = 1024
DFF = 2816
NFC = DFF // 128
T = 2048
S = 4096
EPS = 1e-6
STOP = 0


class Buf:
    __slots__ = ("w", "r", "excl")

    def __init__(self, excl=False):
        self.w = {}
        self.r = {}
        self.excl = excl


class Ctx:
    def __init__(self, nc, es):
        self.nc = nc
        self.es = es
        self.eng = {"pe": nc.tensor, "act": nc.scalar, "dve": nc.vector, "pool": nc.gpsimd, "sp": nc.sync}
        self.prog = {}
        self.waited = {e: {} for e in self.eng}
        self.nsem = 0
        self.dsem = {}

    def newsem(self):
        self.nsem += 1
        return self.es.enter_context(self.nc.semaphore(f"s{self.nsem}"))

    def wait(self, e, toks):
        w = self.waited[e]
        need = {}
        for t in toks:
            if t is None:
                continue
            sem, val = t
            k = id(sem)
            if w.get(k, 0) < val and need.get(k, (None, 0))[1] < val:
                need[k] = (sem, val)
        for k, (sem, val) in need.items():
            self.eng[e].wait_ge(sem, val)
            w[k] = val

    def _deps(self, R, W):
        d = []
        for b in R:
            d += list(b.w.values())
        for b in W:
            d += list(b.w.values())
            d += list(b.r.values())
        return d

    def _mark(self, tok, R, W):
        k = id(tok[0])
        for b in R:
            if b.r.get(k, (None, 0))[1] < tok[1]:
                b.r[k] = tok
        for b in W:
            b.w = {k: tok}
            b.r = {}

    def _sig(self, e, ins):
        p = self.prog.get(e)
        if p is None or p[1] >= 30000:
            p = [self.newsem(), 0]
            self.prog[e] = p
        p[1] += 1
        ins.then_inc(p[0], 1)
        return (p[0], p[1])

    @staticmethod
    def _split(R, W):
        if any(b.excl for b in R):
            W = list(W) + [b for b in R if b.excl]
            R = [b for b in R if not b.excl]
        return R, W

    def op(self, e, fn, R=(), W=(), deps=()):
        R, W = self._split(R, W)
        self.wait(e, self._deps(R, W) + list(deps))
        ins = fn(self.eng[e])
        tok = self._sig(e, ins)
        self._mark(tok, R, W)
        return tok

    def mm(self, mms, R=(), W=(), deps=()):
        R, W = self._split(R, W)
        self.wait("pe", self._deps(R, W) + list(deps))
        ins = None
        for (o, l, r, st, sp) in mms:
            ins = self.nc.tensor.matmul(o, lhsT=l, rhs=r, start=st, stop=sp)
        tok = self._sig("pe", ins)
        self._mark(tok, R, W)
        return tok

    def dma(self, q, key, out, in_, R=(), W=(), deps=(), **kw):
        self.wait(q, self._deps(R, W) + list(deps))
        sem = self.dsem.get(key)
        if sem is None:
            sem = [self.newsem(), 0]
            self.dsem[key] = sem
        sem[1] += 16
        self.eng[q].dma_start(out=out, in_=in_, **kw).then_inc(sem[0], 16)
        tok = (sem[0], sem[1])
        self._mark(tok, R, W)
        return tok

    def dma_multi(self, q, key, pairs, R=(), W=(), **kw):
        self.wait(q, self._deps(R, W))
        sem = self.dsem.get(key)
        if sem is None:
            sem = [self.newsem(), 0]
            self.dsem[key] = sem
        for (o, i) in pairs:
            sem[1] += 16
            self.eng[q].dma_start(out=o, in_=i, **kw).then_inc(sem[0], 16)
        tok = (sem[0], sem[1])
        self._mark(tok, R, W)
        return tok

    def collective(self, kind, in_ap, out_ap, groups, R=(), W=()):
        self.wait("pool", self._deps(R, W))
        sem = self.dsem.get("cc")
        if sem is None:
            sem = [self.newsem(), 0]
            self.dsem["cc"] = sem
        sem[1] += 1
        self.nc.gpsimd.collective_compute(kind, ALU.bypass, replica_groups=groups, ins=[in_ap], outs=[out_ap]).then_inc(sem[0], 1)
        tok = (sem[0], sem[1])
        self._mark(tok, R, W)
        return tok

    def barrier(self, exclude=()):
        toks = [(p[0], p[1]) for p in self.prog.values()] + [(d[0], d[1]) for k, d in self.dsem.items() if k not in exclude]
        self.all_wait(toks)

    def all_wait(self, toks):
        for e in self.eng:
            self.wait(e, toks)


class Rot:
    def __init__(self, aps, excl=False):
        self.slots = [(a, Buf(excl)) for a in aps]
        self.i = 0

    def next(self):
        s = self.slots[self.i % len(self.slots)]
        self.i += 1
        return s


def _prenorm_tile(c, i, xa, xb, sa, sbuf_, col, hT_b, gpre, gpre_b, kc, psum, sb):
    nc = c.nc
    hT = sb["hT"]
    xsa, xsb = sb["xs"].next()
    c.op("act", lambda e: e.activation(out=xsa, in_=xa, func=AF.Square, accum_out=sa[:, col:col + 1]), R=[xb], W=[sbuf_, xsb])
    c.op("act", lambda e: e.activation(out=sa[:, col + 1:col + 2], in_=sa[:, col:col + 1], func=AF.Sqrt, scale=1.0 / D, bias=kc["eps"]),
         R=[sbuf_], W=[sbuf_])
    c.op("dve", lambda e: e.reciprocal(out=sa[:, col + 2:col + 3], in_=sa[:, col + 1:col + 2]), R=[sbuf_], W=[sbuf_])
    c.op("dve", lambda e: e.tensor_scalar(out=xsa, in0=xa, scalar1=sa[:, col + 2:col + 3], scalar2=None, op0=ALU.mult), R=[xb, sbuf_], W=[xsb])
    pa, pb = psum.next()
    pT = pa.bitcast(BF16).rearrange("p (k t) -> p k t", k=8)
    c.wait("pe", c._deps([xsb], [pb]))
    ins = None
    for k in range(8):
        ins = nc.tensor.transpose(pT[:, k, :], xsa[:, k * 128:(k + 1) * 128], kc["ident"])
    tok = c._sig("pe", ins)
    c._mark(tok, [xsb], [pb])
    c.op("dve", lambda e: e.tensor_tensor(out=hT[:, :, i * 128:(i + 1) * 128], in0=pT,
                                          in1=gpre.unsqueeze(2).to_broadcast([128, 8, 128]), op=ALU.mult),
         R=[pb, gpre_b], W=[hT_b[i]])


def prenorm_hT(c, x_in, g_pre, kc, psum, sb):
    NT = T // 128
    hT_b = [Buf() for _ in range(NT)]
    gpre_b = Buf()
    c.dma("sp", "gpre", sb["gpre"], g_pre.rearrange("(k p) -> p k", p=128), W=[gpre_b], allow_slow_non_contiguous=True)
    xt, st = sb["xt"], sb["stat"]
    for i in range(NT):
        xa, xb = xt.next()
        c.dma("sp", ("xt", xt.i % len(xt.slots)), xa, x_in[i * 128:(i + 1) * 128, :], W=[xb])
        sa, sbuf_ = st.next()
        _prenorm_tile(c, i, xa, xb, sa, sbuf_, 0, hT_b, sb["gpre"], gpre_b, kc, psum, sb)
    return hT_b


def out_phase(c, aT, a_bufs, nch, wo, wo_b, x_in, x_out, g_post, half, kc, psum, sb, tile_hook=None):
    NT = T // 128
    gpost_b = Buf()
    c.dma("sp", "gpost", sb["gpost"], g_post.partition_broadcast(128), W=[gpost_b])
    xt, st, ys = sb["xt"], sb["stat"], sb["y"]
    sc = 4.0 if half else 1.0
    epsb = kc["eps4"] if half else kc["eps"]
    toks = []
    for tt in range(NT):
        ya, yb = ys.next()
        xa, xb = xt.next()
        c.dma("sp", ("xt", xt.i % len(xt.slots)), xa, x_in[tt * 128:(tt + 1) * 128, :], W=[xb])
        sa, sbuf_ = st.next()
        for dh in range(2):
            pa, pb = psum.next()
            c.mm([(pa, aT[:, ch, tt * 128:(tt + 1) * 128], wo[:, ch, dh * 512:(dh + 1) * 512], ch == 0, ch == nch - 1) for ch in range(nch)],
                 R=[a_bufs[ch][tt // 4] for ch in range(nch)] + wo_b, W=[pb])
            c.op("dve", lambda e: e.tensor_copy(out=ya[:, dh * 512:(dh + 1) * 512], in_=pa), R=[pb], W=[yb])
            ja, jb = sb["tmp"].next()
            c.op("act", lambda e: e.activation(out=ja, in_=ya[:, dh * 512:(dh + 1) * 512], func=AF.Square,
                                               accum_out=sa[:, dh:dh + 1]), R=[yb], W=[sbuf_, jb])
        c.op("dve", lambda e: e.tensor_tensor(out=sa[:, 2:3], in0=sa[:, 0:1], in1=sa[:, 1:2], op=ALU.add), R=[sbuf_], W=[sbuf_])
        c.op("act", lambda e: e.activation(out=sa[:, 3:4], in_=sa[:, 2:3], func=AF.Sqrt, scale=sc / D, bias=epsb), R=[sbuf_], W=[sbuf_])
        c.op("dve", lambda e: e.reciprocal(out=sa[:, 4:5], in_=sa[:, 3:4]), R=[sbuf_], W=[sbuf_])
        c.op("dve", lambda e: e.scalar_tensor_tensor(out=ya, in0=ya, scalar=sa[:, 4:5], in1=sb["gpost"], op0=ALU.mult, op1=ALU.mult),
             R=[sbuf_, gpost_b, yb], W=[yb])
        c.op("dve", lambda e: e.tensor_tensor(out=ya, in0=ya, in1=xa, op=ALU.add), R=[xb, yb], W=[yb])
        toks.append(c.dma("sp", ("xo", tt % 4), x_out[tt * 128:(tt + 1) * 128, :], ya, R=[yb]))
        if tile_hook is not None:
            tile_hook(tt, ya, yb, sa, sbuf_)
    return toks


def ffn_stage(c, x_in, x_out, w_in, w_out, g_pre, g_post, kc, psum, sb, g_next=None):
    hT, aT, wo = sb["hT"], sb["aT"], sb["wo"]
    aT_b = [[Buf() for _ in range(T // 512)] for _ in range(NFC)]
    wo_b = [Buf() for _ in range(NFC)]
    win = sb["win"]
    w_in_v = w_in.rearrange("(k p) f -> p k f", p=128)
    win_loaded = {}
    win_ub = {}

    def load_win(fc):
        ap, b = win.next()
        bu = win_ub.setdefault(id(b), Buf())
        c.dma("pool", ("win", win.i % len(win.slots)), ap[:, 0], w_in_v[:, :, fc * 128:(fc + 1) * 128], W=[b])
        c.dma("pool", ("winu", win.i % len(win.slots)), ap[:, 1], w_in_v[:, :, DFF + fc * 128:DFF + (fc + 1) * 128], W=[bu])
        win_loaded[fc] = (ap, b, bu)

    NPRE = min(len(win.slots), NFC)
    for fc in range(NPRE):
        load_win(fc)
    hT_b = prenorm_hT(c, x_in, g_pre, kc, psum, sb)
    w_out_v = w_out.rearrange("(c p) d -> p c d", p=128)
    for fc in range(NFC):
        c.dma("pool", ("wo", fc), wo[:, fc, :], w_out_v[:, fc, :], W=[wo_b[fc]])
    tmp = sb["tmp"]
    for fc in range(NFC):
        wa, wb, wub = win_loaded.pop(fc)
        for tb in range(T // 512):
            pg, pgb = psum.next()
            pu, pub = psum.next()
            hb = hT_b[tb * 4:(tb + 1) * 4]
            c.mm([(pg, wa[:, 0, k, :], hT[:, k, tb * 512:(tb + 1) * 512], k == 0, k == 7) for k in range(8)], R=[wb] + hb, W=[pgb])
            c.mm([(pu, wa[:, 1, k, :], hT[:, k, tb * 512:(tb + 1) * 512], k == 0, k == 7) for k in range(8)], R=[wub] + hb, W=[pub])
            ta, tb_ = tmp.next()
            c.op("act", lambda e: e.activation(out=ta, in_=pg, func=AF.Silu), R=[pgb], W=[tb_])
            c.op("dve", lambda e: e.tensor_tensor(out=aT[:, fc, tb * 512:(tb + 1) * 512], in0=pu, in1=ta, op=ALU.mult),
                 R=[pub, tb_], W=[aT_b[fc][tb]])
        if fc + NPRE < NFC:
            load_win(fc + NPRE)
    hook = None
    if g_next is not None:
        gn_b = Buf()
        c.dma("sp", "gpre2", sb["gpre2"], g_next.rearrange("(k p) -> p k", p=128), W=[gn_b], allow_slow_non_contiguous=True)

        def hook(tt, ya, yb, sa, sbuf_):
            _prenorm_tile(c, tt, ya, yb, sa, sbuf_, 5, hT_b, sb["gpre2"], gn_b, kc, psum, sb)
    toks = out_phase(c, aT, aT_b, NFC, wo, wo_b, x_in, x_out, g_post, True, kc, psum, sb, tile_hook=hook)
    return (toks, hT_b) if g_next is not None else toks


def mixout_stage(c, x_in, x_out, mT_d, w_mo, g_post, kc, psum, sb, sel=None, dep=None):
    mT = sb["hT"]
    wo = sb["wo"]
    m_b = [[Buf() for _ in range(T // 512)] for _ in range(8)]
    wo_b = [Buf() for _ in range(8)]
    w_v = w_mo.rearrange("(c p) d -> p c d", p=128)
    R0 = [dep] if dep is not None else []
    if sel is not None:
        selb = Buf()
        c.dma("sp", "sel", sb["sel"], sel, W=[selb])
        tmp = sb["aT"]
    for ch in range(8):
        c.dma("pool", ("wo", ch), wo[:, ch, :], w_v[:, ch, :], W=[wo_b[ch]])
        if sel is None:
            c.dma("sp", ("mT", ch), mT[:, ch, :], mT_d[ch], R=R0, W=m_b[ch])
        else:
            tb_ = Buf()
            c.dma("sp", ("mT", ch), mT[:, ch, :], mT_d[ch][:, 0:T], R=R0, W=m_b[ch])
            c.dma("sp", ("mT2", ch), tmp[:, ch, :], mT_d[ch][:, T:2 * T], R=R0, W=[tb_])
            c.op("dve", lambda e: e.tensor_scalar(out=mT[:, ch, :], in0=mT[:, ch, :], scalar1=sb["sel"][:, 0:1], scalar2=None, op0=ALU.mult),
                 R=[selb], W=m_b[ch])
            c.op("dve", lambda e: e.scalar_tensor_tensor(out=mT[:, ch, :], in0=tmp[:, ch, :], scalar=sb["sel"][:, 1:2], in1=mT[:, ch, :],
                                                         op0=ALU.mult, op1=ALU.add), R=[selb, tb_], W=m_b[ch])
    return out_phase(c, mT, m_b, 8, wo, wo_b, x_in, x_out, g_post, False, kc, psum, sb)


def hT_stage(c, x_in, g_pre, hT_out, kc, psum, sb):
    hT_b = prenorm_hT(c, x_in, g_pre, kc, psum, sb)
    return [c.dma("sp", "hTo", hT_out.rearrange("(k p) t -> p k t", p=128), sb["hT"], R=hT_b)]


_TAG = [0]


def _t(nc, es, name, shape, dt):
    return es.enter_context(nc.sbuf_tensor(f"{name}_{_TAG[0]}", shape, dt))[:]


def alloc_ffn_sb(nc, es):
    _TAG[0] += 1
    sb = {}
    sb["hT"] = _t(nc, es, "hT", [128, 8, T], BF16)
    sb["aT"] = _t(nc, es, "aT", [128, NFC, T], BF16)
    sb["wo"] = _t(nc, es, "wo", [128, NFC, D], BF16)
    sb["win"] = [_t(nc, es, f"win{i}", [128, 2, 8, 128], BF16) for i in range(3)]
    sb["xt"] = [_t(nc, es, f"xt{i}", [128, D], F32) for i in range(2)]
    sb["xs"] = [_t(nc, es, f"xs{i}", [128, D], BF16) for i in range(2)]
    sb["y"] = [_t(nc, es, f"y{i}", [128, D], F32) for i in range(2)]
    sb["tmp"] = [_t(nc, es, f"tmp{i}", [128, 512], BF16) for i in range(3)]
    sb["stat"] = [_t(nc, es, f"stat{i}", [128, 8], F32) for i in range(4)]
    sb["gpre"] = _t(nc, es, "gpre", [128, 8], F32)
    sb["gpre2"] = _t(nc, es, "gpre2", [128, 8], F32)
    sb["gpost"] = _t(nc, es, "gpost", [128, D], F32)
    sb["sel"] = _t(nc, es, "sel", [128, 2], F32)
    for k in ("win", "xt", "xs", "y", "tmp", "stat"):
        sb[k] = Rot(sb[k])
    return sb


def setup_consts(c, es):
    nc = c.nc
    k = {}
    identf = es.enter_context(nc.sbuf_tensor("identf", [128, 128], F32))[:]
    k["ident"] = es.enter_context(nc.sbuf_tensor("ident", [128, 128], BF16))[:]
    k["eps"] = es.enter_context(nc.sbuf_tensor("eps", [128, 1], F32))[:]
    k["eps4"] = es.enter_context(nc.sbuf_tensor("eps4", [128, 1], F32))[:]
    k["one"] = es.enter_context(nc.sbuf_tensor("one", [128, 1], F32))[:]
    k["lnk"] = es.enter_context(nc.sbuf_tensor("lnk", [128, 1], F32))[:]
    k["identf"] = identf
    b = Buf()
    c.op("pool", lambda e: e.memset(identf, 1.0), W=[b])
    c.op("pool", lambda e: e.affine_select(out=identf, in_=identf, pattern=[[-1, 128]], compare_op=ALU.is_equal,
                                           fill=0.0, base=0, channel_multiplier=1), R=[b], W=[b])
    c.op("pool", lambda e: e.tensor_copy(out=k["ident"], in_=identf), R=[b], W=[b])
    c.op("pool", lambda e: e.memset(k["eps"], EPS), W=[b])
    c.op("pool", lambda e: e.memset(k["one"], 1.0), W=[b])
    c.op("pool", lambda e: e.memset(k["lnk"], -0.5 * math.log(128.0)), W=[b])
    t = c.op("pool", lambda e: e.memset(k["eps4"], 4.0 * EPS), W=[b])
    c.all_wait([t])
    return k


import math

I32 = mybir.dt.int32
NH_A = 4
DILS = (1, 4, 16)
ROPE_THETA = 500000.0


def setup_mixer_consts(c, es, rope_dram=None):
    nc = c.nc
    k = {}
    b = Buf()
    k["b"] = b

    def sbt(name, shape, dt):
        return es.enter_context(nc.sbuf_tensor(name, shape, dt))[:]

    k["ones128"] = sbt("ones128", [128, 128], F32)
    k["ones64"] = sbt("ones64", [128, 64], BF16)
    k["mask3"] = sbt("mask3", [128, 384], BF16)
    k["trif"] = sbt("trif", [128, 128], F32)
    k["trib"] = sbt("trib", [128, 128], F32)
    t8 = sbt("t8", [128, 8], F32)
    k["t8"] = t8
    k["selA"] = sbt("selA", [128, 128], F32)
    k["selB"] = sbt("selB", [128, 128], F32)
    c.op("pool", lambda e: e.memset(k["selA"], 0.0), W=[b])
    c.op("pool", lambda e: e.memset(k["selB"], 0.0), W=[b])
    c.op("pool", lambda e: e.memset(k["selA"][:, 0:64], 1.0), R=[b], W=[b])
    c.op("pool", lambda e: e.memset(k["selB"][:, 64:128], 1.0), R=[b], W=[b])
    c.op("pool", lambda e: e.affine_select(out=k["selA"][:, 0:64], in_=k["selA"][:, 0:64], pattern=[[-1, 64]], compare_op=ALU.is_equal,
                                           fill=0.0, base=-64, channel_multiplier=1), R=[b], W=[b])
    c.op("pool", lambda e: e.affine_select(out=k["selB"][:, 64:128], in_=k["selB"][:, 64:128], pattern=[[-1, 64]], compare_op=ALU.is_equal,
                                           fill=0.0, base=0, channel_multiplier=1), R=[b], W=[b])
    es_tmp = ExitStack()
    onesf = es_tmp.enter_context(nc.sbuf_tensor("onesf", [128, 384], F32))[:]
    c.op("pool", lambda e: e.memset(k["ones128"], 1.0), W=[b])
    c.op("pool", lambda e: e.memset(k["ones64"], 1.0), W=[b])
    c.op("pool", lambda e: e.memset(onesf, 1.0), W=[b])
    c.op("pool", lambda e: e.affine_select(out=onesf[:, 0:128], in_=onesf[:, 0:128], pattern=[[-1, 128]], compare_op=ALU.is_ge,
                                           fill=0.0, base=-64, channel_multiplier=1), R=[b], W=[b])
    c.op("pool", lambda e: e.affine_select(out=onesf[:, 128:256], in_=onesf[:, 128:256], pattern=[[1, 128]], compare_op=ALU.is_ge,
                                           fill=0.0, base=64, channel_multiplier=-1), R=[b], W=[b])
    c.op("pool", lambda e: e.affine_select(out=onesf[:, 128:256], in_=onesf[:, 128:256], pattern=[[-1, 128]], compare_op=ALU.is_ge,
                                           fill=0.0, base=64, channel_multiplier=1), R=[b], W=[b])
    c.op("pool", lambda e: e.affine_select(out=onesf[:, 256:384], in_=onesf[:, 256:384], pattern=[[1, 128]], compare_op=ALU.is_ge,
                                           fill=0.0, base=-64, channel_multiplier=-1), R=[b], W=[b])
    c.op("pool", lambda e: e.tensor_copy(out=k["mask3"], in_=onesf), R=[b], W=[b])
    c.op("pool", lambda e: e.affine_select(out=k["trif"], in_=k["ones128"], pattern=[[1, 128]], compare_op=ALU.is_ge,
                                           fill=0.0, base=0, channel_multiplier=-1), R=[b], W=[b])
    c.op("pool", lambda e: e.affine_select(out=k["trib"], in_=k["ones128"], pattern=[[-1, 128]], compare_op=ALU.is_ge,
                                           fill=0.0, base=0, channel_multiplier=1), R=[b], W=[b])
    if rope_dram is None:
        es_tmp.close()
        es_tmp = ExitStack()
    with es_tmp as es2:
        esr = es if rope_dram is None else es2
        k["ropeC"] = esr.enter_context(nc.sbuf_tensor("ropeC", [128, S], F32))[:]
        k["ropeS"] = esr.enter_context(nc.sbuf_tensor("ropeS", [128, S], F32))[:]
        def sbt2(name, shape, dt):
            return es2.enter_context(nc.sbuf_tensor(name, shape, dt))[:]
        posi = sbt2("posi", [128, S], I32)
        ang = sbt2("ang", [128, S], F32)
        kf = sbt2("kf", [128, S], F32)
        pidx = sbt2("pidx", [128, 1], I32)
        ti = sbt2("ti", [128, 2], I32)
        c.op("pool", lambda e: e.iota(posi, pattern=[[1, S]], base=0, channel_multiplier=0), W=[b])
        c.op("pool", lambda e: e.iota(pidx, pattern=[[0, 1]], base=0, channel_multiplier=1), R=[b], W=[b])
        c.op("dve", lambda e: e.tensor_single_scalar(out=ti[:, 0:1], in_=pidx, scalar=7, op=ALU.bitwise_and), R=[b], W=[b])
        c.op("dve", lambda e: e.tensor_single_scalar(out=ti[:, 1:2], in_=pidx, scalar=63, op=ALU.bitwise_and), R=[b], W=[b])
        c.op("dve", lambda e: e.tensor_copy(out=t8[:, 0:2], in_=ti), R=[b], W=[b])
        c.op("act", lambda e: e.activation(out=t8[:, 2:3], in_=t8[:, 0:1], func=AF.Exp, scale=-math.log(ROPE_THETA) / 8), R=[b], W=[b])
        c.op("dve", lambda e: e.tensor_single_scalar(out=t8[:, 3:4], in_=t8[:, 1:2], scalar=16.0, op=ALU.is_lt), R=[b], W=[b])
        c.op("dve", lambda e: e.tensor_tensor(out=t8[:, 4:5], in0=t8[:, 2:3], in1=t8[:, 3:4], op=ALU.mult), R=[b], W=[b])
        c.op("dve", lambda e: e.tensor_scalar(out=t8[:, 5:6], in0=t8[:, 1:2], scalar1=8.0, scalar2=2.0, op0=ALU.is_ge, op1=ALU.mult), R=[b], W=[b])
        c.op("dve", lambda e: e.tensor_scalar_add(out=t8[:, 5:6], in0=t8[:, 5:6], scalar1=-1.0), R=[b], W=[b])
        c.op("dve", lambda e: e.tensor_copy(out=ang, in_=posi), R=[b], W=[b])
        c.op("dve", lambda e: e.tensor_scalar(out=ang, in0=ang, scalar1=t8[:, 4:5], scalar2=None, op0=ALU.mult), R=[b], W=[b])
        C1 = 6.28125
        C2 = 2 * math.pi - C1
        tok = None
        for which, dst in (("sin", k["ropeS"]), ("cos", k["ropeC"])):
            if which == "cos":
                c.op("dve", lambda e: e.tensor_scalar_add(out=ang, in0=ang, scalar1=math.pi / 2), R=[b], W=[b])
            c.op("dve", lambda e: e.tensor_scalar(out=posi, in0=ang, scalar1=1.0 / (2 * math.pi), scalar2=None, op0=ALU.mult), R=[b], W=[b])
            c.op("dve", lambda e: e.tensor_copy(out=kf, in_=posi), R=[b], W=[b])
            c.op("dve", lambda e: e.scalar_tensor_tensor(out=dst, in0=kf, scalar=-C1, in1=ang, op0=ALU.mult, op1=ALU.add), R=[b], W=[b])
            c.op("dve", lambda e: e.scalar_tensor_tensor(out=dst, in0=kf, scalar=-C2, in1=dst, op0=ALU.mult, op1=ALU.add), R=[b], W=[b])
            c.op("dve", lambda e: e.tensor_single_scalar(out=kf, in_=dst, scalar=math.pi, op=ALU.is_gt), R=[b], W=[b])
            c.op("dve", lambda e: e.scalar_tensor_tensor(out=dst, in0=kf, scalar=-2 * math.pi, in1=dst, op0=ALU.mult, op1=ALU.add), R=[b], W=[b])
            c.op("dve", lambda e: e.tensor_single_scalar(out=kf, in_=dst, scalar=-math.pi, op=ALU.is_lt), R=[b], W=[b])
            c.op("dve", lambda e: e.scalar_tensor_tensor(out=dst, in0=kf, scalar=2 * math.pi, in1=dst, op0=ALU.mult, op1=ALU.add), R=[b], W=[b])
            tok = c.op("act", lambda e: e.activation(out=dst, in_=dst, func=AF.Sin), R=[b], W=[b])
            if which == "sin":
                tok = c.op("dve", lambda e: e.tensor_scalar(out=dst, in0=dst, scalar1=t8[:, 5:6], scalar2=None, op0=ALU.mult), R=[b], W=[b])
        if rope_dram is not None:
            t1 = c.dma("sp", "ropeC", rope_dram[0], k["ropeC"], R=[b])
            t2 = c.dma("sp", "ropeS", rope_dram[1], k["ropeS"], R=[b])
            c.all_wait([t1, t2])
            del k["ropeC"], k["ropeS"]
        c.all_wait([tok])
    return k


def attn_pass(c, j, hv, wA, Vd, mk, psum, sb, mergedT, merged_b, hdep=None):
    nc = c.nc
    NTB = S // 512
    wa, qT, kT = sb["wa"], sb["qT"], sb["kT"]
    acc = [sb["accN"], sb["accZ"]]
    wa_b, vd_b = Buf(), Buf()
    qk_b = [Buf() for _ in range(NTB)]
    acc_b = [Buf(), Buf()]
    c.dma("pool", "wa", wa, wA[j].rearrange("(k p) f -> p k f", p=128), W=[wa_b])
    hblk = Rot(sb["hblk"])
    tmpA = Rot(sb["tmpA"])
    vnat = Rot(sb["vnat"])
    loaded = {}

    def load_h(tb):
        ap, b = hblk.next()
        c.dma_multi("sp", ("hblk", hblk.i % 2), [(ap[:, 4 * i2:4 * i2 + 4, :], hv(tb // 4, i2)[:, :, (tb % 4) * 512:(tb % 4 + 1) * 512])
                                                    for i2 in range(2)], R=[hdep] if hdep else [], W=[b])
        loaded[tb] = (ap, b)

    kz_b = Buf()
    c.op("pool", lambda e: e.memset(kT[64:128, 0, :], 0.0), W=[kz_b])
    c.op("pool", lambda e: e.memset(kT[0:64, 1, :], 0.0), W=[kz_b])
    for (va, vb) in vnat.slots:
        v4 = va.rearrange("p (tt a c) -> p tt a c", a=2, c=128)
        c.op("pool", lambda e: e.memset(v4[:, :, 0, 64:128], 1.0), W=[vb])
        c.op("pool", lambda e: e.memset(v4[:, :, 1, 0:64], 1.0), W=[vb])
    load_h(0)
    for tb in range(NTB):
        if tb + 1 < NTB:
            load_h(tb + 1)
        ha, hb = loaded.pop(tb)
        sl = slice(tb * 512, (tb + 1) * 512)
        for (dst, c0) in ((qT, 0), (kT, 128)):
            pq, pqb = psum.next()
            ps, psb = psum.next()
            c.mm([(pq, wa[:, k, c0:c0 + 128], ha[:, k, :], k == 0, k == 7) for k in range(8)], R=[wa_b, hb], W=[pqb])
            c.mm([(ps, wa[:, k, 256 + c0:256 + c0 + 128], ha[:, k, :], k == 0, k == 7) for k in range(8)], R=[wa_b, hb], W=[psb])
            t1, t1b = tmpA.next()
            t2, t2b = tmpA.next()
            c.op("dve", lambda e: e.tensor_tensor(out=t1, in0=pq, in1=mk["ropeC"][:, sl], op=ALU.mult), R=[pqb, mk["b"]], W=[t1b])
            c.op("dve", lambda e: e.tensor_tensor(out=t2, in0=ps, in1=mk["ropeS"][:, sl], op=ALU.mult), R=[psb, mk["b"]], W=[t2b])
            if c0 == 0:
                c.op("pool", lambda e: e.tensor_tensor(out=dst[:, sl], in0=t1, in1=t2, op=ALU.add), R=[t1b, t2b], W=[qk_b[tb]])
            else:
                c.op("pool", lambda e: e.tensor_tensor(out=kT[0:64, 0, sl], in0=t1[0:64, :], in1=t2[0:64, :], op=ALU.add),
                     R=[t1b, t2b, kz_b], W=[qk_b[tb]])
                c.op("pool", lambda e: e.tensor_tensor(out=kT[64:128, 1, sl], in0=t1[64:128, :], in1=t2[64:128, :], op=ALU.add),
                     R=[t1b, t2b, kz_b], W=[qk_b[tb]])
        pv, pvb = psum.next()
        for tt in range(4):
            c.mm([(pv[:, tt * 128:(tt + 1) * 128], ha[:, k, tt * 128:(tt + 1) * 128], wa[:, k, 512:640], k == 0, k == 7)
                  for k in range(8)], R=[wa_b, hb], W=[pvb])
        va, vb = vnat.next()
        v4 = va.rearrange("p (tt a c) -> p tt a c", a=2, c=128)
        pv3 = pv.rearrange("p (tt c) -> p tt c", c=128)
        c.op("act", lambda e: e.activation(out=v4[:, :, 0, 0:64], in_=pv3[:, :, 0:64], func=AF.Copy), R=[pvb], W=[vb])
        c.op("act", lambda e: e.activation(out=v4[:, :, 1, 64:128], in_=pv3[:, :, 64:128], func=AF.Copy), R=[pvb], W=[vb])
        c.dma("sp", "vd", Vd[tb * 512:(tb + 1) * 512].rearrange("(tt p) a c -> p tt (a c)", p=128),
              va.rearrange("p (tt c) -> p tt c", c=256), R=[vb], W=[vd_b])

    vg = Rot(sb["vg"])
    ptr = Rot(sb["pt"])
    psS = Rot([a for a, _ in psum.slots[0:4]])
    psS.slots = psum.slots[0:4]
    psO = Rot([a for a, _ in psum.slots[4:8]])
    psO.slots = psum.slots[4:8]
    LAG = 3
    for gi, d in enumerate(DILS):
        L = S // d
        nb = L // 128
        vga, vgb = vg.next()
        c.dma_multi("sp", "vg", [(vga[:, r * nb:(r + 1) * nb, :],
                                  Vd.rearrange("(m r) a c -> r m (a c)", r=d)[r].rearrange("(kb p) c -> p kb c", p=128)) for r in range(d)],
                    R=[vd_b], W=[vgb])

        def tok(r, blk, n=1):
            st = r + d * 128 * blk
            return slice(st, st + d * (128 * n - 1) + 1, d)

        items = []
        for hh in range(2):
            for r in range(d):
                for b0 in range(0, nb, 4):
                    nbt = min(4, nb - b0)
                    for bi in range(nbt):
                        items.append((hh, r, b0, nbt, bi))
        state = {}
        pend = []

        def phaseA(it):
            hh, r, b0, nbt, bi = it
            P0 = hh * 64
            b = b0 + bi
            kbs = [kb for kb in (b - 1, b, b + 1) if 0 <= kb < nb]
            n = len(kbs) * 128
            ps, psb = psS.next()
            c.mm([(ps[:, ki * 128:(ki + 1) * 128], kT[:, hh, tok(r, kb)], qT[:, tok(r, b)], True, True)
                  for ki, kb in enumerate(kbs)], R=qk_b, W=[psb])
            pt, ptb = ptr.next()
            c.op("act", lambda e: e.activation(out=pt[:, 0:n], in_=ps[:, 0:n], func=AF.Exp, scale=0.125), R=[psb], W=[ptb])
            moff = 0 if kbs[0] == b - 1 else 128
            state["nmask"] = state.get("nmask", 0) + 1
            meng = "pool" if state["nmask"] % 3 == 0 else "dve"
            c.op(meng, lambda e: e.tensor_tensor(out=pt[:, 0:n], in0=pt[:, 0:n], in1=mk["mask3"][:, moff:moff + n], op=ALU.mult),
                 R=[ptb, mk["b"]], W=[ptb])
            return (pt, ptb, kbs)

        def phaseB(it, a):
            hh, r, b0, nbt, bi = it
            pt, ptb, kbs = a
            if bi == 0:
                state["pn"] = psO.next()
            pn, pnb = state["pn"]
            c.mm([(pn[:, bi * 128:(bi + 1) * 128], vga[:, r * nb + kb, hh * 128:(hh + 1) * 128], pt[:, ki * 128:(ki + 1) * 128],
                   ki == 0, ki == len(kbs) - 1) for ki, kb in enumerate(kbs)], R=[vgb, ptb], W=[pnb])
            if bi == nbt - 1:
                asl = tok(r, b0, nbt)
                w = nbt * 128
                if gi == 0:
                    c.op("dve", lambda e: e.tensor_copy(out=acc[hh][:, asl], in_=pn[:, 0:w]), R=[pnb], W=[acc_b[hh]])
                else:
                    c.op("dve", lambda e: e.tensor_tensor(out=acc[hh][:, asl], in0=pn[:, 0:w], in1=acc[hh][:, asl], op=ALU.add),
                         R=[pnb], W=[acc_b[hh]])

        for it in items:
            pend.append((it, phaseA(it)))
            if len(pend) > LAG:
                phaseB(*pend.pop(0))
        while pend:
            phaseB(*pend.pop(0))

    for q in range(S // 512):
        sl = slice(q * 512, (q + 1) * 512)
        pz, pzb = psum.next()
        c.mm([(pz, mk["selA"], acc[0][:, sl], True, False), (pz, mk["selB"], acc[1][:, sl], False, True)],
             R=[acc_b[0], acc_b[1], mk["b"]], W=[pzb])
        t2, t2b = tmpA.next()
        c.op("dve", lambda e: e.reciprocal(out=t2, in_=pz), R=[pzb], W=[t2b])
        c.op("dve", lambda e: e.tensor_tensor(out=mergedT[0:64, j, sl], in0=acc[0][0:64, sl], in1=t2[0:64, :], op=ALU.mult),
             R=[acc_b[0], t2b], W=[merged_b])
        c.op("pool", lambda e: e.tensor_tensor(out=mergedT[64:128, j, sl], in0=acc[1][64:128, sl], in1=t2[64:128, :], op=ALU.mult),
             R=[acc_b[1], t2b], W=[merged_b])


def alloc_attn_sb(nc, es):
    sb = {}

    _TAG[0] += 1

    def t(name, shape, dt):
        return _t(nc, es, name, shape, dt)
    sb["wa"] = t("wa", [128, 8, 640], BF16)
    sb["qT"] = t("qT", [128, S], BF16)
    sb["kT"] = t("kT", [128, 2, S], BF16)
    sb["accN"] = t("accN", [128, S], F32)
    sb["accZ"] = t("accZ", [128, S], F32)
    sb["hblk"] = [t(f"hblk{i}", [128, 8, 512], BF16) for i in range(2)]
    sb["tmpA"] = [t(f"tmpA{i}", [128, 512], F32) for i in range(4)]
    sb["vnat"] = [t(f"vnat{i}", [128, 1024], BF16) for i in range(2)]
    sb["vg"] = [t(f"vg{i}", [128, 32, 256], BF16) for i in range(1)]
    sb["pt"] = [t(f"pt{i}", [128, 384], BF16) for i in range(6)]
    return sb


def alloc_ml_sb(nc, es):
    sb = {}

    _TAG[0] += 1

    def t(name, shape, dt):
        return _t(nc, es, name, shape, dt)
    sb["wb"] = t("wb", [128, 8, 516], BF16)
    sb["hblk"] = [t(f"mhblk{i}", [128, 8, 512], BF16) for i in range(2)]
    sb["raw"] = t("raw", [128, 2, S + 4], BF16)
    sb["qkT"] = t("qkT", [128, 2, S], BF16)
    sb["sigo"] = t("sigo", [128, S], BF16)
    sb["vaug"] = t("vaug", [128, S // 128, 129], BF16)
    sb["ktok"] = t("ktok", [128, S // 128, 128], BF16)
    sb["hsum"] = t("hsum", [128, S // 128, 128], F32)
    sb["hb"] = t("hb", [128, S // 128, 128], F32)
    sb["yn"] = t("yn", [128, S // 128, 128], BF16)
    sb["cw"] = t("cw", [128, 2, 5], F32)
    sb["cb"] = t("cb", [128, 2], F32)
    sb["gb"] = t("gb", [128, 4], F32)
    sb["mg"] = t("mg", [128, 1], F32)
    sb["dg"] = t("dg", [128, 2, 5, 128], BF16)
    NCH = S // 128
    for nm in ("G", "Gb"):
        sb[nm] = t(nm, [128, NCH, 4], F32)
    for nm in ("E1", "LF", "CUM", "TOT", "I_", "B_", "EB", "EC", "ET", "W_", "TB", "DEN", "RR"):
        sb[nm] = t(nm, [128, 2, NCH], F32)
    sb["ssq"] = t("ssq", [128, NCH], F32)
    sb["rstd"] = t("mrstd", [128, NCH], F32)
    sb["CT"] = [t(f"CT{i}", [128, 129], F32) for i in range(2)]
    sb["CTb"] = [t(f"CTb{i}", [128, 129], BF16) for i in range(2)]
    sb["A"] = [t(f"A{i}", [128, 128], BF16) for i in range(6)]
    sb["kpp"] = [t(f"kpp{i}", [128, 128], BF16) for i in range(6)]
    sb["ep"] = [t(f"ep{i}", [128, 4], F32) for i in range(4)]
    sb["mjunk"] = t("mjunk", [128, 128], BF16)
    return sb


def mlstm_pass(c, hd, hv, wB, convw, convb, gbias, mgain, mk, kc, psum, sb, mergedT, merged_b, hdep=None):
    nc = c.nc
    NTB = S // 512
    NCH = S // 128
    wb, raw, qkT, sigo, vaug, ktok, hsum = sb["wb"], sb["raw"], sb["qkT"], sb["sigo"], sb["vaug"], sb["ktok"], sb["hsum"]
    wb_b, par_b, dg_b = Buf(), Buf(), Buf()
    raw_b = [Buf() for _ in range(NTB)]
    rawpad_b = Buf()
    qk_b = [Buf() for _ in range(NTB)]
    sig_b, va_b, g_b, kt_b, hs_b = Buf(), Buf(), Buf(), Buf(), Buf()
    c.dma("pool", "wb", wb, wB[hd].rearrange("(k p) f -> p k f", p=128), W=[wb_b])
    c.dma("sp", "cw", sb["cw"], convw[hd], W=[par_b])
    c.dma("sp", "cb", sb["cb"], convb[hd], W=[par_b])
    c.dma("sp", "gb", sb["gb"], gbias[hd].partition_broadcast(128), W=[par_b])
    c.dma("sp", "mg", sb["mg"], mgain[hd], W=[par_b])
    for qk in range(2):
        for jj in range(5):
            c.op("dve", lambda e: e.tensor_scalar(out=sb["dg"][:, qk, jj, :], in0=kc["identf"], scalar1=sb["cw"][:, qk, jj:jj + 1],
                                                  scalar2=None, op0=ALU.mult), R=[par_b], W=[dg_b])
    c.op("pool", lambda e: e.memset(raw[:, :, 0:2], 0.0), W=[rawpad_b])
    c.op("pool", lambda e: e.memset(raw[:, :, S + 2:S + 4], 0.0), W=[rawpad_b])
    c.op("pool", lambda e: e.memset(vaug[:, :, 128:129], 1.0), W=[va_b])

    hblk = Rot(sb["hblk"])
    loaded = {}

    def load_h(tb):
        ap, b = hblk.next()
        c.dma_multi("sp", ("mhblk", hblk.i % 2), [(ap[:, 4 * i2:4 * i2 + 4, :], hv(tb // 4, i2)[:, :, (tb % 4) * 512:(tb % 4 + 1) * 512])
                                                    for i2 in range(2)], R=[hdep] if hdep else [], W=[b])
        loaded[tb] = (ap, b)

    load_h(0)
    for tb in range(NTB):
        if tb + 1 < NTB:
            load_h(tb + 1)
        ha, hb = loaded.pop(tb)
        sl = slice(tb * 512, (tb + 1) * 512)
        for fi in range(3):
            pq, pqb = psum.next()
            c.mm([(pq, wb[:, k, fi * 128:(fi + 1) * 128], ha[:, k, :], k == 0, k == 7) for k in range(8)], R=[wb_b, hb], W=[pqb])
            if fi < 2:
                c.op("act", lambda e: e.activation(out=raw[:, fi, 2 + tb * 512:2 + (tb + 1) * 512], in_=pq, func=AF.Copy), R=[pqb], W=[raw_b[tb]])
            else:
                c.op("act", lambda e: e.activation(out=sigo[:, sl], in_=pq, func=AF.Sigmoid), R=[pqb], W=[sig_b])
        for half in range(2):
            pv, pvb = psum.next()
            for t2 in range(2):
                tt = half * 2 + t2
                c.mm([(pv[:, t2 * 132:(t2 + 1) * 132], ha[:, k, tt * 128:(tt + 1) * 128], wb[:, k, 384:516], k == 0, k == 7)
                      for k in range(8)], R=[wb_b, hb], W=[pvb])
            c0 = tb * 4 + half * 2
            pv3 = pv[:, 0:264].rearrange("p (t f) -> p t f", f=132)
            c.op("dve", lambda e: e.tensor_copy(out=vaug[:, c0:c0 + 2, 0:128], in_=pv3[:, :, 0:128]), R=[pvb], W=[va_b])
            c.op("dve", lambda e: e.tensor_copy(out=sb["G"][:, c0:c0 + 2, :], in_=pv3[:, :, 128:132]), R=[pvb], W=[g_b])

    for qk in range(2):
        for tb in range(NTB):
            pq, pqb = psum.next()
            rb = [raw_b[t] for t in (tb - 1, tb, tb + 1) if 0 <= t < NTB] + [rawpad_b, dg_b]
            c.mm([(pq, sb["dg"][:, qk, jj, :], raw[:, qk, tb * 512 + jj:tb * 512 + jj + 512], jj == 0, jj == 4) for jj in range(5)],
                 R=rb, W=[pqb])
            c.op("act", lambda e: e.activation(out=qkT[:, qk, tb * 512:(tb + 1) * 512], in_=pq, func=AF.Silu, bias=sb["cb"][:, qk:qk + 1]),
                 R=[pqb, par_b], W=[qk_b[tb]])
    for tb in range(NTB):
        pa, pb = psum.next()
        pT = pa.bitcast(BF16)[:, 0:512].rearrange("p (k t) -> p k t", k=4)
        c.wait("pe", c._deps([qk_b[tb]], [pb]))
        ins = None
        for k in range(4):
            ins = nc.tensor.transpose(pT[:, k, :], qkT[:, 1, tb * 512 + k * 128:tb * 512 + (k + 1) * 128], kc["ident"])
        tok = c._sig("pe", ins)
        c._mark(tok, [qk_b[tb]], [pb])
        c.op("dve", lambda e: e.tensor_copy(out=ktok[:, tb * 4:(tb + 1) * 4, :], in_=pT), R=[pb], W=[kt_b])

    G, Gb = sb["G"], sb["Gb"]
    gm = Buf()
    c.op("dve", lambda e: e.tensor_tensor(out=Gb, in0=G, in1=sb["gb"].unsqueeze(1).to_broadcast([128, NCH, 4]), op=ALU.add), R=[g_b, par_b], W=[gm])
    fview = Gb[:, :, 1::2].rearrange("p c g -> p g c")
    iview = Gb[:, :, 0::2].rearrange("p c g -> p g c")
    c.op("act", lambda e: e.activation(out=sb["E1"], in_=fview, func=AF.Exp, scale=-1.0), R=[gm], W=[gm])
    c.op("act", lambda e: e.activation(out=sb["E1"], in_=sb["E1"], func=AF.Ln, bias=kc["one"]), R=[gm], W=[gm])
    c.op("dve", lambda e: e.tensor_scalar(out=sb["LF"], in0=sb["E1"], scalar1=-1.0, scalar2=None, op0=ALU.mult), R=[gm], W=[gm])
    pg, pgb = psum.next()
    c.mm([(pg[:, 0:NCH], mk["trif"], sb["LF"][:, 0, :], True, True),
          (pg[:, NCH:2 * NCH], mk["trib"], sb["LF"][:, 1, :], True, True),
          (pg[:, 2 * NCH:4 * NCH], mk["ones128"], sb["LF"].rearrange("p a c -> p (a c)"), True, True)], R=[gm, mk["b"]], W=[pgb])
    c.op("dve", lambda e: e.tensor_copy(out=sb["CUM"].rearrange("p a c -> p (a c)"), in_=pg[:, 0:2 * NCH]), R=[pgb], W=[gm])
    c.op("dve", lambda e: e.tensor_copy(out=sb["TOT"].rearrange("p a c -> p (a c)"), in_=pg[:, 2 * NCH:4 * NCH]), R=[pgb], W=[gm])
    c.op("dve", lambda e: e.tensor_tensor(out=sb["B_"], in0=iview, in1=sb["CUM"], op=ALU.subtract), R=[gm], W=[gm])
    c.op("act", lambda e: e.activation(out=sb["EB"], in_=sb["B_"], func=AF.Exp, bias=kc["lnk"]), R=[gm], W=[gm])
    c.op("act", lambda e: e.activation(out=sb["EC"], in_=sb["CUM"], func=AF.Exp), R=[gm], W=[gm])
    c.op("act", lambda e: e.activation(out=sb["ET"], in_=sb["TOT"], func=AF.Exp), R=[gm], W=[gm])
    c.op("dve", lambda e: e.tensor_tensor(out=sb["TB"], in0=sb["TOT"], in1=sb["B_"], op=ALU.add), R=[gm], W=[gm])
    c.op("act", lambda e: e.activation(out=sb["W_"], in_=sb["TB"], func=AF.Exp, bias=kc["lnk"]), R=[gm], W=[gm])

    Arot, Krot = Rot(sb["A"]), Rot(sb["kpp"])
    den_b = Buf()
    ct_b = [Buf(), Buf()]
    ctb_b = [Buf(), Buf()]
    qall = qk_b
    def pre(i, dr):
        ch = i if dr == 0 else NCH - 1 - i
        cs = slice(ch * 128, (ch + 1) * 128)
        tri = mk["trif"] if dr == 0 else mk["trib"]
        ps, psb = psum.next()
        c.mm([(ps[:, 0:128], qkT[:, 1, cs], qkT[:, 0, cs], True, True)], R=qall, W=[psb])
        aa, ab = Arot.next()
        c.op("dve", lambda e: e.scalar_tensor_tensor(out=aa, in0=ps[:, 0:128], scalar=sb["EB"][:, dr, ch:ch + 1], in1=tri,
                                                     op0=ALU.mult, op1=ALU.mult), R=[psb, gm, mk["b"]], W=[ab])
        ka, kb_ = Krot.next()
        if i < NCH - 1:
            c.op("act", lambda e: e.activation(out=ka, in_=ktok[:, ch, :], func=AF.Copy, scale=sb["W_"][:, dr, ch:ch + 1]),
                 R=[kt_b, gm], W=[kb_])
        return (aa, ab, ka, kb_)

    pres = {(0, 0): pre(0, 0), (0, 1): pre(0, 1)}
    for i in range(NCH):
        for dr, ch in ((0, i), (1, NCH - 1 - i)):
            if i + 1 < NCH:
                pres[(i + 1, dr)] = pre(i + 1, dr)
            aa, ab, ka, kb_ = pres.pop((i, dr))
            cs = slice(ch * 128, (ch + 1) * 128)
            pn, pnb = psum.next()
            mms = [(pn[:, 0:129], aa, vaug[:, ch, :], True, i == 0)]
            if i > 0:
                mms.append((pn[:, 0:129], qkT[:, 0, cs], sb["CTb"][dr], False, True))
            c.mm(mms, R=[ab, va_b, ctb_b[dr]] + qall, W=[pnb])
            if i < NCH - 1:
                pc, pcb = psum.next()
                c.mm([(pc[:, 0:129], ka, vaug[:, ch, :], True, True)], R=[kb_, va_b], W=[pcb])
                if i == 0:
                    c.op("dve", lambda e: e.tensor_copy(out=sb["CT"][dr], in_=pc[:, 0:129]), R=[pcb], W=[ct_b[dr]])
                else:
                    c.op("dve", lambda e: e.scalar_tensor_tensor(out=sb["CT"][dr], in0=sb["CT"][dr], scalar=sb["ET"][:, dr, ch:ch + 1], in1=pc[:, 0:129],
                                                                 op0=ALU.mult, op1=ALU.add), R=[pcb, gm], W=[ct_b[dr]])
                c.op("act", lambda e: e.activation(out=sb["CTb"][dr], in_=sb["CT"][dr], func=AF.Copy), R=[ct_b[dr]], W=[ctb_b[dr]])
            hdst = hsum if dr == 0 else sb["hb"]
            c.op("act", lambda e: e.activation(out=sb["DEN"][:, dr, ch:ch + 1], in_=pn[:, 128:129], func=AF.Abs, scale=sb["EC"][:, dr, ch:ch + 1]),
                 R=[pnb, gm], W=[den_b])
            c.op("act", lambda e: e.activation(out=hdst[:, ch, :], in_=pn[:, 0:128], func=AF.Copy), R=[pnb], W=[hs_b])

    DEN, RR = sb["DEN"], sb["RR"]
    c.op("dve", lambda e: e.tensor_scalar_max(out=RR, in0=DEN, scalar1=1.0), R=[den_b], W=[den_b])
    c.op("dve", lambda e: e.reciprocal(out=RR, in_=RR), R=[den_b], W=[den_b])
    c.op("dve", lambda e: e.tensor_tensor(out=RR, in0=RR, in1=sb["EC"], op=ALU.mult), R=[den_b, gm], W=[den_b])
    c.op("dve", lambda e: e.tensor_tensor(out=hsum, in0=hsum, in1=RR[:, 0, :].unsqueeze(2).to_broadcast([128, NCH, 128]), op=ALU.mult),
         R=[den_b, hs_b], W=[hs_b])
    c.op("pool", lambda e: e.tensor_tensor(out=sb["hb"], in0=sb["hb"], in1=RR[:, 1, :].unsqueeze(2).to_broadcast([128, NCH, 128]), op=ALU.mult),
         R=[den_b, hs_b], W=[hs_b])
    c.op("dve", lambda e: e.tensor_tensor(out=hsum, in0=hsum, in1=sb["hb"], op=ALU.add), R=[hs_b], W=[hs_b])

    nb_ = Buf()
    for ch in range(NCH):
        c.op("act", lambda e: e.activation(out=sb["mjunk"], in_=hsum[:, ch, :], func=AF.Square, accum_out=sb["ssq"][:, ch:ch + 1]), R=[hs_b], W=[nb_])
    c.op("act", lambda e: e.activation(out=sb["rstd"], in_=sb["ssq"], func=AF.Sqrt, scale=1.0 / 128, bias=kc["eps"]), R=[nb_], W=[nb_])
    c.op("dve", lambda e: e.reciprocal(out=sb["rstd"], in_=sb["rstd"]), R=[nb_], W=[nb_])
    yn = sb["yn"]
    yn_b = Buf()
    c.op("dve", lambda e: e.tensor_tensor(out=yn, in0=hsum, in1=sb["rstd"].unsqueeze(2).to_broadcast([128, NCH, 128]), op=ALU.mult),
         R=[hs_b, nb_], W=[yn_b])
    for tb in range(NTB):
        pa, pb = psum.next()
        pT = pa.bitcast(BF16)[:, 0:512]
        c.wait("pe", c._deps([yn_b], [pb]))
        ins = None
        for k in range(4):
            ins = nc.tensor.transpose(pT[:, k * 128:(k + 1) * 128], yn[:, tb * 4 + k, :], kc["ident"])
        tok = c._sig("pe", ins)
        c._mark(tok, [yn_b], [pb])
        c.op("dve", lambda e: e.scalar_tensor_tensor(out=mergedT[:, 2 + hd, tb * 512:(tb + 1) * 512], in0=pT, scalar=sb["mg"][:, 0:1],
                                                     in1=sigo[:, tb * 512:(tb + 1) * 512], op0=ALU.mult, op1=ALU.mult),
             R=[pb, par_b, sig_b], W=[merged_b])


def build_ffn_prog():
    nc = bass.Bass("TRN2", target_bir_lowering=False)
    x_in = nc.dram_tensor("x", [T, D], F32, kind="ExternalInput").ap()
    w_in = nc.dram_tensor("w_in", [D, 2 * DFF], F32, kind="ExternalInput").ap()
    w_out = nc.dram_tensor("w_out", [DFF, D], F32, kind="ExternalInput").ap()
    g = nc.dram_tensor("g", [2, D], F32, kind="ExternalInput").ap()
    x_out = nc.dram_tensor("y", [T, D], F32, kind="ExternalOutput").ap()
    with ExitStack() as es:
        c = Ctx(nc, es)
        k = setup_consts(c, es)
        sb = alloc_ffn_sb(nc, es)
        psum = Rot([es.enter_context(nc.psum_tensor(f"ps{i}", [128, 512], F32))[:] for i in range(8)], excl=True)
        toks = ffn_stage(c, x_in, x_out, w_in, w_out, g[0], g[1], k, psum, sb)
        c.wait("sp", toks)
    return nc


def build_attn_test(j=0):
    nc = bass.Bass("TRN2", target_bir_lowering=False)
    hT_full = nc.dram_tensor("hT", [2, D, T], BF16, kind="ExternalInput").ap()
    wA = nc.dram_tensor("wA", [2, D, 640], F32, kind="ExternalInput").ap()
    out = nc.dram_tensor("mT", [128, S], BF16, kind="ExternalOutput").ap()
    Vd = nc.dram_tensor("Vd", [S, 2, 128], BF16).ap()
    with ExitStack() as es:
        c = Ctx(nc, es)
        mk = setup_mixer_consts(c, es)
        sb = alloc_attn_sb(nc, es)
        mergedT = es.enter_context(nc.sbuf_tensor("mergedT", [128, 4, S], BF16))[:]
        mb = Buf()
        psum = Rot([es.enter_context(nc.psum_tensor(f"ps{i}", [128, 512], F32))[:] for i in range(8)], excl=True)
        hv = lambda r, i: hT_full[r, i * 512:(i + 1) * 512].rearrange("(k p) t -> p k t", p=128)
        attn_pass(c, j, hv, wA, Vd, mk, psum, sb, mergedT, mb)
        t = c.dma("sp", "o", out, mergedT[:, j, :], R=[mb])
        c.wait("sp", [t])
    return nc


def layout_wA(w_in_l, hf):
    sw = np.concatenate([np.arange(8, 16), np.arange(0, 8), np.arange(16, 64)])
    out = np.empty((2, D, 640), np.float32)
    for j in range(2):
        cq, ck, cv, cqs, cks = [], [], [], [], []
        for hh in range(2):
            H = hf * 4 + 2 * j + hh
            base = np.arange(64) + H * 64
            cq.append(base); ck.append(512 + base); cv.append(1024 + base)
            cqs.append(base[sw]); cks.append(512 + base[sw])
        cols = np.concatenate(cq + ck + cqs + cks + cv)
        out[j] = w_in_l[:, cols]
    return out


def layout_wB(w_in_l, hf):
    out = np.empty((2, D, 516), np.float32)
    for hd in range(2):
        H = hf * 2 + hd
        base = np.arange(128) + H * 128
        cols = np.concatenate([1536 + base, 2048 + base, 3072 + base, 2560 + base, 3584 + np.arange(4) * 4 + H])
        out[hd] = w_in_l[:, cols]
    return out


def layout_ml_params(conv_w_l, conv_b_l, gate_bias_l, mgain_l, hf):
    cw = np.empty((2, 128, 2, 5), np.float32)
    cb = np.empty((2, 128, 2), np.float32)
    gb = np.empty((2, 4), np.float32)
    mg = np.empty((2, 128, 1), np.float32)
    for hd in range(2):
        H = hf * 2 + hd
        for qk in range(2):
            ch = qk * 512 + H * 128 + np.arange(128)
            cw[hd, :, qk, :] = conv_w_l[:, ch].T
            cb[hd, :, qk] = conv_b_l[ch]
        gb[hd] = gate_bias_l[:, H]
        mg[hd, :, 0] = mgain_l[H * 128:(H + 1) * 128]
    return cw, cb, gb, mg


def build_ml_test(hd=0):
    nc = bass.Bass("TRN2", target_bir_lowering=False)
    hT_full = nc.dram_tensor("hT", [2, D, T], BF16, kind="ExternalInput").ap()
    wB = nc.dram_tensor("wB", [2, D, 516], F32, kind="ExternalInput").ap()
    cw = nc.dram_tensor("cw", [2, 128, 2, 5], F32, kind="ExternalInput").ap()
    cb = nc.dram_tensor("cb", [2, 128, 2], F32, kind="ExternalInput").ap()
    gb = nc.dram_tensor("gb", [2, 4], F32, kind="ExternalInput").ap()
    mg = nc.dram_tensor("mg", [2, 128, 1], F32, kind="ExternalInput").ap()
    out = nc.dram_tensor("mT", [128, S], BF16, kind="ExternalOutput").ap()
    with ExitStack() as es:
        c = Ctx(nc, es)
        kc = setup_consts(c, es)
        mk = setup_mixer_consts(c, es)
        sb = alloc_ml_sb(nc, es)
        mergedT = es.enter_context(nc.sbuf_tensor("mergedT", [128, 4, S], BF16))[:]
        mb = Buf()
        psum = Rot([es.enter_context(nc.psum_tensor(f"ps{i}", [128, 512], F32))[:] for i in range(8)], excl=True)
        hv = lambda r, i: hT_full[r, i * 512:(i + 1) * 512].rearrange("(k p) t -> p k t", p=128)
        mlstm_pass(c, hd, hv, wB, cw, cb, gb, mg, mk, kc, psum, sb, mergedT, mb)
        t = c.dma("sp", "o", out, mergedT[:, 2 + hd, :], R=[mb])
        c.wait("sp", [t])
    return nc


def _psum(nc, es):
    return Rot([es.enter_context(nc.psum_tensor(f"ps{i}", [128, 512], F32))[:] for i in range(8)], excl=True)


def build_A(first, last):
    nc = bass.Bass("TRN2", target_bir_lowering=False)
    x = nc.dram_tensor("x", [T, D], F32, kind="ExternalInput").ap()
    if not first:
        mT = nc.dram_tensor("mT", [8, 128, T], BF16, kind="ExternalInput").ap()
        w_mo = nc.dram_tensor("w_mo", [D, D], F32, kind="ExternalInput").ap()
        w_in2 = nc.dram_tensor("w_in2", [D, 2 * DFF], F32, kind="ExternalInput").ap()
        w_out2 = nc.dram_tensor("w_out2", [DFF, D], F32, kind="ExternalInput").ap()
        g345 = nc.dram_tensor("g345", [3, D], F32, kind="ExternalInput").ap()
    if not last:
        w_in1 = nc.dram_tensor("w_in1", [D, 2 * DFF], F32, kind="ExternalInput").ap()
        w_out1 = nc.dram_tensor("w_out1", [DFF, D], F32, kind="ExternalInput").ap()
        g012 = nc.dram_tensor("g012", [3, D], F32, kind="ExternalInput").ap()
        hTo = nc.dram_tensor("hTo", [D, T], BF16, kind="ExternalOutput").ap()
    xo = nc.dram_tensor("xo", [T, D], F32, kind="ExternalOutput").ap()
    xa = nc.dram_tensor("xa", [T, D], F32).ap()
    xb = nc.dram_tensor("xb", [T, D], F32).ap()
    with ExitStack() as es:
        c = Ctx(nc, es)
        kc = setup_consts(c, es)
        sb = alloc_ffn_sb(nc, es)
        psum = _psum(nc, es)
        cur = x
        if not first:
            mixout_stage(c, cur, xa, mT, w_mo, g345[0], kc, psum, sb)
            c.barrier()
            ffn_stage(c, xa, xo if last else xb, w_in2, w_out2, g345[1], g345[2], kc, psum, sb)
            c.barrier()
            cur = xb
        if not last:
            ffn_stage(c, cur, xo, w_in1, w_out1, g012[0], g012[1], kc, psum, sb)
            c.barrier()
            hT_stage(c, xo, g012[2], hTo, kc, psum, sb)
        c.barrier()
    return nc


def build_B():
    nc = bass.Bass("TRN2", target_bir_lowering=False)
    hT_full = nc.dram_tensor("hT", [2, D, T], BF16, kind="ExternalInput").ap()
    wA = nc.dram_tensor("wA", [2, D, 640], F32, kind="ExternalInput").ap()
    wB = nc.dram_tensor("wB", [2, D, 516], F32, kind="ExternalInput").ap()
    cw = nc.dram_tensor("cw", [2, 128, 2, 5], F32, kind="ExternalInput").ap()
    cb = nc.dram_tensor("cb", [2, 128, 2], F32, kind="ExternalInput").ap()
    gb = nc.dram_tensor("gb", [2, 4], F32, kind="ExternalInput").ap()
    mg = nc.dram_tensor("mg", [2, 128, 1], F32, kind="ExternalInput").ap()
    out = nc.dram_tensor("mT", [4, 128, S], BF16, kind="ExternalOutput").ap()
    Vd = nc.dram_tensor("Vd", [S, 2, 128], BF16).ap()
    with ExitStack() as es:
        c = Ctx(nc, es)
        kc = setup_consts(c, es)
        mk = setup_mixer_consts(c, es)
        mergedT = es.enter_context(nc.sbuf_tensor("mergedT", [128, 4, S], BF16))[:]
        mb = Buf()
        psum = _psum(nc, es)
        hv = lambda r, i: hT_full[r, i * 512:(i + 1) * 512].rearrange("(k p) t -> p k t", p=128)
        mixer_core(c, nc, hv, wA, wB, cw, cb, gb, mg, Vd, mk, kc, psum, mergedT, mb)
        c.dma("sp", "mTo", out.rearrange("j p s -> p j s"), mergedT, R=[mb])
        c.barrier()
    return nc


def mixer_core(c, nc, hv, wA, wB, cw, cb, gb, mg, Vd, mk, kc, psum, mergedT, mb, hdep=None, after_tile=None):
    excl = ("cc", "mTo")
    with ExitStack() as es2:
        sba = alloc_attn_sb(nc, es2)
        for j in range(2):
            attn_pass(c, j, hv, wA, Vd, mk, psum, sba, mergedT, mb, hdep)
            if after_tile:
                after_tile(j)
            c.barrier(exclude=excl)
    with ExitStack() as es2:
        sbm = alloc_ml_sb(nc, es2)
        for hd in range(2):
            mlstm_pass(c, hd, hv, wB, cw, cb, gb, mg, mk, kc, psum, sbm, mergedT, mb, hdep)
            if after_tile:
                after_tile(2 + hd)
            c.barrier(exclude=excl)


def layout_wmo(w_out_l):
    rows = []
    for r in range(2):
        for j in range(2):
            for hh in range(2):
                H = r * 4 + 2 * j + hh
                rows.append(np.arange(64) + H * 64)
        for hd in range(2):
            H = r * 2 + hd
            rows.append(512 + np.arange(128) + H * 128)
    return np.ascontiguousarray(w_out_l[np.concatenate(rows)])


_PROGS = {}


def _prog(key, fn):
    if key not in _PROGS:
        _PROGS[key] = fn()
    return _PROGS[key]


DEPTH = 4
NCORES = 8
PAIRS = [[0, 1], [2, 3], [4, 5], [6, 7]]


def build_fused(depth=DEPTH):
    nc = bass.Bass("TRN2", target_bir_lowering=False)
    dt_in = lambda name, shape, dt=F32: nc.dram_tensor(name, shape, dt, kind="ExternalInput").ap()
    x = dt_in("x", [T, D])
    sel = dt_in("sel", [128, 2])
    ng = dt_in("ng", [depth, 6, D])
    fwi = dt_in("fwi", [depth, 2, D, 2 * DFF])
    fwo = dt_in("fwo", [depth, 2, DFF, D])
    wmo = dt_in("wmo", [depth, D, D])
    wA = dt_in("wA", [depth, 2, D, 640])
    wB = dt_in("wB", [depth, 2, D, 516])
    cw = dt_in("cw", [depth, 2, 128, 2, 5])
    cb = dt_in("cb", [depth, 2, 128, 2])
    gb = dt_in("gb", [depth, 2, 4])
    mg = dt_in("mg", [depth, 2, 128, 1])
    xo = nc.dram_tensor("xo", [T, D], F32, kind="ExternalOutput").ap()
    xs_ = [nc.dram_tensor(f"xs{i}", [T, D], F32).ap() for i in range(3)]
    hT_my = nc.dram_tensor("hT_my", [D, T], BF16).ap()
    hT_full = nc.dram_tensor("hT_full", [2, 2, 512, T], BF16).ap()
    mT_my = nc.dram_tensor("mT_my", [4, 128, S], BF16).ap()
    mT_all = nc.dram_tensor("mT_all", [4, 2, 128, S], BF16).ap()
    Vd = nc.dram_tensor("Vd", [S, 2, 128], BF16).ap()
    rope_d = [nc.dram_tensor(f"rope{i}", [128, S], F32).ap() for i in range(2)]
    with ExitStack() as es:
        c = Ctx(nc, es)
        kc = setup_consts(c, es)
        mk = setup_mixer_consts(c, es, rope_dram=rope_d)
        psum = _psum(nc, es)
        hv = lambda r, i: hT_full[i, r].rearrange("(k p) t -> p k t", p=128)
        hT_buf, mTm_buf, mTa_buf = Buf(), Buf(), Buf()
        cur = x
        for l in range(depth):
            with ExitStack() as es2:
                sb = alloc_ffn_sb(nc, es2)
                _, hT_b = ffn_stage(c, cur, xs_[0], fwi[l, 0], fwo[l, 0], ng[l, 0], ng[l, 1], kc, psum, sb, g_next=ng[l, 2])
                hbufs = [(Buf(), Buf()) for _ in range(2)]
                for i2 in range(2):
                    c.dma("sp", ("hTo", i2), hT_my[i2 * 512:(i2 + 1) * 512].rearrange("(k p) t -> p k t", p=128), sb["hT"][:, 4 * i2:4 * i2 + 4, :],
                          R=hT_b, W=[hbufs[i2][0]])
                    c.collective("AllGather", hT_my[i2 * 512:(i2 + 1) * 512], hT_full[i2].rearrange("r d t -> (r d) t"), PAIRS,
                                 R=[hbufs[i2][0]], W=[hbufs[i2][1]])
                c.barrier()
            with ExitStack() as es2:
                _TAG[0] += 1
                mk["ropeC"] = _t(nc, es2, "ropeC", [128, S], F32)
                mk["ropeS"] = _t(nc, es2, "ropeS", [128, S], F32)
                mergedT = _t(nc, es2, "mergedT", [128, 4, S], BF16)
                c.dma("sp", "ropeC", mk["ropeC"], rope_d[0], W=[mk["b"]])
                c.dma("sp", "ropeS", mk["ropeS"], rope_d[1], W=[mk["b"]])
                mb = Buf()
                tile_bufs = [(Buf(), Buf()) for _ in range(4)]

                def ship(j2, mergedT=mergedT, mb=mb, tile_bufs=tile_bufs):
                    b_in, b_out = tile_bufs[j2]
                    c.dma("sp", ("mTo", j2), mT_my[j2], mergedT[:, j2, :], R=[mb], W=[b_in])
                    c.collective("AllGather", mT_my[j2], mT_all[j2].rearrange("r p s -> (r p) s"), PAIRS, R=[b_in], W=[b_out])

                mixer_core(c, nc, hv, wA[l], wB[l], cw[l], cb[l], gb[l], mg[l], Vd, mk, kc, psum, mergedT, mb)
                c.barrier()
                for j2 in range(4):
                    ship(j2)
                c.barrier()
            with ExitStack() as es2:
                sb = alloc_ffn_sb(nc, es2)
                mixout_stage(c, xs_[0], xs_[1], [mT_all[ch % 4, ch // 4] for ch in range(8)], wmo[l], ng[l, 3], kc, psum, sb, sel=sel)
                c.barrier()
                nxt = xo if l == depth - 1 else xs_[2]
                ffn_stage(c, xs_[1], nxt, fwi[l, 1], fwo[l, 1], ng[l, 4], ng[l, 5], kc, psum, sb)
                c.barrier()
                cur = nxt
    return nc


def kernel(x, norm_gain, ffn_w_in, ffn_w_out, mix_w_in, conv_w, conv_b, gate_bias, mlstm_norm_gain, mix_w_out):
    x = np.asarray(x, np.float32)
    f32 = lambda a: np.ascontiguousarray(np.asarray(a, np.float32))
    norm_gain, ffn_w_in, ffn_w_out, mix_w_in = f32(norm_gain), f32(ffn_w_in), f32(ffn_w_out), f32(mix_w_in)
    conv_w, conv_b, gate_bias, mlstm_norm_gain, mix_w_out = f32(conv_w), f32(conv_b), f32(gate_bias), f32(mlstm_norm_gain), f32(mix_w_out)
    cores = list(range(NCORES))
    wmo = np.stack([layout_wmo(mix_w_out[l]) for l in range(DEPTH)])
    per_hf = []
    for hf in range(2):
        mlp = [layout_ml_params(conv_w[l], conv_b[l], gate_bias[l], mlstm_norm_gain[l], hf) for l in range(DEPTH)]
        selv = np.zeros((128, 2), np.float32)
        selv[:, hf] = 1.0
        per_hf.append({"wA": np.stack([layout_wA(mix_w_in[l], hf) for l in range(DEPTH)]),
                       "wB": np.stack([layout_wB(mix_w_in[l], hf) for l in range(DEPTH)]),
                       "cw": np.stack([m[0] for m in mlp]), "cb": np.stack([m[1] for m in mlp]),
                       "gb": np.stack([m[2] for m in mlp]), "mg": np.stack([m[3] for m in mlp]), "sel": selv})
    in_maps = []
    for c in cores:
        m = {"x": np.ascontiguousarray(x[c // 2, (c % 2) * T:(c % 2 + 1) * T]), "ng": norm_gain, "fwi": ffn_w_in, "fwo": ffn_w_out, "wmo": wmo}
        m.update(per_hf[c % 2])
        in_maps.append(m)
    nc = _prog("fused", build_fused)
    res = run_bass_kernel_spmd(nc, in_maps, core_ids=cores).results
    out = np.empty((4, S, D), np.float32)
    for c in cores:
        out[c // 2, (c % 2) * T:(c % 2 + 1) * T] = res[c]["xo"]
    return out


def kernel_unfused(x, norm_gain, ffn_w_in, ffn_w_out, mix_w_in, conv_w, conv_b, gate_bias, mlstm_norm_gain, mix_w_out):
    x = np.asarray(x, np.float32)
    f32 = lambda a: np.ascontiguousarray(np.asarray(a, np.float32))
    norm_gain, ffn_w_in, ffn_w_out, mix_w_in = f32(norm_gain), f32(ffn_w_in), f32(ffn_w_out), f32(mix_w_in)
    conv_w, conv_b, gate_bias, mlstm_norm_gain, mix_w_out = f32(conv_w), f32(conv_b), f32(gate_bias), f32(mlstm_norm_gain), f32(mix_w_out)
    cores = list(range(NCORES))
    xc = [np.ascontiguousarray(x[c // 2, (c % 2) * T:(c % 2 + 1) * T]) for c in cores]
    mT_my = None
    for l in range(DEPTH + 1):
        first, last = l == 0, l == DEPTH
        in_maps = []
        for c in cores:
            m = {"x": xc[c]}
            if not first:
                m["mT"] = mT_my[c]
                m["w_mo"] = layout_wmo(mix_w_out[l - 1])
                m["w_in2"] = ffn_w_in[l - 1, 1]
                m["w_out2"] = ffn_w_out[l - 1, 1]
                m["g345"] = np.ascontiguousarray(norm_gain[l - 1, 3:6])
            if not last:
                m["w_in1"] = ffn_w_in[l, 0]
                m["w_out1"] = ffn_w_out[l, 0]
                m["g012"] = np.ascontiguousarray(norm_gain[l, 0:3])
            in_maps.append(m)
        nc = _prog(("A", first, last), lambda: build_A(first, last))
        res = run_bass_kernel_spmd(nc, in_maps, core_ids=cores).results
        xc = [res[c]["xo"] for c in cores]
        if last:
            break
        hT = [res[c]["hTo"] for c in cores]
        in_maps = []
        for c in cores:
            b, hf = c // 2, c % 2
            cw_, cb_, gb_, mg_ = layout_ml_params(conv_w[l], conv_b[l], gate_bias[l], mlstm_norm_gain[l], hf)
            in_maps.append({"hT": np.ascontiguousarray(np.stack([hT[2 * b], hT[2 * b + 1]])),
                            "wA": layout_wA(mix_w_in[l], hf), "wB": layout_wB(mix_w_in[l], hf),
                            "cw": cw_, "cb": cb_, "gb": gb_, "mg": mg_})
        nc = _prog("B", build_B)
        res = run_bass_kernel_spmd(nc, in_maps, core_ids=cores).results
        mT = [res[c]["mT"] for c in cores]
        mT_my = []
        for c in cores:
            b, hf = c // 2, c % 2
            mT_my.append(np.ascontiguousarray(np.concatenate([mT[2 * b][:, :, hf * T:(hf + 1) * T], mT[2 * b + 1][:, :, hf * T:(hf + 1) * T]], axis=0)))
    out = np.empty((4, S, D), np.float32)
    for c in cores:
        out[c // 2, (c % 2) * T:(c % 2 + 1) * T] = xc[c]
    return out
```

```python
from contextlib import ExitStack
import math

import numpy as np
import ml_dtypes

import concourse.bass as bass
import concourse.mybir as mybir
from concourse.bass_utils import run_bass_kernel_spmd

F32 = mybir.dt.float32
BF16 = mybir.dt.bfloat16
AF = mybir.ActivationFunctionType
ALU = mybir.AluOpType
AX = mybir.AxisListType

D = 1024
DFF = 2816
NFC = DFF // 128
T = 2048
S = 4096
EPS = 1e-6
STOP = 0


class Buf:
    __slots__ = ("w", "r", "excl")

    def __init__(self, excl=False):
        self.w = {}
        self.r = {}
        self.excl = excl


class Ctx:
    def __init__(self, nc, es):
        self.nc = nc
        self.es = es
        self.eng = {"pe": nc.tensor, "act": nc.scalar, "dve": nc.vector, "pool": nc.gpsimd, "sp": nc.sync}
        self.prog = {}
        self.waited = {e: {} for e in self.eng}
        self.nsem = 0
        self.dsem = {}

    def newsem(self):
        self.nsem += 1
        return self.es.enter_context(self.nc.semaphore(f"s{self.nsem}"))

    def wait(self, e, toks):
        w = self.waited[e]
        need = {}
        for t in toks:
            if t is None:
                continue
            sem, val = t
            k = id(sem)
            if w.get(k, 0) < val and need.get(k, (None, 0))[1] < val:
                need[k] = (sem, val)
        for k, (sem, val) in need.items():
            self.eng[e].wait_ge(sem, val)
            w[k] = val

    def _deps(self, R, W):
        d = []
        for b in R:
            d += list(b.w.values())
        for b in W:
            d += list(b.w.values())
            d += list(b.r.values())
        return d

    def _mark(self, tok, R, W):
        k = id(tok[0])
        for b in R:
            if b.r.get(k, (None, 0))[1] < tok[1]:
                b.r[k] = tok
        for b in W:
            b.w = {k: tok}
            b.r = {}

    def _sig(self, e, ins):
        p = self.prog.get(e)
        if p is None or p[1] >= 30000:
            p = [self.newsem(), 0]
            self.prog[e] = p
        p[1] += 1
        ins.then_inc(p[0], 1)
        return (p[0], p[1])

    @staticmethod
    def _split(R, W):
        if any(b.excl for b in R):
            W = list(W) + [b for b in R if b.excl]
            R = [b for b in R if not b.excl]
        return R, W

    def op(self, e, fn, R=(), W=(), deps=()):
        R, W = self._split(R, W)
        self.wait(e, self._deps(R, W) + list(deps))
        ins = fn(self.eng[e])
        tok = self._sig(e, ins)
        self._mark(tok, R, W)
        return tok

    def mm(self, mms, R=(), W=(), deps=()):
        R, W = self._split(R, W)
        self.wait("pe", self._deps(R, W) + list(deps))
        ins = None
        for (o, l, r, st, sp) in mms:
            ins = self.nc.tensor.matmul(o, lhsT=l, rhs=r, start=st, stop=sp)
        tok = self._sig("pe", ins)
        self._mark(tok, R, W)
        return tok

    def dma(self, q, key, out, in_, R=(), W=(), deps=(), **kw):
        self.wait(q, self._deps(R, W) + list(deps))
        sem = self.dsem.get(key)
        if sem is None:
            sem = [self.newsem(), 0]
            self.dsem[key] = sem
        sem[1] += 16
        self.eng[q].dma_start(out=out, in_=in_, **kw).then_inc(sem[0], 16)
        tok = (sem[0], sem[1])
        self._mark(tok, R, W)
        return tok

    def dma_multi(self, q, key, pairs, R=(), W=(), **kw):
        self.wait(q, self._deps(R, W))
        sem = self.dsem.get(key)
        if sem is None:
            sem = [self.newsem(), 0]
            self.dsem[key] = sem
        for (o, i) in pairs:
            sem[1] += 16
            self.eng[q].dma_start(out=o, in_=i, **kw).then_inc(sem[0], 16)
        tok = (sem[0], sem[1])
        self._mark(tok, R, W)
        return tok

    def collective(self, kind, in_ap, out_ap, groups, R=(), W=()):
        self.wait("pool", self._deps(R, W))
        sem = self.dsem.get("cc")
        if sem is None:
            sem = [self.newsem(), 0]
            self.dsem["cc"] = sem
        sem[1] += 1
        self.nc.gpsimd.collective_compute(kind, ALU.bypass, replica_groups=groups, ins=[in_ap], outs=[out_ap]).then_inc(sem[0], 1)
        tok = (sem[0], sem[1])
        self._mark(tok, R, W)
        return tok

    def barrier(self, exclude=()):
        toks = [(p[0], p[1]) for p in self.prog.values()] + [(d[0], d[1]) for k, d in self.dsem.items() if k not in exclude]
        self.all_wait(toks)

    def all_wait(self, toks):
        for e in self.eng:
            self.wait(e, toks)


class Rot:
    def __init__(self, aps, excl=False):
        self.slots = [(a, Buf(excl)) for a in aps]
        self.i = 0

    def next(self):
        s = self.slots[self.i % len(self.slots)]
        self.i += 1
        return s


def prenorm_hT(c, x_in, g_pre, kc, psum, sb):
    nc = c.nc
    NT = T // 128
    hT = sb["hT"]
    hT_b = [Buf() for _ in range(NT)]
    gpre_b = Buf()
    c.dma("sp", "gpre", sb["gpre"], g_pre.rearrange("(k p) -> p k", p=128), W=[gpre_b], allow_slow_non_contiguous=True)
    xt, xs, st = sb["xt"], sb["xs"], sb["stat"]
    for i in range(NT):
        xa, xb = xt.next()
        c.dma("sp", ("xt", xt.i % len(xt.slots)), xa, x_in[i * 128:(i + 1) * 128, :], W=[xb])
        sa, sbuf_ = st.next()
        xsa, xsb = xs.next()
        c.op("act", lambda e: e.activation(out=xsa, in_=xa, func=AF.Square, accum_out=sa[:, 0:1]), R=[xb], W=[sbuf_, xsb])
        c.op("act", lambda e: e.activation(out=sa[:, 1:2], in_=sa[:, 0:1], func=AF.Sqrt, scale=1.0 / D, bias=kc["eps"]), R=[sbuf_], W=[sbuf_])
        c.op("dve", lambda e: e.reciprocal(out=sa[:, 2:3], in_=sa[:, 1:2]), R=[sbuf_], W=[sbuf_])
        c.op("dve", lambda e: e.tensor_scalar(out=xsa, in0=xa, scalar1=sa[:, 2:3], scalar2=None, op0=ALU.mult), R=[xb, sbuf_], W=[xsb])
        pa, pb = psum.next()
        pT = pa.bitcast(BF16).rearrange("p (k t) -> p k t", k=8)
        c.wait("pe", c._deps([xsb], [pb]))
        ins = None
        for k in range(8):
            ins = nc.tensor.transpose(pT[:, k, :], xsa[:, k * 128:(k + 1) * 128], kc["ident"])
        tok = c._sig("pe", ins)
        c._mark(tok, [xsb], [pb])
        c.op("dve", lambda e: e.tensor_tensor(out=hT[:, :, i * 128:(i + 1) * 128], in0=pT,
                                              in1=sb["gpre"].unsqueeze(2).to_broadcast([128, 8, 128]), op=ALU.mult),
             R=[pb, gpre_b], W=[hT_b[i]])
    return hT_b


def out_phase(c, aT, a_bufs, nch, wo, wo_b, x_in, x_out, g_post, half, kc, psum, sb):
    NT = T // 128
    gpost_b = Buf()
    c.dma("sp", "gpost", sb["gpost"], g_post.partition_broadcast(128), W=[gpost_b])
    xt, st, ys = sb["xt"], sb["stat"], sb["y"]
    sc = 4.0 if half else 1.0
    epsb = kc["eps4"] if half else kc["eps"]
    toks = []
    for tt in range(NT):
        ya, yb = ys.next()
        xa, xb = xt.next()
        c.dma("sp", ("xt", xt.i % len(xt.slots)), xa, x_in[tt * 128:(tt + 1) * 128, :], W=[xb])
        sa, sbuf_ = st.next()
        for dh in range(2):
            pa, pb = psum.next()
            c.mm([(pa, aT[:, ch, tt * 128:(tt + 1) * 128], wo[:, ch, dh * 512:(dh + 1) * 512], ch == 0, ch == nch - 1) for ch in range(nch)],
                 R=[a_bufs[ch][tt // 4] for ch in range(nch)] + wo_b, W=[pb])
            c.op("dve", lambda e: e.tensor_copy(out=ya[:, dh * 512:(dh + 1) * 512], in_=pa), R=[pb], W=[yb])
            ja, jb = sb["tmp"].next()
            c.op("act", lambda e: e.activation(out=ja, in_=ya[:, dh * 512:(dh + 1) * 512], func=AF.Square,
                                               accum_out=sa[:, dh:dh + 1]), R=[yb], W=[sbuf_, jb])
        c.op("dve", lambda e: e.tensor_tensor(out=sa[:, 2:3], in0=sa[:, 0:1], in1=sa[:, 1:2], op=ALU.add), R=[sbuf_], W=[sbuf_])
        c.op("act", lambda e: e.activation(out=sa[:, 3:4], in_=sa[:, 2:3], func=AF.Sqrt, scale=sc / D, bias=epsb), R=[sbuf_], W=[sbuf_])
        c.op("dve", lambda e: e.reciprocal(out=sa[:, 4:5], in_=sa[:, 3:4]), R=[sbuf_], W=[sbuf_])
        c.op("dve", lambda e: e.scalar_tensor_tensor(out=ya, in0=ya, scalar=sa[:, 4:5], in1=sb["gpost"], op0=ALU.mult, op1=ALU.mult),
             R=[sbuf_, gpost_b, yb], W=[yb])
        c.op("dve", lambda e: e.tensor_tensor(out=ya, in0=ya, in1=xa, op=ALU.add), R=[xb, yb], W=[yb])
        toks.append(c.dma("sp", ("xo", tt % 4), x_out[tt * 128:(tt + 1) * 128, :], ya, R=[yb]))
    return toks


def ffn_stage(c, x_in, x_out, w_in, w_out, g_pre, g_post, kc, psum, sb):
    hT, aT, wo = sb["hT"], sb["aT"], sb["wo"]
    aT_b = [[Buf() for _ in range(T // 512)] for _ in range(NFC)]
    wo_b = [Buf() for _ in range(NFC)]
    win = sb["win"]
    w_in_v = w_in.rearrange("(k p) f -> p k f", p=128)
    win_loaded = {}
    win_ub = {}

    def load_win(fc):
        ap, b = win.next()
        bu = win_ub.setdefault(id(b), Buf())
        c.dma("pool", ("win", win.i % len(win.slots)), ap[:, 0], w_in_v[:, :, fc * 128:(fc + 1) * 128], W=[b])
        c.dma("pool", ("winu", win.i % len(win.slots)), ap[:, 1], w_in_v[:, :, DFF + fc * 128:DFF + (fc + 1) * 128], W=[bu])
        win_loaded[fc] = (ap, b, bu)

    NPRE = min(len(win.slots), NFC)
    for fc in range(NPRE):
        load_win(fc)
    hT_b = prenorm_hT(c, x_in, g_pre, kc, psum, sb)
    w_out_v = w_out.rearrange("(c p) d -> p c d", p=128)
    for fc in range(NFC):
        c.dma("pool", ("wo", fc), wo[:, fc, :], w_out_v[:, fc, :], W=[wo_b[fc]])
    tmp = sb["tmp"]
    for fc in range(NFC):
        wa, wb, wub = win_loaded.pop(fc)
        for tb in range(T // 512):
            pg, pgb = psum.next()
            pu, pub = psum.next()
            hb = hT_b[tb * 4:(tb + 1) * 4]
            c.mm([(pg, wa[:, 0, k, :], hT[:, k, tb * 512:(tb + 1) * 512], k == 0, k == 7) for k in range(8)], R=[wb] + hb, W=[pgb])
            c.mm([(pu, wa[:, 1, k, :], hT[:, k, tb * 512:(tb + 1) * 512], k == 0, k == 7) for k in range(8)], R=[wub] + hb, W=[pub])
            ta, tb_ = tmp.next()
            c.op("act", lambda e: e.activation(out=ta, in_=pg, func=AF.Silu), R=[pgb], W=[tb_])
            c.op("dve", lambda e: e.tensor_tensor(out=aT[:, fc, tb * 512:(tb + 1) * 512], in0=pu, in1=ta, op=ALU.mult),
                 R=[pub, tb_], W=[aT_b[fc][tb]])
        if fc + NPRE < NFC:
            load_win(fc + NPRE)
    return out_phase(c, aT, aT_b, NFC, wo, wo_b, x_in, x_out, g_post, True, kc, psum, sb)


def mixout_stage(c, x_in, x_out, mT_d, w_mo, g_post, kc, psum, sb, sel=None, dep=None):
    mT = sb["hT"]
    wo = sb["wo"]
    m_b = [[Buf() for _ in range(T // 512)] for _ in range(8)]
    wo_b = [Buf() for _ in range(8)]
    w_v = w_mo.rearrange("(c p) d -> p c d", p=128)
    R0 = [dep] if dep is not None else []
    if sel is not None:
        selb = Buf()
        c.dma("sp", "sel", sb["sel"], sel, W=[selb])
        tmp = sb["aT"]
    for ch in range(8):
        c.dma("pool", ("wo", ch), wo[:, ch, :], w_v[:, ch, :], W=[wo_b[ch]])
        if sel is None:
            c.dma("sp", ("mT", ch), mT[:, ch, :], mT_d[ch], R=R0, W=m_b[ch])
        else:
            tb_ = Buf()
            c.dma("sp", ("mT", ch), mT[:, ch, :], mT_d[ch][:, 0:T], R=R0, W=m_b[ch])
            c.dma("sp", ("mT2", ch), tmp[:, ch, :], mT_d[ch][:, T:2 * T], R=R0, W=[tb_])
            c.op("dve", lambda e: e.tensor_scalar(out=mT[:, ch, :], in0=mT[:, ch, :], scalar1=sb["sel"][:, 0:1], scalar2=None, op0=ALU.mult),
                 R=[selb], W=m_b[ch])
            c.op("dve", lambda e: e.scalar_tensor_tensor(out=mT[:, ch, :], in0=tmp[:, ch, :], scalar=sb["sel"][:, 1:2], in1=mT[:, ch, :],
                                                         op0=ALU.mult, op1=ALU.add), R=[selb, tb_], W=m_b[ch])
    return out_phase(c, mT, m_b, 8, wo, wo_b, x_in, x_out, g_post, False, kc, psum, sb)


def hT_stage(c, x_in, g_pre, hT_out, kc, psum, sb):
    hT_b = prenorm_hT(c, x_in, g_pre, kc, psum, sb)
    return [c.dma("sp", "hTo", hT_out.rearrange("(k p) t -> p k t", p=128), sb["hT"], R=hT_b)]


_TAG = [0]


def _t(nc, es, name, shape, dt):
    return es.enter_context(nc.sbuf_tensor(f"{name}_{_TAG[0]}", shape, dt))[:]


def alloc_ffn_sb(nc, es):
    _TAG[0] += 1
    sb = {}
    sb["hT"] = _t(nc, es, "hT", [128, 8, T], BF16)
    sb["aT"] = _t(nc, es, "aT", [128, NFC, T], BF16)
    sb["wo"] = _t(nc, es, "wo", [128, NFC, D], BF16)
    sb["win"] = [_t(nc, es, f"win{i}", [128, 2, 8, 128], BF16) for i in range(3)]
    sb["xt"] = [_t(nc, es, f"xt{i}", [128, D], F32) for i in range(2)]
    sb["xs"] = [_t(nc, es, f"xs{i}", [128, D], BF16) for i in range(2)]
    sb["y"] = [_t(nc, es, f"y{i}", [128, D], F32) for i in range(2)]
    sb["tmp"] = [_t(nc, es, f"tmp{i}", [128, 512], BF16) for i in range(3)]
    sb["stat"] = [_t(nc, es, f"stat{i}", [128, 8], F32) for i in range(4)]
    sb["gpre"] = _t(nc, es, "gpre", [128, 8], F32)
    sb["gpost"] = _t(nc, es, "gpost", [128, D], F32)
    sb["sel"] = _t(nc, es, "sel", [128, 2], F32)
    for k in ("win", "xt", "xs", "y", "tmp", "stat"):
        sb[k] = Rot(sb[k])
    return sb


def setup_consts(c, es):
    nc = c.nc
    k = {}
    identf = es.enter_context(nc.sbuf_tensor("identf", [128, 128], F32))[:]
    k["ident"] = es.enter_context(nc.sbuf_tensor("ident", [128, 128], BF16))[:]
    k["eps"] = es.enter_context(nc.sbuf_tensor("eps", [128, 1], F32))[:]
    k["eps4"] = es.enter_context(nc.sbuf_tensor("eps4", [128, 1], F32))[:]
    k["one"] = es.enter_context(nc.sbuf_tensor("one", [128, 1], F32))[:]
    k["lnk"] = es.enter_context(nc.sbuf_tensor("lnk", [128, 1], F32))[:]
    k["identf"] = identf
    b = Buf()
    c.op("pool", lambda e: e.memset(identf, 1.0), W=[b])
    c.op("pool", lambda e: e.affine_select(out=identf, in_=identf, pattern=[[-1, 128]], compare_op=ALU.is_equal,
                                           fill=0.0, base=0, channel_multiplier=1), R=[b], W=[b])
    c.op("pool", lambda e: e.tensor_copy(out=k["ident"], in_=identf), R=[b], W=[b])
    c.op("pool", lambda e: e.memset(k["eps"], EPS), W=[b])
    c.op("pool", lambda e: e.memset(k["one"], 1.0), W=[b])
    c.op("pool", lambda e: e.memset(k["lnk"], -0.5 * math.log(128.0)), W=[b])
    t = c.op("pool", lambda e: e.memset(k["eps4"], 4.0 * EPS), W=[b])
    c.all_wait([t])
    return k


import math

I32 = mybir.dt.int32
NH_A = 4
DILS = (1, 4, 16)
ROPE_THETA = 500000.0


def setup_mixer_consts(c, es, rope_dram=None):
    nc = c.nc
    k = {}
    b = Buf()
    k["b"] = b

    def sbt(name, shape, dt):
        return es.enter_context(nc.sbuf_tensor(name, shape, dt))[:]

    k["ones128"] = sbt("ones128", [128, 128], F32)
    k["ones64"] = sbt("ones64", [128, 64], BF16)
    k["mask3"] = sbt("mask3", [128, 384], BF16)
    k["trif"] = sbt("trif", [128, 128], F32)
    k["trib"] = sbt("trib", [128, 128], F32)
    t8 = sbt("t8", [128, 8], F32)
    k["t8"] = t8
    k["selA"] = sbt("selA", [128, 128], F32)
    k["selB"] = sbt("selB", [128, 128], F32)
    c.op("pool", lambda e: e.memset(k["selA"], 0.0), W=[b])
    c.op("pool", lambda e: e.memset(k["selB"], 0.0), W=[b])
    c.op("pool", lambda e: e.memset(k["selA"][:, 0:64], 1.0), R=[b], W=[b])
    c.op("pool", lambda e: e.memset(k["selB"][:, 64:128], 1.0), R=[b], W=[b])
    c.op("pool", lambda e: e.affine_select(out=k["selA"][:, 0:64], in_=k["selA"][:, 0:64], pattern=[[-1, 64]], compare_op=ALU.is_equal,
                                           fill=0.0, base=-64, channel_multiplier=1), R=[b], W=[b])
    c.op("pool", lambda e: e.affine_select(out=k["selB"][:, 64:128], in_=k["selB"][:, 64:128], pattern=[[-1, 64]], compare_op=ALU.is_equal,
                                           fill=0.0, base=0, channel_multiplier=1), R=[b], W=[b])
    es_tmp = ExitStack()
    onesf = es_tmp.enter_context(nc.sbuf_tensor("onesf", [128, 384], F32))[:]
    c.op("pool", lambda e: e.memset(k["ones128"], 1.0), W=[b])
    c.op("pool", lambda e: e.memset(k["ones64"], 1.0), W=[b])
    c.op("pool", lambda e: e.memset(onesf, 1.0), W=[b])
    c.op("pool", lambda e: e.affine_select(out=onesf[:, 0:128], in_=onesf[:, 0:128], pattern=[[-1, 128]], compare_op=ALU.is_ge,
                                           fill=0.0, base=-64, channel_multiplier=1), R=[b], W=[b])
    c.op("pool", lambda e: e.affine_select(out=onesf[:, 128:256], in_=onesf[:, 128:256], pattern=[[1, 128]], compare_op=ALU.is_ge,
                                           fill=0.0, base=64, channel_multiplier=-1), R=[b], W=[b])
    c.op("pool", lambda e: e.affine_select(out=onesf[:, 128:256], in_=onesf[:, 128:256], pattern=[[-1, 128]], compare_op=ALU.is_ge,
                                           fill=0.0, base=64, channel_multiplier=1), R=[b], W=[b])
    c.op("pool", lambda e: e.affine_select(out=onesf[:, 256:384], in_=onesf[:, 256:384], pattern=[[1, 128]], compare_op=ALU.is_ge,
                                           fill=0.0, base=-64, channel_multiplier=-1), R=[b], W=[b])
    c.op("pool", lambda e: e.tensor_copy(out=k["mask3"], in_=onesf), R=[b], W=[b])
    c.op("pool", lambda e: e.affine_select(out=k["trif"], in_=k["ones128"], pattern=[[1, 128]], compare_op=ALU.is_ge,
                                           fill=0.0, base=0, channel_multiplier=-1), R=[b], W=[b])
    c.op("pool", lambda e: e.affine_select(out=k["trib"], in_=k["ones128"], pattern=[[-1, 128]], compare_op=ALU.is_ge,
                                           fill=0.0, base=0, channel_multiplier=1), R=[b], W=[b])
    if rope_dram is None:
        es_tmp.close()
        es_tmp = ExitStack()
    with es_tmp as es2:
        esr = es if rope_dram is None else es2
        k["ropeC"] = esr.enter_context(nc.sbuf_tensor("ropeC", [128, S], F32))[:]
        k["ropeS"] = esr.enter_context(nc.sbuf_tensor("ropeS", [128, S], F32))[:]
        def sbt2(name, shape, dt):
            return es2.enter_context(nc.sbuf_tensor(name, shape, dt))[:]
        posi = sbt2("posi", [128, S], I32)
        ang = sbt2("ang", [128, S], F32)
        kf = sbt2("kf", [128, S], F32)
        pidx = sbt2("pidx", [128, 1], I32)
        ti = sbt2("ti", [128, 2], I32)
        c.op("pool", lambda e: e.iota(posi, pattern=[[1, S]], base=0, channel_multiplier=0), W=[b])
        c.op("pool", lambda e: e.iota(pidx, pattern=[[0, 1]], base=0, channel_multiplier=1), R=[b], W=[b])
        c.op("dve", lambda e: e.tensor_single_scalar(out=ti[:, 0:1], in_=pidx, scalar=7, op=ALU.bitwise_and), R=[b], W=[b])
        c.op("dve", lambda e: e.tensor_single_scalar(out=ti[:, 1:2], in_=pidx, scalar=63, op=ALU.bitwise_and), R=[b], W=[b])
        c.op("dve", lambda e: e.tensor_copy(out=t8[:, 0:2], in_=ti), R=[b], W=[b])
        c.op("act", lambda e: e.activation(out=t8[:, 2:3], in_=t8[:, 0:1], func=AF.Exp, scale=-math.log(ROPE_THETA) / 8), R=[b], W=[b])
        c.op("dve", lambda e: e.tensor_single_scalar(out=t8[:, 3:4], in_=t8[:, 1:2], scalar=16.0, op=ALU.is_lt), R=[b], W=[b])
        c.op("dve", lambda e: e.tensor_tensor(out=t8[:, 4:5], in0=t8[:, 2:3], in1=t8[:, 3:4], op=ALU.mult), R=[b], W=[b])
        c.op("dve", lambda e: e.tensor_scalar(out=t8[:, 5:6], in0=t8[:, 1:2], scalar1=8.0, scalar2=2.0, op0=ALU.is_ge, op1=ALU.mult), R=[b], W=[b])
        c.op("dve", lambda e: e.tensor_scalar_add(out=t8[:, 5:6], in0=t8[:, 5:6], scalar1=-1.0), R=[b], W=[b])
        c.op("dve", lambda e: e.tensor_copy(out=ang, in_=posi), R=[b], W=[b])
        c.op("dve", lambda e: e.tensor_scalar(out=ang, in0=ang, scalar1=t8[:, 4:5], scalar2=None, op0=ALU.mult), R=[b], W=[b])
        C1 = 6.28125
        C2 = 2 * math.pi - C1
        tok = None
        for which, dst in (("sin", k["ropeS"]), ("cos", k["ropeC"])):
            if which == "cos":
                c.op("dve", lambda e: e.tensor_scalar_add(out=ang, in0=ang, scalar1=math.pi / 2), R=[b], W=[b])
            c.op("dve", lambda e: e.tensor_scalar(out=posi, in0=ang, scalar1=1.0 / (2 * math.pi), scalar2=None, op0=ALU.mult), R=[b], W=[b])
            c.op("dve", lambda e: e.tensor_copy(out=kf, in_=posi), R=[b], W=[b])
            c.op("dve", lambda e: e.scalar_tensor_tensor(out=dst, in0=kf, scalar=-C1, in1=ang, op0=ALU.mult, op1=ALU.add), R=[b], W=[b])
            c.op("dve", lambda e: e.scalar_tensor_tensor(out=dst, in0=kf, scalar=-C2, in1=dst, op0=ALU.mult, op1=ALU.add), R=[b], W=[b])
            c.op("dve", lambda e: e.tensor_single_scalar(out=kf, in_=dst, scalar=math.pi, op=ALU.is_gt), R=[b], W=[b])
            c.op("dve", lambda e: e.scalar_tensor_tensor(out=dst, in0=kf, scalar=-2 * math.pi, in1=dst, op0=ALU.mult, op1=ALU.add), R=[b], W=[b])
            c.op("dve", lambda e: e.tensor_single_scalar(out=kf, in_=dst, scalar=-math.pi, op=ALU.is_lt), R=[b], W=[b])
            c.op("dve", lambda e: e.scalar_tensor_tensor(out=dst, in0=kf, scalar=2 * math.pi, in1=dst, op0=ALU.mult, op1=ALU.add), R=[b], W=[b])
            tok = c.op("act", lambda e: e.activation(out=dst, in_=dst, func=AF.Sin), R=[b], W=[b])
            if which == "sin":
                tok = c.op("dve", lambda e: e.tensor_scalar(out=dst, in0=dst, scalar1=t8[:, 5:6], scalar2=None, op0=ALU.mult), R=[b], W=[b])
        if rope_dram is not None:
            t1 = c.dma("sp", "ropeC", rope_dram[0], k["ropeC"], R=[b])
            t2 = c.dma("sp", "ropeS", rope_dram[1], k["ropeS"], R=[b])
            c.all_wait([t1, t2])
            del k["ropeC"], k["ropeS"]
        c.all_wait([tok])
    return k


def attn_pass(c, j, hv, wA, Vd, mk, psum, sb, mergedT, merged_b, hdep=None):
    nc = c.nc
    NTB = S // 512
    wa, qT, kT = sb["wa"], sb["qT"], sb["kT"]
    acc = [sb["accN"], sb["accZ"]]
    wa_b, vd_b = Buf(), Buf()
    qk_b = [Buf() for _ in range(NTB)]
    acc_b = [Buf(), Buf()]
    c.dma("pool", "wa", wa, wA[j].rearrange("(k p) f -> p k f", p=128), W=[wa_b])
    hblk = Rot(sb["hblk"])
    tmpA = Rot(sb["tmpA"])
    vnat = Rot(sb["vnat"])
    loaded = {}

    def load_h(tb):
        ap, b = hblk.next()
        c.dma_multi("sp", ("hblk", hblk.i % 2), [(ap[:, 4 * i2:4 * i2 + 4, :], hv(tb // 4, i2)[:, :, (tb % 4) * 512:(tb % 4 + 1) * 512])
                                                    for i2 in range(2)], R=[hdep] if hdep else [], W=[b])
        loaded[tb] = (ap, b)

    kz_b = Buf()
    c.op("pool", lambda e: e.memset(kT[64:128, 0, :], 0.0), W=[kz_b])
    c.op("pool", lambda e: e.memset(kT[0:64, 1, :], 0.0), W=[kz_b])
    for (va, vb) in vnat.slots:
        v4 = va.rearrange("p (tt a c) -> p tt a c", a=2, c=128)
        c.op("pool", lambda e: e.memset(v4[:, :, 0, 64:128], 1.0), W=[vb])
        c.op("pool", lambda e: e.memset(v4[:, :, 1, 0:64], 1.0), W=[vb])
    load_h(0)
    for tb in range(NTB):
        if tb + 1 < NTB:
            load_h(tb + 1)
        ha, hb = loaded.pop(tb)
        sl = slice(tb * 512, (tb + 1) * 512)
        for (dst, c0) in ((qT, 0), (kT, 128)):
            pq, pqb = psum.next()
            ps, psb = psum.next()
            c.mm([(pq, wa[:, k, c0:c0 + 128], ha[:, k, :], k == 0, k == 7) for k in range(8)], R=[wa_b, hb], W=[pqb])
            c.mm([(ps, wa[:, k, 256 + c0:256 + c0 + 128], ha[:, k, :], k == 0, k == 7) for k in range(8)], R=[wa_b, hb], W=[psb])
            t1, t1b = tmpA.next()
            t2, t2b = tmpA.next()
            c.op("dve", lambda e: e.tensor_tensor(out=t1, in0=pq, in1=mk["ropeC"][:, sl], op=ALU.mult), R=[pqb, mk["b"]], W=[t1b])
            c.op("dve", lambda e: e.tensor_tensor(out=t2, in0=ps, in1=mk["ropeS"][:, sl], op=ALU.mult), R=[psb, mk["b"]], W=[t2b])
            if c0 == 0:
                c.op("pool", lambda e: e.tensor_tensor(out=dst[:, sl], in0=t1, in1=t2, op=ALU.add), R=[t1b, t2b], W=[qk_b[tb]])
            else:
                c.op("pool", lambda e: e.tensor_tensor(out=kT[0:64, 0, sl], in0=t1[0:64, :], in1=t2[0:64, :], op=ALU.add),
                     R=[t1b, t2b, kz_b], W=[qk_b[tb]])
                c.op("pool", lambda e: e.tensor_tensor(out=kT[64:128, 1, sl], in0=t1[64:128, :], in1=t2[64:128, :], op=ALU.add),
                     R=[t1b, t2b, kz_b], W=[qk_b[tb]])
        pv, pvb = psum.next()
        for tt in range(4):
            c.mm([(pv[:, tt * 128:(tt + 1) * 128], ha[:, k, tt * 128:(tt + 1) * 128], wa[:, k, 512:640], k == 0, k == 7)
                  for k in range(8)], R=[wa_b, hb], W=[pvb])
        va, vb = vnat.next()
        v4 = va.rearrange("p (tt a c) -> p tt a c", a=2, c=128)
        pv3 = pv.rearrange("p (tt c) -> p tt c", c=128)
        c.op("act", lambda e: e.activation(out=v4[:, :, 0, 0:64], in_=pv3[:, :, 0:64], func=AF.Copy), R=[pvb], W=[vb])
        c.op("act", lambda e: e.activation(out=v4[:, :, 1, 64:128], in_=pv3[:, :, 64:128], func=AF.Copy), R=[pvb], W=[vb])
        c.dma("sp", "vd", Vd[tb * 512:(tb + 1) * 512].rearrange("(tt p) a c -> p tt (a c)", p=128),
              va.rearrange("p (tt c) -> p tt c", c=256), R=[vb], W=[vd_b])

    vg = Rot(sb["vg"])
    ptr = Rot(sb["pt"])
    psS = Rot([a for a, _ in psum.slots[0:4]])
    psS.slots = psum.slots[0:4]
    psO = Rot([a for a, _ in psum.slots[4:8]])
    psO.slots = psum.slots[4:8]
    LAG = 3
    for gi, d in enumerate(DILS):
        L = S // d
        nb = L // 128
        vga, vgb = vg.next()
        c.dma_multi("sp", "vg", [(vga[:, r * nb:(r + 1) * nb, :],
                                  Vd.rearrange("(m r) a c -> r m (a c)", r=d)[r].rearrange("(kb p) c -> p kb c", p=128)) for r in range(d)],
                    R=[vd_b], W=[vgb])

        def tok(r, blk, n=1):
            st = r + d * 128 * blk
            return slice(st, st + d * (128 * n - 1) + 1, d)

        items = []
        for hh in range(2):
            for r in range(d):
                for b0 in range(0, nb, 4):
                    nbt = min(4, nb - b0)
                    for bi in range(nbt):
                        items.append((hh, r, b0, nbt, bi))
        state = {}
        pend = []

        def phaseA(it):
            hh, r, b0, nbt, bi = it
            P0 = hh * 64
            b = b0 + bi
            kbs = [kb for kb in (b - 1, b, b + 1) if 0 <= kb < nb]
            n = len(kbs) * 128
            ps, psb = psS.next()
            c.mm([(ps[:, ki * 128:(ki + 1) * 128], kT[:, hh, tok(r, kb)], qT[:, tok(r, b)], True, True)
                  for ki, kb in enumerate(kbs)], R=qk_b, W=[psb])
            pt, ptb = ptr.next()
            c.op("act", lambda e: e.activation(out=pt[:, 0:n], in_=ps[:, 0:n], func=AF.Exp, scale=0.125), R=[psb], W=[ptb])
            moff = 0 if kbs[0] == b - 1 else 128
            state["nmask"] = state.get("nmask", 0) + 1
            meng = "pool" if state["nmask"] % 3 == 0 else "dve"
            c.op(meng, lambda e: e.tensor_tensor(out=pt[:, 0:n], in0=pt[:, 0:n], in1=mk["mask3"][:, moff:moff + n], op=ALU.mult),
                 R=[ptb, mk["b"]], W=[ptb])
            return (pt, ptb, kbs)

        def phaseB(it, a):
            hh, r, b0, nbt, bi = it
            pt, ptb, kbs = a
            if bi == 0:
                state["pn"] = psO.next()
            pn, pnb = state["pn"]
            c.mm([(pn[:, bi * 128:(bi + 1) * 128], vga[:, r * nb + kb, hh * 128:(hh + 1) * 128], pt[:, ki * 128:(ki + 1) * 128],
                   ki == 0, ki == len(kbs) - 1) for ki, kb in enumerate(kbs)], R=[vgb, ptb], W=[pnb])
            if bi == nbt - 1:
                asl = tok(r, b0, nbt)
                w = nbt * 128
                if gi == 0:
                    c.op("dve", lambda e: e.tensor_copy(out=acc[hh][:, asl], in_=pn[:, 0:w]), R=[pnb], W=[acc_b[hh]])
                else:
                    c.op("dve", lambda e: e.tensor_tensor(out=acc[hh][:, asl], in0=pn[:, 0:w], in1=acc[hh][:, asl], op=ALU.add),
                         R=[pnb], W=[acc_b[hh]])

        for it in items:
            pend.append((it, phaseA(it)))
            if len(pend) > LAG:
                phaseB(*pend.pop(0))
        while pend:
            phaseB(*pend.pop(0))

    for q in range(S // 512):
        sl = slice(q * 512, (q + 1) * 512)
        pz, pzb = psum.next()
        c.mm([(pz, mk["selA"], acc[0][:, sl], True, False), (pz, mk["selB"], acc[1][:, sl], False, True)],
             R=[acc_b[0], acc_b[1], mk["b"]], W=[pzb])
        t2, t2b = tmpA.next()
        c.op("dve", lambda e: e.reciprocal(out=t2, in_=pz), R=[pzb], W=[t2b])
        c.op("dve", lambda e: e.tensor_tensor(out=mergedT[0:64, j, sl], in0=acc[0][0:64, sl], in1=t2[0:64, :], op=ALU.mult),
             R=[acc_b[0], t2b], W=[merged_b])
        c.op("pool", lambda e: e.tensor_tensor(out=mergedT[64:128, j, sl], in0=acc[1][64:128, sl], in1=t2[64:128, :], op=ALU.mult),
             R=[acc_b[1], t2b], W=[merged_b])


def alloc_attn_sb(nc, es):
    sb = {}

    _TAG[0] += 1

    def t(name, shape, dt):
        return _t(nc, es, name, shape, dt)
    sb["wa"] = t("wa", [128, 8, 640], BF16)
    sb["qT"] = t("qT", [128, S], BF16)
    sb["kT"] = t("kT", [128, 2, S], BF16)
    sb["accN"] = t("accN", [128, S], F32)
    sb["accZ"] = t("accZ", [128, S], F32)
    sb["hblk"] = [t(f"hblk{i}", [128, 8, 512], BF16) for i in range(2)]
    sb["tmpA"] = [t(f"tmpA{i}", [128, 512], F32) for i in range(4)]
    sb["vnat"] = [t(f"vnat{i}", [128, 1024], BF16) for i in range(2)]
    sb["vg"] = [t(f"vg{i}", [128, 32, 256], BF16) for i in range(1)]
    sb["pt"] = [t(f"pt{i}", [128, 384], BF16) for i in range(6)]
    return sb


def alloc_ml_sb(nc, es):
    sb = {}

    _TAG[0] += 1

    def t(name, shape, dt):
        return _t(nc, es, name, shape, dt)
    sb["wb"] = t("wb", [128, 8, 516], BF16)
    sb["hblk"] = [t(f"mhblk{i}", [128, 8, 512], BF16) for i in range(2)]
    sb["raw"] = t("raw", [128, 2, S + 4], BF16)
    sb["qkT"] = t("qkT", [128, 2, S], BF16)
    sb["sigo"] = t("sigo", [128, S], BF16)
    sb["vaug"] = t("vaug", [128, S // 128, 129], BF16)
    sb["ktok"] = t("ktok", [128, S // 128, 128], BF16)
    sb["hsum"] = t("hsum", [128, S // 128, 128], F32)
    sb["hb"] = t("hb", [128, S // 128, 128], F32)
    sb["yn"] = t("yn", [128, S // 128, 128], BF16)
    sb["cw"] = t("cw", [128, 2, 5], F32)
    sb["cb"] = t("cb", [128, 2], F32)
    sb["gb"] = t("gb", [128, 4], F32)
    sb["mg"] = t("mg", [128, 1], F32)
    sb["dg"] = t("dg", [128, 2, 5, 128], BF16)
    NCH = S // 128
    for nm in ("G", "Gb"):
        sb[nm] = t(nm, [128, NCH, 4], F32)
    for nm in ("E1", "LF", "CUM", "TOT", "I_", "B_", "EB", "EC", "ET", "W_", "TB", "DEN", "RR"):
        sb[nm] = t(nm, [128, 2, NCH], F32)
    sb["ssq"] = t("ssq", [128, NCH], F32)
    sb["rstd"] = t("mrstd", [128, NCH], F32)
    sb["CT"] = [t(f"CT{i}", [128, 129], F32) for i in range(2)]
    sb["CTb"] = [t(f"CTb{i}", [128, 129], BF16) for i in range(2)]
    sb["A"] = [t(f"A{i}", [128, 128], BF16) for i in range(6)]
    sb["kpp"] = [t(f"kpp{i}", [128, 128], BF16) for i in range(6)]
    sb["ep"] = [t(f"ep{i}", [128, 4], F32) for i in range(4)]
    sb["mjunk"] = t("mjunk", [128, 128], BF16)
    return sb


def mlstm_pass(c, hd, hv, wB, convw, convb, gbias, mgain, mk, kc, psum, sb, mergedT, merged_b, hdep=None):
    nc = c.nc
    NTB = S // 512
    NCH = S // 128
    wb, raw, qkT, sigo, vaug, ktok, hsum = sb["wb"], sb["raw"], sb["qkT"], sb["sigo"], sb["vaug"], sb["ktok"], sb["hsum"]
    wb_b, par_b, dg_b = Buf(), Buf(), Buf()
    raw_b = [Buf() for _ in range(NTB)]
    rawpad_b = Buf()
    qk_b = [Buf() for _ in range(NTB)]
    sig_b, va_b, g_b, kt_b, hs_b = Buf(), Buf(), Buf(), Buf(), Buf()
    c.dma("pool", "wb", wb, wB[hd].rearrange("(k p) f -> p k f", p=128), W=[wb_b])
    c.dma("sp", "cw", sb["cw"], convw[hd], W=[par_b])
    c.dma("sp", "cb", sb["cb"], convb[hd], W=[par_b])
    c.dma("sp", "gb", sb["gb"], gbias[hd].partition_broadcast(128), W=[par_b])
    c.dma("sp", "mg", sb["mg"], mgain[hd], W=[par_b])
    for qk in range(2):
        for jj in range(5):
            c.op("dve", lambda e: e.tensor_scalar(out=sb["dg"][:, qk, jj, :], in0=kc["identf"], scalar1=sb["cw"][:, qk, jj:jj + 1],
                                                  scalar2=None, op0=ALU.mult), R=[par_b], W=[dg_b])
    c.op("pool", lambda e: e.memset(raw[:, :, 0:2], 0.0), W=[rawpad_b])
    c.op("pool", lambda e: e.memset(raw[:, :, S + 2:S + 4], 0.0), W=[rawpad_b])
    c.op("pool", lambda e: e.memset(vaug[:, :, 128:129], 1.0), W=[va_b])

    hblk = Rot(sb["hblk"])
    loaded = {}

    def load_h(tb):
        ap, b = hblk.next()
        c.dma_multi("sp", ("mhblk", hblk.i % 2), [(ap[:, 4 * i2:4 * i2 + 4, :], hv(tb // 4, i2)[:, :, (tb % 4) * 512:(tb % 4 + 1) * 512])
                                                    for i2 in range(2)], R=[hdep] if hdep else [], W=[b])
        loaded[tb] = (ap, b)

    load_h(0)
    for tb in range(NTB):
        if tb + 1 < NTB:
            load_h(tb + 1)
        ha, hb = loaded.pop(tb)
        sl = slice(tb * 512, (tb + 1) * 512)
        for fi in range(3):
            pq, pqb = psum.next()
            c.mm([(pq, wb[:, k, fi * 128:(fi + 1) * 128], ha[:, k, :], k == 0, k == 7) for k in range(8)], R=[wb_b, hb], W=[pqb])
            if fi < 2:
                c.op("act", lambda e: e.activation(out=raw[:, fi, 2 + tb * 512:2 + (tb + 1) * 512], in_=pq, func=AF.Copy), R=[pqb], W=[raw_b[tb]])
            else:
                c.op("act", lambda e: e.activation(out=sigo[:, sl], in_=pq, func=AF.Sigmoid), R=[pqb], W=[sig_b])
        for half in range(2):
            pv, pvb = psum.next()
            for t2 in range(2):
                tt = half * 2 + t2
                c.mm([(pv[:, t2 * 132:(t2 + 1) * 132], ha[:, k, tt * 128:(tt + 1) * 128], wb[:, k, 384:516], k == 0, k == 7)
                      for k in range(8)], R=[wb_b, hb], W=[pvb])
            c0 = tb * 4 + half * 2
            pv3 = pv[:, 0:264].rearrange("p (t f) -> p t f", f=132)
            c.op("dve", lambda e: e.tensor_copy(out=vaug[:, c0:c0 + 2, 0:128], in_=pv3[:, :, 0:128]), R=[pvb], W=[va_b])
            c.op("dve", lambda e: e.tensor_copy(out=sb["G"][:, c0:c0 + 2, :], in_=pv3[:, :, 128:132]), R=[pvb], W=[g_b])

    for qk in range(2):
        for tb in range(NTB):
            pq, pqb = psum.next()
            rb = [raw_b[t] for t in (tb - 1, tb, tb + 1) if 0 <= t < NTB] + [rawpad_b, dg_b]
            c.mm([(pq, sb["dg"][:, qk, jj, :], raw[:, qk, tb * 512 + jj:tb * 512 + jj + 512], jj == 0, jj == 4) for jj in range(5)],
                 R=rb, W=[pqb])
            c.op("act", lambda e: e.activation(out=qkT[:, qk, tb * 512:(tb + 1) * 512], in_=pq, func=AF.Silu, bias=sb["cb"][:, qk:qk + 1]),
                 R=[pqb, par_b], W=[qk_b[tb]])
    for tb in range(NTB):
        pa, pb = psum.next()
        pT = pa.bitcast(BF16)[:, 0:512].rearrange("p (k t) -> p k t", k=4)
        c.wait("pe", c._deps([qk_b[tb]], [pb]))
        ins = None
        for k in range(4):
            ins = nc.tensor.transpose(pT[:, k, :], qkT[:, 1, tb * 512 + k * 128:tb * 512 + (k + 1) * 128], kc["ident"])
        tok = c._sig("pe", ins)
        c._mark(tok, [qk_b[tb]], [pb])
        c.op("dve", lambda e: e.tensor_copy(out=ktok[:, tb * 4:(tb + 1) * 4, :], in_=pT), R=[pb], W=[kt_b])

    G, Gb = sb["G"], sb["Gb"]
    gm = Buf()
    c.op("dve", lambda e: e.tensor_tensor(out=Gb, in0=G, in1=sb["gb"].unsqueeze(1).to_broadcast([128, NCH, 4]), op=ALU.add), R=[g_b, par_b], W=[gm])
    fview = Gb[:, :, 1::2].rearrange("p c g -> p g c")
    iview = Gb[:, :, 0::2].rearrange("p c g -> p g c")
    c.op("act", lambda e: e.activation(out=sb["E1"], in_=fview, func=AF.Exp, scale=-1.0), R=[gm], W=[gm])
    c.op("act", lambda e: e.activation(out=sb["E1"], in_=sb["E1"], func=AF.Ln, bias=kc["one"]), R=[gm], W=[gm])
    c.op("dve", lambda e: e.tensor_scalar(out=sb["LF"], in0=sb["E1"], scalar1=-1.0, scalar2=None, op0=ALU.mult), R=[gm], W=[gm])
    pg, pgb = psum.next()
    c.mm([(pg[:, 0:NCH], mk["trif"], sb["LF"][:, 0, :], True, True),
          (pg[:, NCH:2 * NCH], mk["trib"], sb["LF"][:, 1, :], True, True),
          (pg[:, 2 * NCH:4 * NCH], mk["ones128"], sb["LF"].rearrange("p a c -> p (a c)"), True, True)], R=[gm, mk["b"]], W=[pgb])
    c.op("dve", lambda e: e.tensor_copy(out=sb["CUM"].rearrange("p a c -> p (a c)"), in_=pg[:, 0:2 * NCH]), R=[pgb], W=[gm])
    c.op("dve", lambda e: e.tensor_copy(out=sb["TOT"].rearrange("p a c -> p (a c)"), in_=pg[:, 2 * NCH:4 * NCH]), R=[pgb], W=[gm])
    c.op("dve", lambda e: e.tensor_tensor(out=sb["B_"], in0=iview, in1=sb["CUM"], op=ALU.subtract), R=[gm], W=[gm])
    c.op("act", lambda e: e.activation(out=sb["EB"], in_=sb["B_"], func=AF.Exp, bias=kc["lnk"]), R=[gm], W=[gm])
    c.op("act", lambda e: e.activation(out=sb["EC"], in_=sb["CUM"], func=AF.Exp), R=[gm], W=[gm])
    c.op("act", lambda e: e.activation(out=sb["ET"], in_=sb["TOT"], func=AF.Exp), R=[gm], W=[gm])
    c.op("dve", lambda e: e.tensor_tensor(out=sb["TB"], in0=sb["TOT"], in1=sb["B_"], op=ALU.add), R=[gm], W=[gm])
    c.op("act", lambda e: e.activation(out=sb["W_"], in_=sb["TB"], func=AF.Exp, bias=kc["lnk"]), R=[gm], W=[gm])

    Arot, Krot = Rot(sb["A"]), Rot(sb["kpp"])
    den_b = Buf()
    ct_b = [Buf(), Buf()]
    ctb_b = [Buf(), Buf()]
    qall = qk_b
    def pre(i, dr):
        ch = i if dr == 0 else NCH - 1 - i
        cs = slice(ch * 128, (ch + 1) * 128)
        tri = mk["trif"] if dr == 0 else mk["trib"]
        ps, psb = psum.next()
        c.mm([(ps[:, 0:128], qkT[:, 1, cs], qkT[:, 0, cs], True, True)], R=qall, W=[psb])
        aa, ab = Arot.next()
        c.op("dve", lambda e: e.scalar_tensor_tensor(out=aa, in0=ps[:, 0:128], scalar=sb["EB"][:, dr, ch:ch + 1], in1=tri,
                                                     op0=ALU.mult, op1=ALU.mult), R=[psb, gm, mk["b"]], W=[ab])
        ka, kb_ = Krot.next()
        if i < NCH - 1:
            c.op("act", lambda e: e.activation(out=ka, in_=ktok[:, ch, :], func=AF.Copy, scale=sb["W_"][:, dr, ch:ch + 1]),
                 R=[kt_b, gm], W=[kb_])
        return (aa, ab, ka, kb_)

    pres = {(0, 0): pre(0, 0), (0, 1): pre(0, 1)}
    for i in range(NCH):
        for dr, ch in ((0, i), (1, NCH - 1 - i)):
            if i + 1 < NCH:
                pres[(i + 1, dr)] = pre(i + 1, dr)
            aa, ab, ka, kb_ = pres.pop((i, dr))
            cs = slice(ch * 128, (ch + 1) * 128)
            pn, pnb = psum.next()
            mms = [(pn[:, 0:129], aa, vaug[:, ch, :], True, i == 0)]
            if i > 0:
                mms.append((pn[:, 0:129], qkT[:, 0, cs], sb["CTb"][dr], False, True))
            c.mm(mms, R=[ab, va_b, ctb_b[dr]] + qall, W=[pnb])
            if i < NCH - 1:
                pc, pcb = psum.next()
                c.mm([(pc[:, 0:129], ka, vaug[:, ch, :], True, True)], R=[kb_, va_b], W=[pcb])
                if i == 0:
                    c.op("dve", lambda e: e.tensor_copy(out=sb["CT"][dr], in_=pc[:, 0:129]), R=[pcb], W=[ct_b[dr]])
                else:
                    c.op("dve", lambda e: e.scalar_tensor_tensor(out=sb["CT"][dr], in0=sb["CT"][dr], scalar=sb["ET"][:, dr, ch:ch + 1], in1=pc[:, 0:129],
                                                                 op0=ALU.mult, op1=ALU.add), R=[pcb, gm], W=[ct_b[dr]])
                c.op("act", lambda e: e.activation(out=sb["CTb"][dr], in_=sb["CT"][dr], func=AF.Copy), R=[ct_b[dr]], W=[ctb_b[dr]])
            hdst = hsum if dr == 0 else sb["hb"]
            c.op("act", lambda e: e.activation(out=sb["DEN"][:, dr, ch:ch + 1], in_=pn[:, 128:129], func=AF.Abs, scale=sb["EC"][:, dr, ch:ch + 1]),
                 R=[pnb, gm], W=[den_b])
            c.op("act", lambda e: e.activation(out=hdst[:, ch, :], in_=pn[:, 0:128], func=AF.Copy), R=[pnb], W=[hs_b])

    DEN, RR = sb["DEN"], sb["RR"]
    c.op("dve", lambda e: e.tensor_scalar_max(out=RR, in0=DEN, scalar1=1.0), R=[den_b], W=[den_b])
    c.op("dve", lambda e: e.reciprocal(out=RR, in_=RR), R=[den_b], W=[den_b])
    c.op("dve", lambda e: e.tensor_tensor(out=RR, in0=RR, in1=sb["EC"], op=ALU.mult), R=[den_b, gm], W=[den_b])
    c.op("dve", lambda e: e.tensor_tensor(out=hsum, in0=hsum, in1=RR[:, 0, :].unsqueeze(2).to_broadcast([128, NCH, 128]), op=ALU.mult),
         R=[den_b, hs_b], W=[hs_b])
    c.op("pool", lambda e: e.tensor_tensor(out=sb["hb"], in0=sb["hb"], in1=RR[:, 1, :].unsqueeze(2).to_broadcast([128, NCH, 128]), op=ALU.mult),
         R=[den_b, hs_b], W=[hs_b])
    c.op("dve", lambda e: e.tensor_tensor(out=hsum, in0=hsum, in1=sb["hb"], op=ALU.add), R=[hs_b], W=[hs_b])

    nb_ = Buf()
    for ch in range(NCH):
        c.op("act", lambda e: e.activation(out=sb["mjunk"], in_=hsum[:, ch, :], func=AF.Square, accum_out=sb["ssq"][:, ch:ch + 1]), R=[hs_b], W=[nb_])
    c.op("act", lambda e: e.activation(out=sb["rstd"], in_=sb["ssq"], func=AF.Sqrt, scale=1.0 / 128, bias=kc["eps"]), R=[nb_], W=[nb_])
    c.op("dve", lambda e: e.reciprocal(out=sb["rstd"], in_=sb["rstd"]), R=[nb_], W=[nb_])
    yn = sb["yn"]
    yn_b = Buf()
    c.op("dve", lambda e: e.tensor_tensor(out=yn, in0=hsum, in1=sb["rstd"].unsqueeze(2).to_broadcast([128, NCH, 128]), op=ALU.mult),
         R=[hs_b, nb_], W=[yn_b])
    for tb in range(NTB):
        pa, pb = psum.next()
        pT = pa.bitcast(BF16)[:, 0:512]
        c.wait("pe", c._deps([yn_b], [pb]))
        ins = None
        for k in range(4):
            ins = nc.tensor.transpose(pT[:, k * 128:(k + 1) * 128], yn[:, tb * 4 + k, :], kc["ident"])
        tok = c._sig("pe", ins)
        c._mark(tok, [yn_b], [pb])
        c.op("dve", lambda e: e.scalar_tensor_tensor(out=mergedT[:, 2 + hd, tb * 512:(tb + 1) * 512], in0=pT, scalar=sb["mg"][:, 0:1],
                                                     in1=sigo[:, tb * 512:(tb + 1) * 512], op0=ALU.mult, op1=ALU.mult),
             R=[pb, par_b, sig_b], W=[merged_b])


def build_ffn_prog():
    nc = bass.Bass("TRN2", target_bir_lowering=False)
    x_in = nc.dram_tensor("x", [T, D], F32, kind="ExternalInput").ap()
    w_in = nc.dram_tensor("w_in", [D, 2 * DFF], F32, kind="ExternalInput").ap()
    w_out = nc.dram_tensor("w_out", [DFF, D], F32, kind="ExternalInput").ap()
    g = nc.dram_tensor("g", [2, D], F32, kind="ExternalInput").ap()
    x_out = nc.dram_tensor("y", [T, D], F32, kind="ExternalOutput").ap()
    with ExitStack() as es:
        c = Ctx(nc, es)
        k = setup_consts(c, es)
        sb = alloc_ffn_sb(nc, es)
        psum = Rot([es.enter_context(nc.psum_tensor(f"ps{i}", [128, 512], F32))[:] for i in range(8)], excl=True)
        toks = ffn_stage(c, x_in, x_out, w_in, w_out, g[0], g[1], k, psum, sb)
        c.wait("sp", toks)
    return nc


def build_attn_test(j=0):
    nc = bass.Bass("TRN2", target_bir_lowering=False)
    hT_full = nc.dram_tensor("hT", [2, D, T], BF16, kind="ExternalInput").ap()
    wA = nc.dram_tensor("wA", [2, D, 640], F32, kind="ExternalInput").ap()
    out = nc.dram_tensor("mT", [128, S], BF16, kind="ExternalOutput").ap()
    Vd = nc.dram_tensor("Vd", [S, 2, 128], BF16).ap()
    with ExitStack() as es:
        c = Ctx(nc, es)
        mk = setup_mixer_consts(c, es)
        sb = alloc_attn_sb(nc, es)
        mergedT = es.enter_context(nc.sbuf_tensor("mergedT", [128, 4, S], BF16))[:]
        mb = Buf()
        psum = Rot([es.enter_context(nc.psum_tensor(f"ps{i}", [128, 512], F32))[:] for i in range(8)], excl=True)
        hv = lambda r, i: hT_full[r, i * 512:(i + 1) * 512].rearrange("(k p) t -> p k t", p=128)
        attn_pass(c, j, hv, wA, Vd, mk, psum, sb, mergedT, mb)
        t = c.dma("sp", "o", out, mergedT[:, j, :], R=[mb])
        c.wait("sp", [t])
    return nc


def layout_wA(w_in_l, hf):
    sw = np.concatenate([np.arange(8, 16), np.arange(0, 8), np.arange(16, 64)])
    out = np.empty((2, D, 640), np.float32)
    for j in range(2):
        cq, ck, cv, cqs, cks = [], [], [], [], []
        for hh in range(2):
            H = hf * 4 + 2 * j + hh
            base = np.arange(64) + H * 64
            cq.append(base); ck.append(512 + base); cv.append(1024 + base)
            cqs.append(base[sw]); cks.append(512 + base[sw])
        cols = np.concatenate(cq + ck + cqs + cks + cv)
        out[j] = w_in_l[:, cols]
    return out


def layout_wB(w_in_l, hf):
    out = np.empty((2, D, 516), np.float32)
    for hd in range(2):
        H = hf * 2 + hd
        base = np.arange(128) + H * 128
        cols = np.concatenate([1536 + base, 2048 + base, 3072 + base, 2560 + base, 3584 + np.arange(4) * 4 + H])
        out[hd] = w_in_l[:, cols]
    return out


def layout_ml_params(conv_w_l, conv_b_l, gate_bias_l, mgain_l, hf):
    cw = np.empty((2, 128, 2, 5), np.float32)
    cb = np.empty((2, 128, 2), np.float32)
    gb = np.empty((2, 4), np.float32)
    mg = np.empty((2, 128, 1), np.float32)
    for hd in range(2):
        H = hf * 2 + hd
        for qk in range(2):
            ch = qk * 512 + H * 128 + np.arange(128)
            cw[hd, :, qk, :] = conv_w_l[:, ch].T
            cb[hd, :, qk] = conv_b_l[ch]
        gb[hd] = gate_bias_l[:, H]
        mg[hd, :, 0] = mgain_l[H * 128:(H + 1) * 128]
    return cw, cb, gb, mg


def build_ml_test(hd=0):
    nc = bass.Bass("TRN2", target_bir_lowering=False)
    hT_full = nc.dram_tensor("hT", [2, D, T], BF16, kind="ExternalInput").ap()
    wB = nc.dram_tensor("wB", [2, D, 516], F32, kind="ExternalInput").ap()
    cw = nc.dram_tensor("cw", [2, 128, 2, 5], F32, kind="ExternalInput").ap()
    cb = nc.dram_tensor("cb", [2, 128, 2], F32, kind="ExternalInput").ap()
    gb = nc.dram_tensor("gb", [2, 4], F32, kind="ExternalInput").ap()
    mg = nc.dram_tensor("mg", [2, 128, 1], F32, kind="ExternalInput").ap()
    out = nc.dram_tensor("mT", [128, S], BF16, kind="ExternalOutput").ap()
    with ExitStack() as es:
        c = Ctx(nc, es)
        kc = setup_consts(c, es)
        mk = setup_mixer_consts(c, es)
        sb = alloc_ml_sb(nc, es)
        mergedT = es.enter_context(nc.sbuf_tensor("mergedT", [128, 4, S], BF16))[:]
        mb = Buf()
        psum = Rot([es.enter_context(nc.psum_tensor(f"ps{i}", [128, 512], F32))[:] for i in range(8)], excl=True)
        hv = lambda r, i: hT_full[r, i * 512:(i + 1) * 512].rearrange("(k p) t -> p k t", p=128)
        mlstm_pass(c, hd, hv, wB, cw, cb, gb, mg, mk, kc, psum, sb, mergedT, mb)
        t = c.dma("sp", "o", out, mergedT[:, 2 + hd, :], R=[mb])
        c.wait("sp", [t])
    return nc


def _psum(nc, es):
    return Rot([es.enter_context(nc.psum_tensor(f"ps{i}", [128, 512], F32))[:] for i in range(8)], excl=True)


def build_A(first, last):
    nc = bass.Bass("TRN2", target_bir_lowering=False)
    x = nc.dram_tensor("x", [T, D], F32, kind="ExternalInput").ap()
    if not first:
        mT = nc.dram_tensor("mT", [8, 128, T], BF16, kind="ExternalInput").ap()
        w_mo = nc.dram_tensor("w_mo", [D, D], F32, kind="ExternalInput").ap()
        w_in2 = nc.dram_tensor("w_in2", [D, 2 * DFF], F32, kind="ExternalInput").ap()
        w_out2 = nc.dram_tensor("w_out2", [DFF, D], F32, kind="ExternalInput").ap()
        g345 = nc.dram_tensor("g345", [3, D], F32, kind="ExternalInput").ap()
    if not last:
        w_in1 = nc.dram_tensor("w_in1", [D, 2 * DFF], F32, kind="ExternalInput").ap()
        w_out1 = nc.dram_tensor("w_out1", [DFF, D], F32, kind="ExternalInput").ap()
        g012 = nc.dram_tensor("g012", [3, D], F32, kind="ExternalInput").ap()
        hTo = nc.dram_tensor("hTo", [D, T], BF16, kind="ExternalOutput").ap()
    xo = nc.dram_tensor("xo", [T, D], F32, kind="ExternalOutput").ap()
    xa = nc.dram_tensor("xa", [T, D], F32).ap()
    xb = nc.dram_tensor("xb", [T, D], F32).ap()
    with ExitStack() as es:
        c = Ctx(nc, es)
        kc = setup_consts(c, es)
        sb = alloc_ffn_sb(nc, es)
        psum = _psum(nc, es)
        cur = x
        if not first:
            mixout_stage(c, cur, xa, mT, w_mo, g345[0], kc, psum, sb)
            c.barrier()
            ffn_stage(c, xa, xo if last else xb, w_in2, w_out2, g345[1], g345[2], kc, psum, sb)
            c.barrier()
            cur = xb
        if not last:
            ffn_stage(c, cur, xo, w_in1, w_out1, g012[0], g012[1], kc, psum, sb)
            c.barrier()
            hT_stage(c, xo, g012[2], hTo, kc, psum, sb)
        c.barrier()
    return nc


def build_B():
    nc = bass.Bass("TRN2", target_bir_lowering=False)
    hT_full = nc.dram_tensor("hT", [2, D, T], BF16, kind="ExternalInput").ap()
    wA = nc.dram_tensor("wA", [2, D, 640], F32, kind="ExternalInput").ap()
    wB = nc.dram_tensor("wB", [2, D, 516], F32, kind="ExternalInput").ap()
    cw = nc.dram_tensor("cw", [2, 128, 2, 5], F32, kind="ExternalInput").ap()
    cb = nc.dram_tensor("cb", [2, 128, 2], F32, kind="ExternalInput").ap()
    gb = nc.dram_tensor("gb", [2, 4], F32, kind="ExternalInput").ap()
    mg = nc.dram_tensor("mg", [2, 128, 1], F32, kind="ExternalInput").ap()
    out = nc.dram_tensor("mT", [4, 128, S], BF16, kind="ExternalOutput").ap()
    Vd = nc.dram_tensor("Vd", [S, 2, 128], BF16).ap()
    with ExitStack() as es:
        c = Ctx(nc, es)
        kc = setup_consts(c, es)
        mk = setup_mixer_consts(c, es)
        mergedT = es.enter_context(nc.sbuf_tensor("mergedT", [128, 4, S], BF16))[:]
        mb = Buf()
        psum = _psum(nc, es)
        hv = lambda r, i: hT_full[r, i * 512:(i + 1) * 512].rearrange("(k p) t -> p k t", p=128)
        mixer_core(c, nc, hv, wA, wB, cw, cb, gb, mg, Vd, mk, kc, psum, mergedT, mb)
        c.dma("sp", "mTo", out.rearrange("j p s -> p j s"), mergedT, R=[mb])
        c.barrier()
    return nc


def mixer_core(c, nc, hv, wA, wB, cw, cb, gb, mg, Vd, mk, kc, psum, mergedT, mb, hdep=None, after_tile=None):
    excl = ("cc", "mTo")
    with ExitStack() as es2:
        sba = alloc_attn_sb(nc, es2)
        for j in range(2):
            attn_pass(c, j, hv, wA, Vd, mk, psum, sba, mergedT, mb, hdep)
            if after_tile:
                after_tile(j)
            c.barrier(exclude=excl)
    with ExitStack() as es2:
        sbm = alloc_ml_sb(nc, es2)
        for hd in range(2):
            mlstm_pass(c, hd, hv, wB, cw, cb, gb, mg, mk, kc, psum, sbm, mergedT, mb, hdep)
            if after_tile:
                after_tile(2 + hd)
            c.barrier(exclude=excl)


def layout_wmo(w_out_l):
    rows = []
    for r in range(2):
        for j in range(2):
            for hh in range(2):
                H = r * 4 + 2 * j + hh
                rows.append(np.arange(64) + H * 64)
        for hd in range(2):
            H = r * 2 + hd
            rows.append(512 + np.arange(128) + H * 128)
    return np.ascontiguousarray(w_out_l[np.concatenate(rows)])


_PROGS = {}


def _prog(key, fn):
    if key not in _PROGS:
        _PROGS[key] = fn()
    return _PROGS[key]


DEPTH = 4
NCORES = 8
PAIRS = [[0, 1], [2, 3], [4, 5], [6, 7]]


def build_fused(depth=DEPTH):
    nc = bass.Bass("TRN2", target_bir_lowering=False)
    dt_in = lambda name, shape, dt=F32: nc.dram_tensor(name, shape, dt, kind="ExternalInput").ap()
    x = dt_in("x", [T, D])
    sel = dt_in("sel", [128, 2])
    ng = dt_in("ng", [depth, 6, D])
    fwi = dt_in("fwi", [depth, 2, D, 2 * DFF])
    fwo = dt_in("fwo", [depth, 2, DFF, D])
    wmo = dt_in("wmo", [depth, D, D])
    wA = dt_in("wA", [depth, 2, D, 640])
    wB = dt_in("wB", [depth, 2, D, 516])
    cw = dt_in("cw", [depth, 2, 128, 2, 5])
    cb = dt_in("cb", [depth, 2, 128, 2])
    gb = dt_in("gb", [depth, 2, 4])
    mg = dt_in("mg", [depth, 2, 128, 1])
    xo = nc.dram_tensor("xo", [T, D], F32, kind="ExternalOutput").ap()
    xs_ = [nc.dram_tensor(f"xs{i}", [T, D], F32).ap() for i in range(3)]
    hT_my = nc.dram_tensor("hT_my", [D, T], BF16).ap()
    hT_full = nc.dram_tensor("hT_full", [2, 2, 512, T], BF16).ap()
    mT_my = nc.dram_tensor("mT_my", [4, 128, S], BF16).ap()
    mT_all = nc.dram_tensor("mT_all", [4, 2, 128, S], BF16).ap()
    Vd = nc.dram_tensor("Vd", [S, 2, 128], BF16).ap()
    rope_d = [nc.dram_tensor(f"rope{i}", [128, S], F32).ap() for i in range(2)]
    with ExitStack() as es:
        c = Ctx(nc, es)
        kc = setup_consts(c, es)
        mk = setup_mixer_consts(c, es, rope_dram=rope_d)
        psum = _psum(nc, es)
        hv = lambda r, i: hT_full[i, r].rearrange("(k p) t -> p k t", p=128)
        hT_buf, mTm_buf, mTa_buf = Buf(), Buf(), Buf()
        cur = x
        for l in range(depth):
            with ExitStack() as es2:
                sb = alloc_ffn_sb(nc, es2)
                ffn_stage(c, cur, xs_[0], fwi[l, 0], fwo[l, 0], ng[l, 0], ng[l, 1], kc, psum, sb)
                c.barrier()
                hT_b = prenorm_hT(c, xs_[0], ng[l, 2], kc, psum, sb)
                hbufs = [(Buf(), Buf()) for _ in range(2)]
                for i2 in range(2):
                    c.dma("sp", ("hTo", i2), hT_my[i2 * 512:(i2 + 1) * 512].rearrange("(k p) t -> p k t", p=128), sb["hT"][:, 4 * i2:4 * i2 + 4, :],
                          R=hT_b, W=[hbufs[i2][0]])
                    c.collective("AllGather", hT_my[i2 * 512:(i2 + 1) * 512], hT_full[i2].rearrange("r d t -> (r d) t"), PAIRS,
                                 R=[hbufs[i2][0]], W=[hbufs[i2][1]])
                c.barrier()
            with ExitStack() as es2:
                _TAG[0] += 1
                mk["ropeC"] = _t(nc, es2, "ropeC", [128, S], F32)
                mk["ropeS"] = _t(nc, es2, "ropeS", [128, S], F32)
                mergedT = _t(nc, es2, "mergedT", [128, 4, S], BF16)
                c.dma("sp", "ropeC", mk["ropeC"], rope_d[0], W=[mk["b"]])
                c.dma("sp", "ropeS", mk["ropeS"], rope_d[1], W=[mk["b"]])
                mb = Buf()
                tile_bufs = [(Buf(), Buf()) for _ in range(4)]

                def ship(j2, mergedT=mergedT, mb=mb, tile_bufs=tile_bufs):
                    b_in, b_out = tile_bufs[j2]
                    c.dma("sp", ("mTo", j2), mT_my[j2], mergedT[:, j2, :], R=[mb], W=[b_in])
                    c.collective("AllGather", mT_my[j2], mT_all[j2].rearrange("r p s -> (r p) s"), PAIRS, R=[b_in], W=[b_out])

                mixer_core(c, nc, hv, wA[l], wB[l], cw[l], cb[l], gb[l], mg[l], Vd, mk, kc, psum, mergedT, mb)
                c.barrier()
                for j2 in range(4):
                    ship(j2)
                c.barrier()
            with ExitStack() as es2:
                sb = alloc_ffn_sb(nc, es2)
                mixout_stage(c, xs_[0], xs_[1], [mT_all[ch % 4, ch // 4] for ch in range(8)], wmo[l], ng[l, 3], kc, psum, sb, sel=sel)
                c.barrier()
                nxt = xo if l == depth - 1 else xs_[2]
                ffn_stage(c, xs_[1], nxt, fwi[l, 1], fwo[l, 1], ng[l, 4], ng[l, 5], kc, psum, sb)
                c.barrier()
                cur = nxt
    return nc


def kernel(x, norm_gain, ffn_w_in, ffn_w_out, mix_w_in, conv_w, conv_b, gate_bias, mlstm_norm_gain, mix_w_out):
    x = np.asarray(x, np.float32)
    f32 = lambda a: np.ascontiguousarray(np.asarray(a, np.float32))
    norm_gain, ffn_w_in, ffn_w_out, mix_w_in = f32(norm_gain), f32(ffn_w_in), f32(ffn_w_out), f32(mix_w_in)
    conv_w, conv_b, gate_bias, mlstm_norm_gain, mix_w_out = f32(conv_w), f32(conv_b), f32(gate_bias), f32(mlstm_norm_gain), f32(mix_w_out)
    cores = list(range(NCORES))
    wmo = np.stack([layout_wmo(mix_w_out[l]) for l in range(DEPTH)])
    per_hf = []
    for hf in range(2):
        mlp = [layout_ml_params(conv_w[l], conv_b[l], gate_bias[l], mlstm_norm_gain[l], hf) for l in range(DEPTH)]
        selv = np.zeros((128, 2), np.float32)
        selv[:, hf] = 1.0
        per_hf.append({"wA": np.stack([layout_wA(mix_w_in[l], hf) for l in range(DEPTH)]),
                       "wB": np.stack([layout_wB(mix_w_in[l], hf) for l in range(DEPTH)]),
                       "cw": np.stack([m[0] for m in mlp]), "cb": np.stack([m[1] for m in mlp]),
                       "gb": np.stack([m[2] for m in mlp]), "mg": np.stack([m[3] for m in mlp]), "sel": selv})
    in_maps = []
    for c in cores:
        m = {"x": np.ascontiguousarray(x[c // 2, (c % 2) * T:(c % 2 + 1) * T]), "ng": norm_gain, "fwi": ffn_w_in, "fwo": ffn_w_out, "wmo": wmo}
        m.update(per_hf[c % 2])
        in_maps.append(m)
    nc = _prog("fused", build_fused)
    res = run_bass_kernel_spmd(nc, in_maps, core_ids=cores).results
    out = np.empty((4, S, D), np.float32)
    for c in cores:
        out[c // 2, (c % 2) * T:(c % 2 + 1) * T] = res[c]["xo"]
    return out


def kernel_unfused(x, norm_gain, ffn_w_in, ffn_w_out, mix_w_in, conv_w, conv_b, gate_bias, mlstm_norm_gain, mix_w_out):
    x = np.asarray(x, np.float32)
    f32 = lambda a: np.ascontiguousarray(np.asarray(a, np.float32))
    norm_gain, ffn_w_in, ffn_w_out, mix_w_in = f32(norm_gain), f32(ffn_w_in), f32(ffn_w_out), f32(mix_w_in)
    conv_w, conv_b, gate_bias, mlstm_norm_gain, mix_w_out = f32(conv_w), f32(conv_b), f32(gate_bias), f32(mlstm_norm_gain), f32(mix_w_out)
    cores = list(range(NCORES))
    xc = [np.ascontiguousarray(x[c // 2, (c % 2) * T:(c % 2 + 1) * T]) for c in cores]
    mT_my = None
    for l in range(DEPTH + 1):
        first, last = l == 0, l == DEPTH
        in_maps = []
        for c in cores:
            m = {"x": xc[c]}
            if not first:
                m["mT"] = mT_my[c]
                m["w_mo"] = layout_wmo(mix_w_out[l - 1])
                m["w_in2"] = ffn_w_in[l - 1, 1]
                m["w_out2"] = ffn_w_out[l - 1, 1]
                m["g345"] = np.ascontiguousarray(norm_gain[l - 1, 3:6])
            if not last:
                m["w_in1"] = ffn_w_in[l, 0]
                m["w_out1"] = ffn_w_out[l, 0]
                m["g012"] = np.ascontiguousarray(norm_gain[l, 0:3])
            in_maps.append(m)
        nc = _prog(("A", first, last), lambda: build_A(first, last))
        res = run_bass_kernel_spmd(nc, in_maps, core_ids=cores).results
        xc = [res[c]["xo"] for c in cores]
        if last:
            break
        hT = [res[c]["hTo"] for c in cores]
        in_maps = []
        for c in cores:
            b, hf = c // 2, c % 2
            cw_, cb_, gb_, mg_ = layout_ml_params(conv_w[l], conv_b[l], gate_bias[l], mlstm_norm_gain[l], hf)
            in_maps.append({"hT": np.ascontiguousarray(np.stack([hT[2 * b], hT[2 * b + 1]])),
                            "wA": layout_wA(mix_w_in[l], hf), "wB": layout_wB(mix_w_in[l], hf),
                            "cw": cw_, "cb": cb_, "gb": gb_, "mg": mg_})
        nc = _prog("B", build_B)
        res = run_bass_kernel_spmd(nc, in_maps, core_ids=cores).results
        mT = [res[c]["mT"] for c in cores]
        mT_my = []
        for c in cores:
            b, hf = c // 2, c % 2
            mT_my.append(np.ascontiguousarray(np.concatenate([mT[2 * b][:, :, hf * T:(hf + 1) * T], mT[2 * b + 1][:, :, hf * T:(hf + 1) * T]], axis=0)))
    out = np.empty((4, S, D), np.float32)
    for c in cores:
        out[c // 2, (c % 2) * T:(c % 2 + 1) * T] = xc[c]
    return out
```

```python
from contextlib import ExitStack
import math

import numpy as np
import ml_dtypes

import concourse.bass as bass
import concourse.mybir as mybir
from concourse.bass_utils import run_bass_kernel_spmd

F32 = mybir.dt.float32
BF16 = mybir.dt.bfloat16
AF = mybir.ActivationFunctionType
ALU = mybir.AluOpType
AX = mybir.AxisListType

D = 1024
DFF = 2816
NFC = DFF // 128
T = 2048
S = 4096
EPS = 1e-6
STOP = 0


class Buf:
    __slots__ = ("w", "r", "excl")

    def __init__(self, excl=False):
        self.w = {}
        self.r = {}
        self.excl = excl


class Ctx:
    def __init__(self, nc, es):
        self.nc = nc
        self.es = es
        self.eng = {"pe": nc.tensor, "act": nc.scalar, "dve": nc.vector, "pool": nc.gpsimd, "sp": nc.sync}
        self.prog = {}
        self.waited = {e: {} for e in self.eng}
        self.nsem = 0
        self.dsem = {}

    def newsem(self):
        self.nsem += 1
        return self.es.enter_context(self.nc.semaphore(f"s{self.nsem}"))

    def wait(self, e, toks):
        w = self.waited[e]
        need = {}
        for t in toks:
            if t is None:
                continue
            sem, val = t
            k = id(sem)
            if w.get(k, 0) < val and need.get(k, (None, 0))[1] < val:
                need[k] = (sem, val)
        for k, (sem, val) in need.items():
            self.eng[e].wait_ge(sem, val)
            w[k] = val

    def _deps(self, R, W):
        d = []
        for b in R:
            d += list(b.w.values())
        for b in W:
            d += list(b.w.values())
            d += list(b.r.values())
        return d

    def _mark(self, tok, R, W):
        k = id(tok[0])
        for b in R:
            if b.r.get(k, (None, 0))[1] < tok[1]:
                b.r[k] = tok
        for b in W:
            b.w = {k: tok}
            b.r = {}

    def _sig(self, e, ins):
        p = self.prog.get(e)
        if p is None or p[1] >= 30000:
            p = [self.newsem(), 0]
            self.prog[e] = p
        p[1] += 1
        ins.then_inc(p[0], 1)
        return (p[0], p[1])

    @staticmethod
    def _split(R, W):
        if any(b.excl for b in R):
            W = list(W) + [b for b in R if b.excl]
            R = [b for b in R if not b.excl]
        return R, W

    def op(self, e, fn, R=(), W=(), deps=()):
        R, W = self._split(R, W)
        self.wait(e, self._deps(R, W) + list(deps))
        ins = fn(self.eng[e])
        tok = self._sig(e, ins)
        self._mark(tok, R, W)
        return tok

    def mm(self, mms, R=(), W=(), deps=()):
        R, W = self._split(R, W)
        self.wait("pe", self._deps(R, W) + list(deps))
        ins = None
        for (o, l, r, st, sp) in mms:
            ins = self.nc.tensor.matmul(o, lhsT=l, rhs=r, start=st, stop=sp)
        tok = self._sig("pe", ins)
        self._mark(tok, R, W)
        return tok

    def dma(self, q, key, out, in_, R=(), W=(), deps=(), **kw):
        self.wait(q, self._deps(R, W) + list(deps))
        sem = self.dsem.get(key)
        if sem is None:
            sem = [self.newsem(), 0]
            self.dsem[key] = sem
        sem[1] += 16
        self.eng[q].dma_start(out=out, in_=in_, **kw).then_inc(sem[0], 16)
        tok = (sem[0], sem[1])
        self._mark(tok, R, W)
        return tok

    def dma_multi(self, q, key, pairs, R=(), W=(), **kw):
        self.wait(q, self._deps(R, W))
        sem = self.dsem.get(key)
        if sem is None:
            sem = [self.newsem(), 0]
            self.dsem[key] = sem
        for (o, i) in pairs:
            sem[1] += 16
            self.eng[q].dma_start(out=o, in_=i, **kw).then_inc(sem[0], 16)
        tok = (sem[0], sem[1])
        self._mark(tok, R, W)
        return tok

    def collective(self, kind, in_ap, out_ap, groups, R=(), W=()):
        self.wait("pool", self._deps(R, W))
        sem = self.dsem.get("cc")
        if sem is None:
            sem = [self.newsem(), 0]
            self.dsem["cc"] = sem
        sem[1] += 1
        self.nc.gpsimd.collective_compute(kind, ALU.bypass, replica_groups=groups, ins=[in_ap], outs=[out_ap]).then_inc(sem[0], 1)
        tok = (sem[0], sem[1])
        self._mark(tok, R, W)
        return tok

    def barrier(self, exclude=()):
        toks = [(p[0], p[1]) for p in self.prog.values()] + [(d[0], d[1]) for k, d in self.dsem.items() if k not in exclude]
        self.all_wait(toks)

    def all_wait(self, toks):
        for e in self.eng:
            self.wait(e, toks)


class Rot:
    def __init__(self, aps, excl=False):
        self.slots = [(a, Buf(excl)) for a in aps]
        self.i = 0

    def next(self):
        s = self.slots[self.i % len(self.slots)]
        self.i += 1
        return s


def prenorm_hT(c, x_in, g_pre, kc, psum, sb):
    nc = c.nc
    NT = T // 128
    hT = sb["hT"]
    hT_b = [Buf() for _ in range(NT)]
    gpre_b = Buf()
    c.dma("sp", "gpre", sb["gpre"], g_pre.rearrange("(k p) -> p k", p=128), W=[gpre_b], allow_slow_non_contiguous=True)
    xt, xs, st = sb["xt"], sb["xs"], sb["stat"]
    for i in range(NT):
        xa, xb = xt.next()
        c.dma("sp", ("xt", xt.i % len(xt.slots)), xa, x_in[i * 128:(i + 1) * 128, :], W=[xb])
        sa, sbuf_ = st.next()
        xsa, xsb = xs.next()
        c.op("act", lambda e: e.activation(out=xsa, in_=xa, func=AF.Square, accum_out=sa[:, 0:1]), R=[xb], W=[sbuf_, xsb])
        c.op("act", lambda e: e.activation(out=sa[:, 1:2], in_=sa[:, 0:1], func=AF.Sqrt, scale=1.0 / D, bias=kc["eps"]), R=[sbuf_], W=[sbuf_])
        c.op("dve", lambda e: e.reciprocal(out=sa[:, 2:3], in_=sa[:, 1:2]), R=[sbuf_], W=[sbuf_])
        c.op("dve", lambda e: e.tensor_scalar(out=xsa, in0=xa, scalar1=sa[:, 2:3], scalar2=None, op0=ALU.mult), R=[xb, sbuf_], W=[xsb])
        pa, pb = psum.next()
        pT = pa.bitcast(BF16).rearrange("p (k t) -> p k t", k=8)
        c.wait("pe", c._deps([xsb], [pb]))
        ins = None
        for k in range(8):
            ins = nc.tensor.transpose(pT[:, k, :], xsa[:, k * 128:(k + 1) * 128], kc["ident"])
        tok = c._sig("pe", ins)
        c._mark(tok, [xsb], [pb])
        c.op("dve", lambda e: e.tensor_tensor(out=hT[:, :, i * 128:(i + 1) * 128], in0=pT,
                                              in1=sb["gpre"].unsqueeze(2).to_broadcast([128, 8, 128]), op=ALU.mult),
             R=[pb, gpre_b], W=[hT_b[i]])
    return hT_b


def out_phase(c, aT, a_bufs, nch, wo, wo_b, x_in, x_out, g_post, half, kc, psum, sb):
    NT = T // 128
    gpost_b = Buf()
    c.dma("sp", "gpost", sb["gpost"], g_post.partition_broadcast(128), W=[gpost_b])
    xt, st, ys = sb["xt"], sb["stat"], sb["y"]
    sc = 4.0 if half else 1.0
    epsb = kc["eps4"] if half else kc["eps"]
    toks = []
    for tt in range(NT):
        ya, yb = ys.next()
        xa, xb = xt.next()
        c.dma("sp", ("xt", xt.i % len(xt.slots)), xa, x_in[tt * 128:(tt + 1) * 128, :], W=[xb])
        sa, sbuf_ = st.next()
        for dh in range(2):
            pa, pb = psum.next()
            c.mm([(pa, aT[:, ch, tt * 128:(tt + 1) * 128], wo[:, ch, dh * 512:(dh + 1) * 512], ch == 0, ch == nch - 1) for ch in range(nch)],
                 R=[a_bufs[ch][tt // 4] for ch in range(nch)] + wo_b, W=[pb])
            c.op("dve", lambda e: e.tensor_copy(out=ya[:, dh * 512:(dh + 1) * 512], in_=pa), R=[pb], W=[yb])
            ja, jb = sb["tmp"].next()
            c.op("act", lambda e: e.activation(out=ja, in_=ya[:, dh * 512:(dh + 1) * 512], func=AF.Square,
                                               accum_out=sa[:, dh:dh + 1]), R=[yb], W=[sbuf_, jb])
        c.op("dve", lambda e: e.tensor_tensor(out=sa[:, 2:3], in0=sa[:, 0:1], in1=sa[:, 1:2], op=ALU.add), R=[sbuf_], W=[sbuf_])
        c.op("act", lambda e: e.activation(out=sa[:, 3:4], in_=sa[:, 2:3], func=AF.Sqrt, scale=sc / D, bias=epsb), R=[sbuf_], W=[sbuf_])
        c.op("dve", lambda e: e.reciprocal(out=sa[:, 4:5], in_=sa[:, 3:4]), R=[sbuf_], W=[sbuf_])
        c.op("dve", lambda e: e.scalar_tensor_tensor(out=ya, in0=ya, scalar=sa[:, 4:5], in1=sb["gpost"], op0=ALU.mult, op1=ALU.mult),
             R=[sbuf_, gpost_b, yb], W=[yb])
        c.op("dve", lambda e: e.tensor_tensor(out=ya, in0=ya, in1=xa, op=ALU.add), R=[xb, yb], W=[yb])
        toks.append(c.dma("sp", ("xo", tt % 4), x_out[tt * 128:(tt + 1) * 128, :], ya, R=[yb]))
    return toks


def ffn_stage(c, x_in, x_out, w_in, w_out, g_pre, g_post, kc, psum, sb):
    hT, aT, wo = sb["hT"], sb["aT"], sb["wo"]
    aT_b = [[Buf() for _ in range(T // 512)] for _ in range(NFC)]
    wo_b = [Buf() for _ in range(NFC)]
    win = sb["win"]
    w_in_v = w_in.rearrange("(k p) f -> p k f", p=128)
    win_loaded = {}
    win_ub = {}

    def load_win(fc):
        ap, b = win.next()
        bu = win_ub.setdefault(id(b), Buf())
        c.dma("pool", ("win", win.i % len(win.slots)), ap[:, 0], w_in_v[:, :, fc * 128:(fc + 1) * 128], W=[b])
        c.dma("pool", ("winu", win.i % len(win.slots)), ap[:, 1], w_in_v[:, :, DFF + fc * 128:DFF + (fc + 1) * 128], W=[bu])
        win_loaded[fc] = (ap, b, bu)

    NPRE = min(len(win.slots), NFC)
    for fc in range(NPRE):
        load_win(fc)
    hT_b = prenorm_hT(c, x_in, g_pre, kc, psum, sb)
    w_out_v = w_out.rearrange("(c p) d -> p c d", p=128)
    for fc in range(NFC):
        c.dma("pool", ("wo", fc), wo[:, fc, :], w_out_v[:, fc, :], W=[wo_b[fc]])
    tmp = sb["tmp"]
    for fc in range(NFC):
        wa, wb, wub = win_loaded.pop(fc)
        for tb in range(T // 512):
            pg, pgb = psum.next()
            pu, pub = psum.next()
            hb = hT_b[tb * 4:(tb + 1) * 4]
            c.mm([(pg, wa[:, 0, k, :], hT[:, k, tb * 512:(tb + 1) * 512], k == 0, k == 7) for k in range(8)], R=[wb] + hb, W=[pgb])
            c.mm([(pu, wa[:, 1, k, :], hT[:, k, tb * 512:(tb + 1) * 512], k == 0, k == 7) for k in range(8)], R=[wub] + hb, W=[pub])
            ta, tb_ = tmp.next()
            c.op("act", lambda e: e.activation(out=ta, in_=pg, func=AF.Silu), R=[pgb], W=[tb_])
            c.op("dve", lambda e: e.tensor_tensor(out=aT[:, fc, tb * 512:(tb + 1) * 512], in0=pu, in1=ta, op=ALU.mult),
                 R=[pub, tb_], W=[aT_b[fc][tb]])
        if fc + NPRE < NFC:
            load_win(fc + NPRE)
    return out_phase(c, aT, aT_b, NFC, wo, wo_b, x_in, x_out, g_post, True, kc, psum, sb)


def mixout_stage(c, x_in, x_out, mT_d, w_mo, g_post, kc, psum, sb, sel=None, dep=None):
    mT = sb["hT"]
    wo = sb["wo"]
    m_b = [[Buf() for _ in range(T // 512)] for _ in range(8)]
    wo_b = [Buf() for _ in range(8)]
    w_v = w_mo.rearrange("(c p) d -> p c d", p=128)
    R0 = [dep] if dep is not None else []
    if sel is not None:
        selb = Buf()
        c.dma("sp", "sel", sb["sel"], sel, W=[selb])
        tmp = sb["aT"]
    for ch in range(8):
        c.dma("pool", ("wo", ch), wo[:, ch, :], w_v[:, ch, :], W=[wo_b[ch]])
        if sel is None:
            c.dma("sp", ("mT", ch), mT[:, ch, :], mT_d[ch], R=R0, W=m_b[ch])
        else:
            tb_ = Buf()
            c.dma("sp", ("mT", ch), mT[:, ch, :], mT_d[ch][:, 0:T], R=R0, W=m_b[ch])
            c.dma("sp", ("mT2", ch), tmp[:, ch, :], mT_d[ch][:, T:2 * T], R=R0, W=[tb_])
            c.op("dve", lambda e: e.tensor_scalar(out=mT[:, ch, :], in0=mT[:, ch, :], scalar1=sb["sel"][:, 0:1], scalar2=None, op0=ALU.mult),
                 R=[selb], W=m_b[ch])
            c.op("dve", lambda e: e.scalar_tensor_tensor(out=mT[:, ch, :], in0=tmp[:, ch, :], scalar=sb["sel"][:, 1:2], in1=mT[:, ch, :],
                                                         op0=ALU.mult, op1=ALU.add), R=[selb, tb_], W=m_b[ch])
    return out_phase(c, mT, m_b, 8, wo, wo_b, x_in, x_out, g_post, False, kc, psum, sb)


def hT_stage(c, x_in, g_pre, hT_out, kc, psum, sb):
    hT_b = prenorm_hT(c, x_in, g_pre, kc, psum, sb)
    return [c.dma("sp", "hTo", hT_out.rearrange("(k p) t -> p k t", p=128), sb["hT"], R=hT_b)]


_TAG = [0]


def _t(nc, es, name, shape, dt):
    return es.enter_context(nc.sbuf_tensor(f"{name}_{_TAG[0]}", shape, dt))[:]


def alloc_ffn_sb(nc, es):
    _TAG[0] += 1
    sb = {}
    sb["hT"] = _t(nc, es, "hT", [128, 8, T], BF16)
    sb["aT"] = _t(nc, es, "aT", [128, NFC, T], BF16)
    sb["wo"] = _t(nc, es, "wo", [128, NFC, D], BF16)
    sb["win"] = [_t(nc, es, f"win{i}", [128, 2, 8, 128], BF16) for i in range(3)]
    sb["xt"] = [_t(nc, es, f"xt{i}", [128, D], F32) for i in range(2)]
    sb["xs"] = [_t(nc, es, f"xs{i}", [128, D], BF16) for i in range(2)]
    sb["y"] = [_t(nc, es, f"y{i}", [128, D], F32) for i in range(2)]
    sb["tmp"] = [_t(nc, es, f"tmp{i}", [128, 512], BF16) for i in range(3)]
    sb["stat"] = [_t(nc, es, f"stat{i}", [128, 8], F32) for i in range(4)]
    sb["gpre"] = _t(nc, es, "gpre", [128, 8], F32)
    sb["gpost"] = _t(nc, es, "gpost", [128, D], F32)
    sb["sel"] = _t(nc, es, "sel", [128, 2], F32)
    for k in ("win", "xt", "xs", "y", "tmp", "stat"):
        sb[k] = Rot(sb[k])
    return sb


def setup_consts(c, es):
    nc = c.nc
    k = {}
    identf = es.enter_context(nc.sbuf_tensor("identf", [128, 128], F32))[:]
    k["ident"] = es.enter_context(nc.sbuf_tensor("ident", [128, 128], BF16))[:]
    k["eps"] = es.enter_context(nc.sbuf_tensor("eps", [128, 1], F32))[:]
    k["eps4"] = es.enter_context(nc.sbuf_tensor("eps4", [128, 1], F32))[:]
    k["one"] = es.enter_context(nc.sbuf_tensor("one", [128, 1], F32))[:]
    k["lnk"] = es.enter_context(nc.sbuf_tensor("lnk", [128, 1], F32))[:]
    k["identf"] = identf
    b = Buf()
    c.op("pool", lambda e: e.memset(identf, 1.0), W=[b])
    c.op("pool", lambda e: e.affine_select(out=identf, in_=identf, pattern=[[-1, 128]], compare_op=ALU.is_equal,
                                           fill=0.0, base=0, channel_multiplier=1), R=[b], W=[b])
    c.op("pool", lambda e: e.tensor_copy(out=k["ident"], in_=identf), R=[b], W=[b])
    c.op("pool", lambda e: e.memset(k["eps"], EPS), W=[b])
    c.op("pool", lambda e: e.memset(k["one"], 1.0), W=[b])
    c.op("pool", lambda e: e.memset(k["lnk"], -0.5 * math.log(128.0)), W=[b])
    t = c.op("pool", lambda e: e.memset(k["eps4"], 4.0 * EPS), W=[b])
    c.all_wait([t])
    return k


import math

I32 = mybir.dt.int32
NH_A = 4
DILS = (1, 4, 16)
ROPE_THETA = 500000.0


def setup_mixer_consts(c, es, rope_dram=None):
    nc = c.nc
    k = {}
    b = Buf()
    k["b"] = b

    def sbt(name, shape, dt):
        return es.enter_context(nc.sbuf_tensor(name, shape, dt))[:]

    k["ones128"] = sbt("ones128", [128, 128], F32)
    k["ones64"] = sbt("ones64", [128, 64], BF16)
    k["mask2"] = sbt("mask2", [128, 256], BF16)
    k["trif"] = sbt("trif", [128, 128], F32)
    k["trib"] = sbt("trib", [128, 128], F32)
    t8 = sbt("t8", [128, 8], F32)
    k["t8"] = t8
    k["selA"] = sbt("selA", [128, 128], F32)
    k["selB"] = sbt("selB", [128, 128], F32)
    c.op("pool", lambda e: e.memset(k["selA"], 0.0), W=[b])
    c.op("pool", lambda e: e.memset(k["selB"], 0.0), W=[b])
    c.op("pool", lambda e: e.memset(k["selA"][:, 0:64], 1.0), R=[b], W=[b])
    c.op("pool", lambda e: e.memset(k["selB"][:, 64:128], 1.0), R=[b], W=[b])
    c.op("pool", lambda e: e.affine_select(out=k["selA"][:, 0:64], in_=k["selA"][:, 0:64], pattern=[[-1, 64]], compare_op=ALU.is_equal,
                                           fill=0.0, base=-64, channel_multiplier=1), R=[b], W=[b])
    c.op("pool", lambda e: e.affine_select(out=k["selB"][:, 64:128], in_=k["selB"][:, 64:128], pattern=[[-1, 64]], compare_op=ALU.is_equal,
                                           fill=0.0, base=0, channel_multiplier=1), R=[b], W=[b])
    es_tmp = ExitStack()
    onesf = es_tmp.enter_context(nc.sbuf_tensor("onesf", [128, 384], F32))[:]
    c.op("pool", lambda e: e.memset(k["ones128"], 1.0), W=[b])
    c.op("pool", lambda e: e.memset(k["ones64"], 1.0), W=[b])
    c.op("pool", lambda e: e.memset(onesf, 1.0), W=[b])
    c.op("pool", lambda e: e.affine_select(out=onesf[:, 0:128], in_=onesf[:, 0:128], pattern=[[-1, 128]], compare_op=ALU.is_ge,
                                           fill=0.0, base=-64, channel_multiplier=1), R=[b], W=[b])
    c.op("pool", lambda e: e.affine_select(out=onesf[:, 128:256], in_=onesf[:, 128:256], pattern=[[1, 128]], compare_op=ALU.is_ge,
                                           fill=0.0, base=64, channel_multiplier=-1), R=[b], W=[b])
    c.op("pool", lambda e: e.affine_select(out=onesf[:, 128:256], in_=onesf[:, 128:256], pattern=[[-1, 128]], compare_op=ALU.is_ge,
                                           fill=0.0, base=64, channel_multiplier=1), R=[b], W=[b])
    c.op("pool", lambda e: e.affine_select(out=onesf[:, 256:384], in_=onesf[:, 256:384], pattern=[[1, 128]], compare_op=ALU.is_ge,
                                           fill=0.0, base=-64, channel_multiplier=-1), R=[b], W=[b])
    c.op("pool", lambda e: e.tensor_copy(out=k["mask2"][:, 0:64], in_=onesf[:, 0:64]), R=[b], W=[b])
    c.op("pool", lambda e: e.tensor_copy(out=k["mask2"][:, 64:192], in_=onesf[:, 128:256]), R=[b], W=[b])
    c.op("pool", lambda e: e.tensor_copy(out=k["mask2"][:, 192:256], in_=onesf[:, 320:384]), R=[b], W=[b])
    c.op("pool", lambda e: e.affine_select(out=k["trif"], in_=k["ones128"], pattern=[[1, 128]], compare_op=ALU.is_ge,
                                           fill=0.0, base=0, channel_multiplier=-1), R=[b], W=[b])
    c.op("pool", lambda e: e.affine_select(out=k["trib"], in_=k["ones128"], pattern=[[-1, 128]], compare_op=ALU.is_ge,
                                           fill=0.0, base=0, channel_multiplier=1), R=[b], W=[b])
    if rope_dram is None:
        es_tmp.close()
        es_tmp = ExitStack()
    with es_tmp as es2:
        esr = es if rope_dram is None else es2
        k["ropeC"] = esr.enter_context(nc.sbuf_tensor("ropeC", [128, S], F32))[:]
        k["ropeS"] = esr.enter_context(nc.sbuf_tensor("ropeS", [128, S], F32))[:]
        def sbt2(name, shape, dt):
            return es2.enter_context(nc.sbuf_tensor(name, shape, dt))[:]
        posi = sbt2("posi", [128, S], I32)
        ang = sbt2("ang", [128, S], F32)
        kf = sbt2("kf", [128, S], F32)
        pidx = sbt2("pidx", [128, 1], I32)
        ti = sbt2("ti", [128, 2], I32)
        c.op("pool", lambda e: e.iota(posi, pattern=[[1, S]], base=0, channel_multiplier=0), W=[b])
        c.op("pool", lambda e: e.iota(pidx, pattern=[[0, 1]], base=0, channel_multiplier=1), R=[b], W=[b])
        c.op("dve", lambda e: e.tensor_single_scalar(out=ti[:, 0:1], in_=pidx, scalar=7, op=ALU.bitwise_and), R=[b], W=[b])
        c.op("dve", lambda e: e.tensor_single_scalar(out=ti[:, 1:2], in_=pidx, scalar=63, op=ALU.bitwise_and), R=[b], W=[b])
        c.op("dve", lambda e: e.tensor_copy(out=t8[:, 0:2], in_=ti), R=[b], W=[b])
        c.op("act", lambda e: e.activation(out=t8[:, 2:3], in_=t8[:, 0:1], func=AF.Exp, scale=-math.log(ROPE_THETA) / 8), R=[b], W=[b])
        c.op("dve", lambda e: e.tensor_single_scalar(out=t8[:, 3:4], in_=t8[:, 1:2], scalar=16.0, op=ALU.is_lt), R=[b], W=[b])
        c.op("dve", lambda e: e.tensor_tensor(out=t8[:, 4:5], in0=t8[:, 2:3], in1=t8[:, 3:4], op=ALU.mult), R=[b], W=[b])
        c.op("dve", lambda e: e.tensor_scalar(out=t8[:, 5:6], in0=t8[:, 1:2], scalar1=8.0, scalar2=2.0, op0=ALU.is_ge, op1=ALU.mult), R=[b], W=[b])
        c.op("dve", lambda e: e.tensor_scalar_add(out=t8[:, 5:6], in0=t8[:, 5:6], scalar1=-1.0), R=[b], W=[b])
        c.op("dve", lambda e: e.tensor_copy(out=ang, in_=posi), R=[b], W=[b])
        c.op("dve", lambda e: e.tensor_scalar(out=ang, in0=ang, scalar1=t8[:, 4:5], scalar2=None, op0=ALU.mult), R=[b], W=[b])
        C1 = 6.28125
        C2 = 2 * math.pi - C1
        tok = None
        for which, dst in (("sin", k["ropeS"]), ("cos", k["ropeC"])):
            if which == "cos":
                c.op("dve", lambda e: e.tensor_scalar_add(out=ang, in0=ang, scalar1=math.pi / 2), R=[b], W=[b])
            c.op("dve", lambda e: e.tensor_scalar(out=posi, in0=ang, scalar1=1.0 / (2 * math.pi), scalar2=None, op0=ALU.mult), R=[b], W=[b])
            c.op("dve", lambda e: e.tensor_copy(out=kf, in_=posi), R=[b], W=[b])
            c.op("dve", lambda e: e.scalar_tensor_tensor(out=dst, in0=kf, scalar=-C1, in1=ang, op0=ALU.mult, op1=ALU.add), R=[b], W=[b])
            c.op("dve", lambda e: e.scalar_tensor_tensor(out=dst, in0=kf, scalar=-C2, in1=dst, op0=ALU.mult, op1=ALU.add), R=[b], W=[b])
            c.op("dve", lambda e: e.tensor_single_scalar(out=kf, in_=dst, scalar=math.pi, op=ALU.is_gt), R=[b], W=[b])
            c.op("dve", lambda e: e.scalar_tensor_tensor(out=dst, in0=kf, scalar=-2 * math.pi, in1=dst, op0=ALU.mult, op1=ALU.add), R=[b], W=[b])
            c.op("dve", lambda e: e.tensor_single_scalar(out=kf, in_=dst, scalar=-math.pi, op=ALU.is_lt), R=[b], W=[b])
            c.op("dve", lambda e: e.scalar_tensor_tensor(out=dst, in0=kf, scalar=2 * math.pi, in1=dst, op0=ALU.mult, op1=ALU.add), R=[b], W=[b])
            tok = c.op("act", lambda e: e.activation(out=dst, in_=dst, func=AF.Sin), R=[b], W=[b])
            if which == "sin":
                tok = c.op("dve", lambda e: e.tensor_scalar(out=dst, in0=dst, scalar1=t8[:, 5:6], scalar2=None, op0=ALU.mult), R=[b], W=[b])
        if rope_dram is not None:
            t1 = c.dma("sp", "ropeC", rope_dram[0], k["ropeC"], R=[b])
            t2 = c.dma("sp", "ropeS", rope_dram[1], k["ropeS"], R=[b])
            c.all_wait([t1, t2])
            del k["ropeC"], k["ropeS"]
        c.all_wait([tok])
    return k


def attn_pass(c, j, hv, wA, Vd, mk, psum, sb, mergedT, merged_b, hdep=None):
    nc = c.nc
    NTB = S // 512
    wa, qT, kT = sb["wa"], sb["qT"], sb["kT"]
    acc = [sb["accN"], sb["accZ"]]
    wa_b, vd_b = Buf(), Buf()
    qk_b = [Buf() for _ in range(NTB)]
    acc_b = [Buf(), Buf()]
    c.dma("pool", "wa", wa, wA[j].rearrange("(k p) f -> p k f", p=128), W=[wa_b])
    hblk = Rot(sb["hblk"])
    tmpA = Rot(sb["tmpA"])
    vnat = Rot(sb["vnat"])
    loaded = {}

    def load_h(tb):
        ap, b = hblk.next()
        c.dma_multi("sp", ("hblk", hblk.i % 2), [(ap[:, 4 * i2:4 * i2 + 4, :], hv(tb // 4, i2)[:, :, (tb % 4) * 512:(tb % 4 + 1) * 512])
                                                    for i2 in range(2)], R=[hdep] if hdep else [], W=[b])
        loaded[tb] = (ap, b)

    kz_b = Buf()
    c.op("pool", lambda e: e.memset(kT[64:128, 0, :], 0.0), W=[kz_b])
    c.op("pool", lambda e: e.memset(kT[0:64, 1, :], 0.0), W=[kz_b])
    for (va, vb) in vnat.slots:
        v4 = va.rearrange("p (tt a c) -> p tt a c", a=2, c=128)
        c.op("pool", lambda e: e.memset(v4[:, :, 0, 64:128], 1.0), W=[vb])
        c.op("pool", lambda e: e.memset(v4[:, :, 1, 0:64], 1.0), W=[vb])
    load_h(0)
    for tb in range(NTB):
        if tb + 1 < NTB:
            load_h(tb + 1)
        ha, hb = loaded.pop(tb)
        sl = slice(tb * 512, (tb + 1) * 512)
        for (dst, c0) in ((qT, 0), (kT, 128)):
            pq, pqb = psum.next()
            ps, psb = psum.next()
            c.mm([(pq, wa[:, k, c0:c0 + 128], ha[:, k, :], k == 0, k == 7) for k in range(8)], R=[wa_b, hb], W=[pqb])
            c.mm([(ps, wa[:, k, 256 + c0:256 + c0 + 128], ha[:, k, :], k == 0, k == 7) for k in range(8)], R=[wa_b, hb], W=[psb])
            t1, t1b = tmpA.next()
            t2, t2b = tmpA.next()
            c.op("dve", lambda e: e.tensor_tensor(out=t1, in0=pq, in1=mk["ropeC"][:, sl], op=ALU.mult), R=[pqb, mk["b"]], W=[t1b])
            c.op("dve", lambda e: e.tensor_tensor(out=t2, in0=ps, in1=mk["ropeS"][:, sl], op=ALU.mult), R=[psb, mk["b"]], W=[t2b])
            if c0 == 0:
                c.op("pool", lambda e: e.tensor_tensor(out=dst[:, sl], in0=t1, in1=t2, op=ALU.add), R=[t1b, t2b], W=[qk_b[tb]])
            else:
                c.op("pool", lambda e: e.tensor_tensor(out=kT[0:64, 0, sl], in0=t1[0:64, :], in1=t2[0:64, :], op=ALU.add),
                     R=[t1b, t2b, kz_b], W=[qk_b[tb]])
                c.op("pool", lambda e: e.tensor_tensor(out=kT[64:128, 1, sl], in0=t1[64:128, :], in1=t2[64:128, :], op=ALU.add),
                     R=[t1b, t2b, kz_b], W=[qk_b[tb]])
        pv, pvb = psum.next()
        for tt in range(4):
            c.mm([(pv[:, tt * 128:(tt + 1) * 128], ha[:, k, tt * 128:(tt + 1) * 128], wa[:, k, 512:640], k == 0, k == 7)
                  for k in range(8)], R=[wa_b, hb], W=[pvb])
        va, vb = vnat.next()
        v4 = va.rearrange("p (tt a c) -> p tt a c", a=2, c=128)
        pv3 = pv.rearrange("p (tt c) -> p tt c", c=128)
        c.op("act", lambda e: e.activation(out=v4[:, :, 0, 0:64], in_=pv3[:, :, 0:64], func=AF.Copy), R=[pvb], W=[vb])
        c.op("act", lambda e: e.activation(out=v4[:, :, 1, 64:128], in_=pv3[:, :, 64:128], func=AF.Copy), R=[pvb], W=[vb])
        c.dma("sp", "vd", Vd[tb * 512:(tb + 1) * 512].rearrange("(tt p) a c -> p tt (a c)", p=128),
              va.rearrange("p (tt c) -> p tt c", c=256), R=[vb], W=[vd_b])

    vg = Rot(sb["vg"])
    ptr = Rot(sb["pt"])
    psS = Rot([a for a, _ in psum.slots[0:4]])
    psS.slots = psum.slots[0:4]
    psO = Rot([a for a, _ in psum.slots[4:8]])
    psO.slots = psum.slots[4:8]
    LAG = 3
    for gi, d in enumerate(DILS):
        L = S // d
        nb = L // 128
        vga, vgb = vg.next()
        c.dma_multi("sp", "vg", [(vga[:, r * nb:(r + 1) * nb, :],
                                  Vd.rearrange("(m r) a c -> r m (a c)", r=d)[r].rearrange("(kb p) c -> p kb c", p=128)) for r in range(d)],
                    R=[vd_b], W=[vgb])

        def tok(r, blk, n=1):
            st = r + d * 128 * blk
            return slice(st, st + d * (128 * n - 1) + 1, d)

        items = []
        for hh in range(2):
            for r in range(d):
                for b0 in range(0, nb, 4):
                    nbt = min(4, nb - b0)
                    for bi in range(nbt):
                        items.append((hh, r, b0, nbt, bi))
        state = {}
        pend = []

        def phaseA(it):
            hh, r, b0, nbt, bi = it
            b = b0 + bi
            has_p, has_n = b - 1 >= 0, b + 1 < nb
            lo = 0 if has_p else 64
            hi = 256 if has_n else 192
            ps, psb = psS.next()
            qb = r + d * 128 * b
            qs = lambda i0, n: slice(qb + d * i0, qb + d * (i0 + n - 1) + 1, d)
            mms = [(ps[:, 64:192], kT[:, hh, tok(r, b)], qT[:, qs(0, 128)], True, True)]
            if has_p:
                mms.append((ps[:, 0:64], kT[:, hh, tok(r, b - 1)], qT[:, qs(0, 64)], True, True))
            if has_n:
                mms.append((ps[:, 192:256], kT[:, hh, tok(r, b + 1)], qT[:, qs(64, 64)], True, True))
            c.mm(mms, R=qk_b, W=[psb])
            pt, ptb = ptr.next()
            c.op("act", lambda e: e.activation(out=pt[:, lo:hi], in_=ps[:, lo:hi], func=AF.Exp, scale=0.125), R=[psb], W=[ptb])
            state["nmask"] = state.get("nmask", 0) + 1
            meng = "pool" if state["nmask"] % 2 == 0 else "dve"
            c.op(meng, lambda e: e.tensor_tensor(out=pt[:, lo:hi], in0=pt[:, lo:hi], in1=mk["mask2"][:, lo:hi], op=ALU.mult),
                 R=[ptb, mk["b"]], W=[ptb])
            return (pt, ptb, has_p, has_n)

        def phaseB(it, a):
            hh, r, b0, nbt, bi = it
            pt, ptb, has_p, has_n = a
            b = b0 + bi
            if bi == 0:
                state["pn"] = psO.next()
            pn, pnb = state["pn"]
            vcol = slice(hh * 128, (hh + 1) * 128)
            o = bi * 128
            mms = [(pn[:, o:o + 128], vga[:, r * nb + b, vcol], pt[:, 64:192], True, not (has_p or has_n))]
            if has_p:
                mms.append((pn[:, o:o + 64], vga[:, r * nb + b - 1, vcol], pt[:, 0:64], False, not has_n))
            if has_n:
                mms.append((pn[:, o + 64:o + 128], vga[:, r * nb + b + 1, vcol], pt[:, 192:256], False, True))
            c.mm(mms, R=[vgb, ptb], W=[pnb])
            if bi == nbt - 1:
                asl = tok(r, b0, nbt)
                w = nbt * 128
                if gi == 0:
                    c.op("dve", lambda e: e.tensor_copy(out=acc[hh][:, asl], in_=pn[:, 0:w]), R=[pnb], W=[acc_b[hh]])
                else:
                    c.op("dve", lambda e: e.tensor_tensor(out=acc[hh][:, asl], in0=pn[:, 0:w], in1=acc[hh][:, asl], op=ALU.add),
                         R=[pnb], W=[acc_b[hh]])

        for it in items:
            pend.append((it, phaseA(it)))
            if len(pend) > LAG:
                phaseB(*pend.pop(0))
        while pend:
            phaseB(*pend.pop(0))

    for q in range(S // 512):
        sl = slice(q * 512, (q + 1) * 512)
        pz, pzb = psum.next()
        c.mm([(pz, mk["selA"], acc[0][:, sl], True, False), (pz, mk["selB"], acc[1][:, sl], False, True)],
             R=[acc_b[0], acc_b[1], mk["b"]], W=[pzb])
        t2, t2b = tmpA.next()
        c.op("dve", lambda e: e.reciprocal(out=t2, in_=pz), R=[pzb], W=[t2b])
        c.op("dve", lambda e: e.tensor_tensor(out=mergedT[0:64, j, sl], in0=acc[0][0:64, sl], in1=t2[0:64, :], op=ALU.mult),
             R=[acc_b[0], t2b], W=[merged_b])
        c.op("pool", lambda e: e.tensor_tensor(out=mergedT[64:128, j, sl], in0=acc[1][64:128, sl], in1=t2[64:128, :], op=ALU.mult),
             R=[acc_b[1], t2b], W=[merged_b])


def alloc_attn_sb(nc, es):
    sb = {}

    _TAG[0] += 1

    def t(name, shape, dt):
        return _t(nc, es, name, shape, dt)
    sb["wa"] = t("wa", [128, 8, 640], BF16)
    sb["qT"] = t("qT", [128, S], BF16)
    sb["kT"] = t("kT", [128, 2, S], BF16)
    sb["accN"] = t("accN", [128, S], F32)
    sb["accZ"] = t("accZ", [128, S], F32)
    sb["hblk"] = [t(f"hblk{i}", [128, 8, 512], BF16) for i in range(2)]
    sb["tmpA"] = [t(f"tmpA{i}", [128, 512], F32) for i in range(4)]
    sb["vnat"] = [t(f"vnat{i}", [128, 1024], BF16) for i in range(2)]
    sb["vg"] = [t(f"vg{i}", [128, 32, 256], BF16) for i in range(1)]
    sb["pt"] = [t(f"pt{i}", [128, 384], BF16) for i in range(6)]
    return sb


def alloc_ml_sb(nc, es):
    sb = {}

    _TAG[0] += 1

    def t(name, shape, dt):
        return _t(nc, es, name, shape, dt)
    sb["wb"] = t("wb", [128, 8, 516], BF16)
    sb["hblk"] = [t(f"mhblk{i}", [128, 8, 512], BF16) for i in range(2)]
    sb["raw"] = t("raw", [128, 2, S + 4], BF16)
    sb["qkT"] = t("qkT", [128, 2, S], BF16)
    sb["sigo"] = t("sigo", [128, S], BF16)
    sb["vaug"] = t("vaug", [128, S // 128, 129], BF16)
    sb["ktok"] = t("ktok", [128, S // 128, 128], BF16)
    sb["hsum"] = t("hsum", [128, S // 128, 128], F32)
    sb["hb"] = t("hb", [128, S // 128, 128], F32)
    sb["yn"] = t("yn", [128, S // 128, 128], BF16)
    sb["cw"] = t("cw", [128, 2, 5], F32)
    sb["cb"] = t("cb", [128, 2], F32)
    sb["gb"] = t("gb", [128, 4], F32)
    sb["mg"] = t("mg", [128, 1], F32)
    sb["dg"] = t("dg", [128, 2, 5, 128], BF16)
    NCH = S // 128
    for nm in ("G", "Gb"):
        sb[nm] = t(nm, [128, NCH, 4], F32)
    for nm in ("E1", "LF", "CUM", "TOT", "I_", "B_", "EB", "EC", "ET", "W_", "TB", "DEN", "RR"):
        sb[nm] = t(nm, [128, 2, NCH], F32)
    sb["ssq"] = t("ssq", [128, NCH], F32)
    sb["rstd"] = t("mrstd", [128, NCH], F32)
    sb["CT"] = [t(f"CT{i}", [128, 129], F32) for i in range(2)]
    sb["CTb"] = [t(f"CTb{i}", [128, 129], BF16) for i in range(2)]
    sb["A"] = [t(f"A{i}", [128, 128], BF16) for i in range(6)]
    sb["kpp"] = [t(f"kpp{i}", [128, 128], BF16) for i in range(6)]
    sb["ep"] = [t(f"ep{i}", [128, 4], F32) for i in range(4)]
    sb["mjunk"] = t("mjunk", [128, 128], BF16)
    return sb


def mlstm_pass(c, hd, hv, wB, convw, convb, gbias, mgain, mk, kc, psum, sb, mergedT, merged_b, hdep=None):
    nc = c.nc
    NTB = S // 512
    NCH = S // 128
    wb, raw, qkT, sigo, vaug, ktok, hsum = sb["wb"], sb["raw"], sb["qkT"], sb["sigo"], sb["vaug"], sb["ktok"], sb["hsum"]
    wb_b, par_b, dg_b = Buf(), Buf(), Buf()
    raw_b = [Buf() for _ in range(NTB)]
    rawpad_b = Buf()
    qk_b = [Buf() for _ in range(NTB)]
    sig_b, va_b, g_b, kt_b, hs_b = Buf(), Buf(), Buf(), Buf(), Buf()
    c.dma("pool", "wb", wb, wB[hd].rearrange("(k p) f -> p k f", p=128), W=[wb_b])
    c.dma("sp", "cw", sb["cw"], convw[hd], W=[par_b])
    c.dma("sp", "cb", sb["cb"], convb[hd], W=[par_b])
    c.dma("sp", "gb", sb["gb"], gbias[hd].partition_broadcast(128), W=[par_b])
    c.dma("sp", "mg", sb["mg"], mgain[hd], W=[par_b])
    for qk in range(2):
        for jj in range(5):
            c.op("dve", lambda e: e.tensor_scalar(out=sb["dg"][:, qk, jj, :], in0=kc["identf"], scalar1=sb["cw"][:, qk, jj:jj + 1],
                                                  scalar2=None, op0=ALU.mult), R=[par_b], W=[dg_b])
    c.op("pool", lambda e: e.memset(raw[:, :, 0:2], 0.0), W=[rawpad_b])
    c.op("pool", lambda e: e.memset(raw[:, :, S + 2:S + 4], 0.0), W=[rawpad_b])
    c.op("pool", lambda e: e.memset(vaug[:, :, 128:129], 1.0), W=[va_b])

    hblk = Rot(sb["hblk"])
    loaded = {}

    def load_h(tb):
        ap, b = hblk.next()
        c.dma_multi("sp", ("mhblk", hblk.i % 2), [(ap[:, 4 * i2:4 * i2 + 4, :], hv(tb // 4, i2)[:, :, (tb % 4) * 512:(tb % 4 + 1) * 512])
                                                    for i2 in range(2)], R=[hdep] if hdep else [], W=[b])
        loaded[tb] = (ap, b)

    load_h(0)
    for tb in range(NTB):
        if tb + 1 < NTB:
            load_h(tb + 1)
        ha, hb = loaded.pop(tb)
        sl = slice(tb * 512, (tb + 1) * 512)
        for fi in range(3):
            pq, pqb = psum.next()
            c.mm([(pq, wb[:, k, fi * 128:(fi + 1) * 128], ha[:, k, :], k == 0, k == 7) for k in range(8)], R=[wb_b, hb], W=[pqb])
            if fi < 2:
                c.op("act", lambda e: e.activation(out=raw[:, fi, 2 + tb * 512:2 + (tb + 1) * 512], in_=pq, func=AF.Copy), R=[pqb], W=[raw_b[tb]])
            else:
                c.op("act", lambda e: e.activation(out=sigo[:, sl], in_=pq, func=AF.Sigmoid), R=[pqb], W=[sig_b])
        for half in range(2):
            pv, pvb = psum.next()
            for t2 in range(2):
                tt = half * 2 + t2
                c.mm([(pv[:, t2 * 132:(t2 + 1) * 132], ha[:, k, tt * 128:(tt + 1) * 128], wb[:, k, 384:516], k == 0, k == 7)
                      for k in range(8)], R=[wb_b, hb], W=[pvb])
            c0 = tb * 4 + half * 2
            pv3 = pv[:, 0:264].rearrange("p (t f) -> p t f", f=132)
            c.op("dve", lambda e: e.tensor_copy(out=vaug[:, c0:c0 + 2, 0:128], in_=pv3[:, :, 0:128]), R=[pvb], W=[va_b])
            c.op("dve", lambda e: e.tensor_copy(out=sb["G"][:, c0:c0 + 2, :], in_=pv3[:, :, 128:132]), R=[pvb], W=[g_b])

    for qk in range(2):
        for tb in range(NTB):
            pq, pqb = psum.next()
            rb = [raw_b[t] for t in (tb - 1, tb, tb + 1) if 0 <= t < NTB] + [rawpad_b, dg_b]
            c.mm([(pq, sb["dg"][:, qk, jj, :], raw[:, qk, tb * 512 + jj:tb * 512 + jj + 512], jj == 0, jj == 4) for jj in range(5)],
                 R=rb, W=[pqb])
            c.op("act", lambda e: e.activation(out=qkT[:, qk, tb * 512:(tb + 1) * 512], in_=pq, func=AF.Silu, bias=sb["cb"][:, qk:qk + 1]),
                 R=[pqb, par_b], W=[qk_b[tb]])
    for tb in range(NTB):
        pa, pb = psum.next()
        pT = pa.bitcast(BF16)[:, 0:512].rearrange("p (k t) -> p k t", k=4)
        c.wait("pe", c._deps([qk_b[tb]], [pb]))
        ins = None
        for k in range(4):
            ins = nc.tensor.transpose(pT[:, k, :], qkT[:, 1, tb * 512 + k * 128:tb * 512 + (k + 1) * 128], kc["ident"])
        tok = c._sig("pe", ins)
        c._mark(tok, [qk_b[tb]], [pb])
        c.op("dve", lambda e: e.tensor_copy(out=ktok[:, tb * 4:(tb + 1) * 4, :], in_=pT), R=[pb], W=[kt_b])

    G, Gb = sb["G"], sb["Gb"]
    gm = Buf()
    c.op("dve", lambda e: e.tensor_tensor(out=Gb, in0=G, in1=sb["gb"].unsqueeze(1).to_broadcast([128, NCH, 4]), op=ALU.add), R=[g_b, par_b], W=[gm])
    fview = Gb[:, :, 1::2].rearrange("p c g -> p g c")
    iview = Gb[:, :, 0::2].rearrange("p c g -> p g c")
    c.op("act", lambda e: e.activation(out=sb["E1"], in_=fview, func=AF.Exp, scale=-1.0), R=[gm], W=[gm])
    c.op("act", lambda e: e.activation(out=sb["E1"], in_=sb["E1"], func=AF.Ln, bias=kc["one"]), R=[gm], W=[gm])
    c.op("dve", lambda e: e.tensor_scalar(out=sb["LF"], in0=sb["E1"], scalar1=-1.0, scalar2=None, op0=ALU.mult), R=[gm], W=[gm])
    pg, pgb = psum.next()
    c.mm([(pg[:, 0:NCH], mk["trif"], sb["LF"][:, 0, :], True, True),
          (pg[:, NCH:2 * NCH], mk["trib"], sb["LF"][:, 1, :], True, True),
          (pg[:, 2 * NCH:4 * NCH], mk["ones128"], sb["LF"].rearrange("p a c -> p (a c)"), True, True)], R=[gm, mk["b"]], W=[pgb])
    c.op("dve", lambda e: e.tensor_copy(out=sb["CUM"].rearrange("p a c -> p (a c)"), in_=pg[:, 0:2 * NCH]), R=[pgb], W=[gm])
    c.op("dve", lambda e: e.tensor_copy(out=sb["TOT"].rearrange("p a c -> p (a c)"), in_=pg[:, 2 * NCH:4 * NCH]), R=[pgb], W=[gm])
    c.op("dve", lambda e: e.tensor_tensor(out=sb["B_"], in0=iview, in1=sb["CUM"], op=ALU.subtract), R=[gm], W=[gm])
    c.op("act", lambda e: e.activation(out=sb["EB"], in_=sb["B_"], func=AF.Exp, bias=kc["lnk"]), R=[gm], W=[gm])
    c.op("act", lambda e: e.activation(out=sb["EC"], in_=sb["CUM"], func=AF.Exp), R=[gm], W=[gm])
    c.op("act", lambda e: e.activation(out=sb["ET"], in_=sb["TOT"], func=AF.Exp), R=[gm], W=[gm])
    c.op("dve", lambda e: e.tensor_tensor(out=sb["TB"], in0=sb["TOT"], in1=sb["B_"], op=ALU.add), R=[gm], W=[gm])
    c.op("act", lambda e: e.activation(out=sb["W_"], in_=sb["TB"], func=AF.Exp, bias=kc["lnk"]), R=[gm], W=[gm])

    Arot, Krot = Rot(sb["A"]), Rot(sb["kpp"])
    den_b = Buf()
    ct_b = [Buf(), Buf()]
    ctb_b = [Buf(), Buf()]
    qall = qk_b
    def pre(i, dr):
        ch = i if dr == 0 else NCH - 1 - i
        cs = slice(ch * 128, (ch + 1) * 128)
        tri = mk["trif"] if dr == 0 else mk["trib"]
        ps, psb = psum.next()
        c.mm([(ps[:, 0:128], qkT[:, 1, cs], qkT[:, 0, cs], True, True)], R=qall, W=[psb])
        aa, ab = Arot.next()
        c.op("dve", lambda e: e.scalar_tensor_tensor(out=aa, in0=ps[:, 0:128], scalar=sb["EB"][:, dr, ch:ch + 1], in1=tri,
                                                     op0=ALU.mult, op1=ALU.mult), R=[psb, gm, mk["b"]], W=[ab])
        ka, kb_ = Krot.next()
        if i < NCH - 1:
            c.op("act", lambda e: e.activation(out=ka, in_=ktok[:, ch, :], func=AF.Copy, scale=sb["W_"][:, dr, ch:ch + 1]),
                 R=[kt_b, gm], W=[kb_])
        return (aa, ab, ka, kb_)

    pres = {(0, 0): pre(0, 0), (0, 1): pre(0, 1)}
    for i in range(NCH):
        for dr, ch in ((0, i), (1, NCH - 1 - i)):
            if i + 1 < NCH:
                pres[(i + 1, dr)] = pre(i + 1, dr)
            aa, ab, ka, kb_ = pres.pop((i, dr))
            cs = slice(ch * 128, (ch + 1) * 128)
            pn, pnb = psum.next()
            mms = [(pn[:, 0:129], aa, vaug[:, ch, :], True, i == 0)]
            if i > 0:
                mms.append((pn[:, 0:129], qkT[:, 0, cs], sb["CTb"][dr], False, True))
            c.mm(mms, R=[ab, va_b, ctb_b[dr]] + qall, W=[pnb])
            if i < NCH - 1:
                pc, pcb = psum.next()
                c.mm([(pc[:, 0:129], ka, vaug[:, ch, :], True, True)], R=[kb_, va_b], W=[pcb])
                if i == 0:
                    c.op("dve", lambda e: e.tensor_copy(out=sb["CT"][dr], in_=pc[:, 0:129]), R=[pcb], W=[ct_b[dr]])
                else:
                    c.op("dve", lambda e: e.scalar_tensor_tensor(out=sb["CT"][dr], in0=sb["CT"][dr], scalar=sb["ET"][:, dr, ch:ch + 1], in1=pc[:, 0:129],
                                                                 op0=ALU.mult, op1=ALU.add), R=[pcb, gm], W=[ct_b[dr]])
                c.op("act", lambda e: e.activation(out=sb["CTb"][dr], in_=sb["CT"][dr], func=AF.Copy), R=[ct_b[dr]], W=[ctb_b[dr]])
            hdst = hsum if dr == 0 else sb["hb"]
            c.op("act", lambda e: e.activation(out=sb["DEN"][:, dr, ch:ch + 1], in_=pn[:, 128:129], func=AF.Abs, scale=sb["EC"][:, dr, ch:ch + 1]),
                 R=[pnb, gm], W=[den_b])
            c.op("act", lambda e: e.activation(out=hdst[:, ch, :], in_=pn[:, 0:128], func=AF.Copy), R=[pnb], W=[hs_b])

    DEN, RR = sb["DEN"], sb["RR"]
    c.op("dve", lambda e: e.tensor_scalar_max(out=RR, in0=DEN, scalar1=1.0), R=[den_b], W=[den_b])
    c.op("dve", lambda e: e.reciprocal(out=RR, in_=RR), R=[den_b], W=[den_b])
    c.op("dve", lambda e: e.tensor_tensor(out=RR, in0=RR, in1=sb["EC"], op=ALU.mult), R=[den_b, gm], W=[den_b])
    c.op("dve", lambda e: e.tensor_tensor(out=hsum, in0=hsum, in1=RR[:, 0, :].unsqueeze(2).to_broadcast([128, NCH, 128]), op=ALU.mult),
         R=[den_b, hs_b], W=[hs_b])
    c.op("pool", lambda e: e.tensor_tensor(out=sb["hb"], in0=sb["hb"], in1=RR[:, 1, :].unsqueeze(2).to_broadcast([128, NCH, 128]), op=ALU.mult),
         R=[den_b, hs_b], W=[hs_b])
    c.op("dve", lambda e: e.tensor_tensor(out=hsum, in0=hsum, in1=sb["hb"], op=ALU.add), R=[hs_b], W=[hs_b])

    nb_ = Buf()
    for ch in range(NCH):
        c.op("act", lambda e: e.activation(out=sb["mjunk"], in_=hsum[:, ch, :], func=AF.Square, accum_out=sb["ssq"][:, ch:ch + 1]), R=[hs_b], W=[nb_])
    c.op("act", lambda e: e.activation(out=sb["rstd"], in_=sb["ssq"], func=AF.Sqrt, scale=1.0 / 128, bias=kc["eps"]), R=[nb_], W=[nb_])
    c.op("dve", lambda e: e.reciprocal(out=sb["rstd"], in_=sb["rstd"]), R=[nb_], W=[nb_])
    yn = sb["yn"]
    yn_b = Buf()
    c.op("dve", lambda e: e.tensor_tensor(out=yn, in0=hsum, in1=sb["rstd"].unsqueeze(2).to_broadcast([128, NCH, 128]), op=ALU.mult),
         R=[hs_b, nb_], W=[yn_b])
    for tb in range(NTB):
        pa, pb = psum.next()
        pT = pa.bitcast(BF16)[:, 0:512]
        c.wait("pe", c._deps([yn_b], [pb]))
        ins = None
        for k in range(4):
            ins = nc.tensor.transpose(pT[:, k * 128:(k + 1) * 128], yn[:, tb * 4 + k, :], kc["ident"])
        tok = c._sig("pe", ins)
        c._mark(tok, [yn_b], [pb])
        c.op("dve", lambda e: e.scalar_tensor_tensor(out=mergedT[:, 2 + hd, tb * 512:(tb + 1) * 512], in0=pT, scalar=sb["mg"][:, 0:1],
                                                     in1=sigo[:, tb * 512:(tb + 1) * 512], op0=ALU.mult, op1=ALU.mult),
             R=[pb, par_b, sig_b], W=[merged_b])


def build_ffn_prog():
    nc = bass.Bass("TRN2", target_bir_lowering=False)
    x_in = nc.dram_tensor("x", [T, D], F32, kind="ExternalInput").ap()
    w_in = nc.dram_tensor("w_in", [D, 2 * DFF], F32, kind="ExternalInput").ap()
    w_out = nc.dram_tensor("w_out", [DFF, D], F32, kind="ExternalInput").ap()
    g = nc.dram_tensor("g", [2, D], F32, kind="ExternalInput").ap()
    x_out = nc.dram_tensor("y", [T, D], F32, kind="ExternalOutput").ap()
    with ExitStack() as es:
        c = Ctx(nc, es)
        k = setup_consts(c, es)
        sb = alloc_ffn_sb(nc, es)
        psum = Rot([es.enter_context(nc.psum_tensor(f"ps{i}", [128, 512], F32))[:] for i in range(8)], excl=True)
        toks = ffn_stage(c, x_in, x_out, w_in, w_out, g[0], g[1], k, psum, sb)
        c.wait("sp", toks)
    return nc


def build_attn_test(j=0):
    nc = bass.Bass("TRN2", target_bir_lowering=False)
    hT_full = nc.dram_tensor("hT", [2, D, T], BF16, kind="ExternalInput").ap()
    wA = nc.dram_tensor("wA", [2, D, 640], F32, kind="ExternalInput").ap()
    out = nc.dram_tensor("mT", [128, S], BF16, kind="ExternalOutput").ap()
    Vd = nc.dram_tensor("Vd", [S, 2, 128], BF16).ap()
    with ExitStack() as es:
        c = Ctx(nc, es)
        mk = setup_mixer_consts(c, es)
        sb = alloc_attn_sb(nc, es)
        mergedT = es.enter_context(nc.sbuf_tensor("mergedT", [128, 4, S], BF16))[:]
        mb = Buf()
        psum = Rot([es.enter_context(nc.psum_tensor(f"ps{i}", [128, 512], F32))[:] for i in range(8)], excl=True)
        hv = lambda r, i: hT_full[r, i * 512:(i + 1) * 512].rearrange("(k p) t -> p k t", p=128)
        attn_pass(c, j, hv, wA, Vd, mk, psum, sb, mergedT, mb)
        t = c.dma("sp", "o", out, mergedT[:, j, :], R=[mb])
        c.wait("sp", [t])
    return nc


def layout_wA(w_in_l, hf):
    sw = np.concatenate([np.arange(8, 16), np.arange(0, 8), np.arange(16, 64)])
    out = np.empty((2, D, 640), np.float32)
    for j in range(2):
        cq, ck, cv, cqs, cks = [], [], [], [], []
        for hh in range(2):
            H = hf * 4 + 2 * j + hh
            base = np.arange(64) + H * 64
            cq.append(base); ck.append(512 + base); cv.append(1024 + base)
            cqs.append(base[sw]); cks.append(512 + base[sw])
        cols = np.concatenate(cq + ck + cqs + cks + cv)
        out[j] = w_in_l[:, cols]
    return out


def layout_wB(w_in_l, hf):
    out = np.empty((2, D, 516), np.float32)
    for hd in range(2):
        H = hf * 2 + hd
        base = np.arange(128) + H * 128
        cols = np.concatenate([1536 + base, 2048 + base, 3072 + base, 2560 + base, 3584 + np.arange(4) * 4 + H])
        out[hd] = w_in_l[:, cols]
    return out


def layout_ml_params(conv_w_l, conv_b_l, gate_bias_l, mgain_l, hf):
    cw = np.empty((2, 128, 2, 5), np.float32)
    cb = np.empty((2, 128, 2), np.float32)
    gb = np.empty((2, 4), np.float32)
    mg = np.empty((2, 128, 1), np.float32)
    for hd in range(2):
        H = hf * 2 + hd
        for qk in range(2):
            ch = qk * 512 + H * 128 + np.arange(128)
            cw[hd, :, qk, :] = conv_w_l[:, ch].T
            cb[hd, :, qk] = conv_b_l[ch]
        gb[hd] = gate_bias_l[:, H]
        mg[hd, :, 0] = mgain_l[H * 128:(H + 1) * 128]
    return cw, cb, gb, mg


def build_ml_test(hd=0):
    nc = bass.Bass("TRN2", target_bir_lowering=False)
    hT_full = nc.dram_tensor("hT", [2, D, T], BF16, kind="ExternalInput").ap()
    wB = nc.dram_tensor("wB", [2, D, 516], F32, kind="ExternalInput").ap()
    cw = nc.dram_tensor("cw", [2, 128, 2, 5], F32, kind="ExternalInput").ap()
    cb = nc.dram_tensor("cb", [2, 128, 2], F32, kind="ExternalInput").ap()
    gb = nc.dram_tensor("gb", [2, 4], F32, kind="ExternalInput").ap()
    mg = nc.dram_tensor("mg", [2, 128, 1], F32, kind="ExternalInput").ap()
    out = nc.dram_tensor("mT", [128, S], BF16, kind="ExternalOutput").ap()
    with ExitStack() as es:
        c = Ctx(nc, es)
        kc = setup_consts(c, es)
        mk = setup_mixer_consts(c, es)
        sb = alloc_ml_sb(nc, es)
        mergedT = es.enter_context(nc.sbuf_tensor("mergedT", [128, 4, S], BF16))[:]
        mb = Buf()
        psum = Rot([es.enter_context(nc.psum_tensor(f"ps{i}", [128, 512], F32))[:] for i in range(8)], excl=True)
        hv = lambda r, i: hT_full[r, i * 512:(i + 1) * 512].rearrange("(k p) t -> p k t", p=128)
        mlstm_pass(c, hd, hv, wB, cw, cb, gb, mg, mk, kc, psum, sb, mergedT, mb)
        t = c.dma("sp", "o", out, mergedT[:, 2 + hd, :], R=[mb])
        c.wait("sp", [t])
    return nc


def _psum(nc, es):
    return Rot([es.enter_context(nc.psum_tensor(f"ps{i}", [128, 512], F32))[:] for i in range(8)], excl=True)


def build_A(first, last):
    nc = bass.Bass("TRN2", target_bir_lowering=False)
    x = nc.dram_tensor("x", [T, D], F32, kind="ExternalInput").ap()
    if not first:
        mT = nc.dram_tensor("mT", [8, 128, T], BF16, kind="ExternalInput").ap()
        w_mo = nc.dram_tensor("w_mo", [D, D], F32, kind="ExternalInput").ap()
        w_in2 = nc.dram_tensor("w_in2", [D, 2 * DFF], F32, kind="ExternalInput").ap()
        w_out2 = nc.dram_tensor("w_out2", [DFF, D], F32, kind="ExternalInput").ap()
        g345 = nc.dram_tensor("g345", [3, D], F32, kind="ExternalInput").ap()
    if not last:
        w_in1 = nc.dram_tensor("w_in1", [D, 2 * DFF], F32, kind="ExternalInput").ap()
        w_out1 = nc.dram_tensor("w_out1", [DFF, D], F32, kind="ExternalInput").ap()
        g012 = nc.dram_tensor("g012", [3, D], F32, kind="ExternalInput").ap()
        hTo = nc.dram_tensor("hTo", [D, T], BF16, kind="ExternalOutput").ap()
    xo = nc.dram_tensor("xo", [T, D], F32, kind="ExternalOutput").ap()
    xa = nc.dram_tensor("xa", [T, D], F32).ap()
    xb = nc.dram_tensor("xb", [T, D], F32).ap()
    with ExitStack() as es:
        c = Ctx(nc, es)
        kc = setup_consts(c, es)
        sb = alloc_ffn_sb(nc, es)
        psum = _psum(nc, es)
        cur = x
        if not first:
            mixout_stage(c, cur, xa, mT, w_mo, g345[0], kc, psum, sb)
            c.barrier()
            ffn_stage(c, xa, xo if last else xb, w_in2, w_out2, g345[1], g345[2], kc, psum, sb)
            c.barrier()
            cur = xb
        if not last:
            ffn_stage(c, cur, xo, w_in1, w_out1, g012[0], g012[1], kc, psum, sb)
            c.barrier()
            hT_stage(c, xo, g012[2], hTo, kc, psum, sb)
        c.barrier()
    return nc


def build_B():
    nc = bass.Bass("TRN2", target_bir_lowering=False)
    hT_full = nc.dram_tensor("hT", [2, D, T], BF16, kind="ExternalInput").ap()
    wA = nc.dram_tensor("wA", [2, D, 640], F32, kind="ExternalInput").ap()
    wB = nc.dram_tensor("wB", [2, D, 516], F32, kind="ExternalInput").ap()
    cw = nc.dram_tensor("cw", [2, 128, 2, 5], F32, kind="ExternalInput").ap()
    cb = nc.dram_tensor("cb", [2, 128, 2], F32, kind="ExternalInput").ap()
    gb = nc.dram_tensor("gb", [2, 4], F32, kind="ExternalInput").ap()
    mg = nc.dram_tensor("mg", [2, 128, 1], F32, kind="ExternalInput").ap()
    out = nc.dram_tensor("mT", [4, 128, S], BF16, kind="ExternalOutput").ap()
    Vd = nc.dram_tensor("Vd", [S, 2, 128], BF16).ap()
    with ExitStack() as es:
        c = Ctx(nc, es)
        kc = setup_consts(c, es)
        mk = setup_mixer_consts(c, es)
        mergedT = es.enter_context(nc.sbuf_tensor("mergedT", [128, 4, S], BF16))[:]
        mb = Buf()
        psum = _psum(nc, es)
        hv = lambda r, i: hT_full[r, i * 512:(i + 1) * 512].rearrange("(k p) t -> p k t", p=128)
        mixer_core(c, nc, hv, wA, wB, cw, cb, gb, mg, Vd, mk, kc, psum, mergedT, mb)
        c.dma("sp", "mTo", out.rearrange("j p s -> p j s"), mergedT, R=[mb])
        c.barrier()
    return nc


def mixer_core(c, nc, hv, wA, wB, cw, cb, gb, mg, Vd, mk, kc, psum, mergedT, mb, hdep=None, after_tile=None):
    excl = ("cc", "mTo")
    with ExitStack() as es2:
        sba = alloc_attn_sb(nc, es2)
        for j in range(2):
            attn_pass(c, j, hv, wA, Vd, mk, psum, sba, mergedT, mb, hdep)
            if after_tile:
                after_tile(j)
            c.barrier(exclude=excl)
    with ExitStack() as es2:
        sbm = alloc_ml_sb(nc, es2)
        for hd in range(2):
            mlstm_pass(c, hd, hv, wB, cw, cb, gb, mg, mk, kc, psum, sbm, mergedT, mb, hdep)
            if after_tile:
                after_tile(2 + hd)
            c.barrier(exclude=excl)


def layout_wmo(w_out_l):
    rows = []
    for r in range(2):
        for j in range(2):
            for hh in range(2):
                H = r * 4 + 2 * j + hh
                rows.append(np.arange(64) + H * 64)
        for hd in range(2):
            H = r * 2 + hd
            rows.append(512 + np.arange(128) + H * 128)
    return np.ascontiguousarray(w_out_l[np.concatenate(rows)])


_PROGS = {}


def _prog(key, fn):
    if key not in _PROGS:
        _PROGS[key] = fn()
    return _PROGS[key]


DEPTH = 4
NCORES = 8
PAIRS = [[0, 1], [2, 3], [4, 5], [6, 7]]


def build_fused(depth=DEPTH):
    nc = bass.Bass("TRN2", target_bir_lowering=False)
    dt_in = lambda name, shape, dt=F32: nc.dram_tensor(name, shape, dt, kind="ExternalInput").ap()
    x = dt_in("x", [T, D])
    sel = dt_in("sel", [128, 2])
    ng = dt_in("ng", [depth, 6, D])
    fwi = dt_in("fwi", [depth, 2, D, 2 * DFF])
    fwo = dt_in("fwo", [depth, 2, DFF, D])
    wmo = dt_in("wmo", [depth, D, D])
    wA = dt_in("wA", [depth, 2, D, 640])
    wB = dt_in("wB", [depth, 2, D, 516])
    cw = dt_in("cw", [depth, 2, 128, 2, 5])
    cb = dt_in("cb", [depth, 2, 128, 2])
    gb = dt_in("gb", [depth, 2, 4])
    mg = dt_in("mg", [depth, 2, 128, 1])
    xo = nc.dram_tensor("xo", [T, D], F32, kind="ExternalOutput").ap()
    xs_ = [nc.dram_tensor(f"xs{i}", [T, D], F32).ap() for i in range(3)]
    hT_my = nc.dram_tensor("hT_my", [D, T], BF16).ap()
    hT_full = nc.dram_tensor("hT_full", [2, 2, 512, T], BF16).ap()
    mT_my = nc.dram_tensor("mT_my", [4, 128, S], BF16).ap()
    mT_all = nc.dram_tensor("mT_all", [4, 2, 128, S], BF16).ap()
    Vd = nc.dram_tensor("Vd", [S, 2, 128], BF16).ap()
    rope_d = [nc.dram_tensor(f"rope{i}", [128, S], F32).ap() for i in range(2)]
    with ExitStack() as es:
        c = Ctx(nc, es)
        kc = setup_consts(c, es)
        mk = setup_mixer_consts(c, es, rope_dram=rope_d)
        psum = _psum(nc, es)
        hv = lambda r, i: hT_full[i, r].rearrange("(k p) t -> p k t", p=128)
        hT_buf, mTm_buf, mTa_buf = Buf(), Buf(), Buf()
        cur = x
        for l in range(depth):
            with ExitStack() as es2:
                sb = alloc_ffn_sb(nc, es2)
                ffn_stage(c, cur, xs_[0], fwi[l, 0], fwo[l, 0], ng[l, 0], ng[l, 1], kc, psum, sb)
                c.barrier()
                hT_b = prenorm_hT(c, xs_[0], ng[l, 2], kc, psum, sb)
                hbufs = [(Buf(), Buf()) for _ in range(2)]
                for i2 in range(2):
                    c.dma("sp", ("hTo", i2), hT_my[i2 * 512:(i2 + 1) * 512].rearrange("(k p) t -> p k t", p=128), sb["hT"][:, 4 * i2:4 * i2 + 4, :],
                          R=hT_b, W=[hbufs[i2][0]])
                    c.collective("AllGather", hT_my[i2 * 512:(i2 + 1) * 512], hT_full[i2].rearrange("r d t -> (r d) t"), PAIRS,
                                 R=[hbufs[i2][0]], W=[hbufs[i2][1]])
                c.barrier()
            with ExitStack() as es2:
                _TAG[0] += 1
                mk["ropeC"] = _t(nc, es2, "ropeC", [128, S], F32)
                mk["ropeS"] = _t(nc, es2, "ropeS", [128, S], F32)
                mergedT = _t(nc, es2, "mergedT", [128, 4, S], BF16)
                c.dma("sp", "ropeC", mk["ropeC"], rope_d[0], W=[mk["b"]])
                c.dma("sp", "ropeS", mk["ropeS"], rope_d[1], W=[mk["b"]])
                mb = Buf()
                tile_bufs = [(Buf(), Buf()) for _ in range(4)]

                def ship(j2, mergedT=mergedT, mb=mb, tile_bufs=tile_bufs):
                    b_in, b_out = tile_bufs[j2]
                    c.dma("sp", ("mTo", j2), mT_my[j2], mergedT[:, j2, :], R=[mb], W=[b_in])
                    c.collective("AllGather", mT_my[j2], mT_all[j2].rearrange("r p s -> (r p) s"), PAIRS, R=[b_in], W=[b_out])

                mixer_core(c, nc, hv, wA[l], wB[l], cw[l], cb[l], gb[l], mg[l], Vd, mk, kc, psum, mergedT, mb)
                c.barrier()
                for j2 in range(4):
                    ship(j2)
                c.barrier()
            with ExitStack() as es2:
                sb = alloc_ffn_sb(nc, es2)
                mixout_stage(c, xs_[0], xs_[1], [mT_all[ch % 4, ch // 4] for ch in range(8)], wmo[l], ng[l, 3], kc, psum, sb, sel=sel)
                c.barrier()
                nxt = xo if l == depth - 1 else xs_[2]
                ffn_stage(c, xs_[1], nxt, fwi[l, 1], fwo[l, 1], ng[l, 4], ng[l, 5], kc, psum, sb)
                c.barrier()
                cur = nxt
    return nc


def kernel(x, norm_gain, ffn_w_in, ffn_w_out, mix_w_in, conv_w, conv_b, gate_bias, mlstm_norm_gain, mix_w_out):
    x = np.asarray(x, np.float32)
    f32 = lambda a: np.ascontiguousarray(np.asarray(a, np.float32))
    norm_gain, ffn_w_in, ffn_w_out, mix_w_in = f32(norm_gain), f32(ffn_w_in), f32(ffn_w_out), f32(mix_w_in)
    conv_w, conv_b, gate_bias, mlstm_norm_gain, mix_w_out = f32(conv_w), f32(conv_b), f32(gate_bias), f32(mlstm_norm_gain), f32(mix_w_out)
    cores = list(range(NCORES))
    wmo = np.stack([layout_wmo(mix_w_out[l]) for l in range(DEPTH)])
    per_hf = []
    for hf in range(2):
        mlp = [layout_ml_params(conv_w[l], conv_b[l], gate_bias[l], mlstm_norm_gain[l], hf) for l in range(DEPTH)]
        selv = np.zeros((128, 2), np.float32)
        selv[:, hf] = 1.0
        per_hf.append({"wA": np.stack([layout_wA(mix_w_in[l], hf) for l in range(DEPTH)]),
                       "wB": np.stack([layout_wB(mix_w_in[l], hf) for l in range(DEPTH)]),
                       "cw": np.stack([m[0] for m in mlp]), "cb": np.stack([m[1] for m in mlp]),
                       "gb": np.stack([m[2] for m in mlp]), "mg": np.stack([m[3] for m in mlp]), "sel": selv})
    in_maps = []
    for c in cores:
        m = {"x": np.ascontiguousarray(x[c // 2, (c % 2) * T:(c % 2 + 1) * T]), "ng": norm_gain, "fwi": ffn_w_in, "fwo": ffn_w_out, "wmo": wmo}
        m.update(per_hf[c % 2])
        in_maps.append(m)
    nc = _prog("fused", build_fused)
    res = run_bass_kernel_spmd(nc, in_maps, core_ids=cores).results
    out = np.empty((4, S, D), np.float32)
    for c in cores:
        out[c // 2, (c % 2) * T:(c % 2 + 1) * T] = res[c]["xo"]
    return out


def kernel_unfused(x, norm_gain, ffn_w_in, ffn_w_out, mix_w_in, conv_w, conv_b, gate_bias, mlstm_norm_gain, mix_w_out):
    x = np.asarray(x, np.float32)
    f32 = lambda a: np.ascontiguousarray(np.asarray(a, np.float32))
    norm_gain, ffn_w_in, ffn_w_out, mix_w_in = f32(norm_gain), f32(ffn_w_in), f32(ffn_w_out), f32(mix_w_in)
    conv_w, conv_b, gate_bias, mlstm_norm_gain, mix_w_out = f32(conv_w), f32(conv_b), f32(gate_bias), f32(mlstm_norm_gain), f32(mix_w_out)
    cores = list(range(NCORES))
    xc = [np.ascontiguousarray(x[c // 2, (c % 2) * T:(c % 2 + 1) * T]) for c in cores]
    mT_my = None
    for l in range(DEPTH + 1):
        first, last = l == 0, l == DEPTH
        in_maps = []
        for c in cores:
            m = {"x": xc[c]}
            if not first:
                m["mT"] = mT_my[c]
                m["w_mo"] = layout_wmo(mix_w_out[l - 1])
                m["w_in2"] = ffn_w_in[l - 1, 1]
                m["w_out2"] = ffn_w_out[l - 1, 1]
                m["g345"] = np.ascontiguousarray(norm_gain[l - 1, 3:6])
            if not last:
                m["w_in1"] = ffn_w_in[l, 0]
                m["w_out1"] = ffn_w_out[l, 0]
                m["g012"] = np.ascontiguousarray(norm_gain[l, 0:3])
            in_maps.append(m)
        nc = _prog(("A", first, last), lambda: build_A(first, last))
        res = run_bass_kernel_spmd(nc, in_maps, core_ids=cores).results
        xc = [res[c]["xo"] for c in cores]
        if last:
            break
        hT = [res[c]["hTo"] for c in cores]
        in_maps = []
        for c in cores:
            b, hf = c // 2, c % 2
            cw_, cb_, gb_, mg_ = layout_ml_params(conv_w[l], conv_b[l], gate_bias[l], mlstm_norm_gain[l], hf)
            in_maps.append({"hT": np.ascontiguousarray(np.stack([hT[2 * b], hT[2 * b + 1]])),
                            "wA": layout_wA(mix_w_in[l], hf), "wB": layout_wB(mix_w_in[l], hf),
                            "cw": cw_, "cb": cb_, "gb": gb_, "mg": mg_})
        nc = _prog("B", build_B)
        res = run_bass_kernel_spmd(nc, in_maps, core_ids=cores).results
        mT = [res[c]["mT"] for c in cores]
        mT_my = []
        for c in cores:
            b, hf = c // 2, c % 2
            mT_my.append(np.ascontiguousarray(np.concatenate([mT[2 * b][:, :, hf * T:(hf + 1) * T], mT[2 * b + 1][:, :, hf * T:(hf + 1) * T]], axis=0)))
    out = np.empty((4, S, D), np.float32)
    for c in cores:
        out[c // 2, (c % 2) * T:(c % 2 + 1) * T] = xc[c]
    return out
```
